# Optimizing a Trainium2 kernel written in Bass

```python
import math
import jax
import jax.numpy as jnp
from jax import lax
import numpy as np

D_MODEL = 1024
BATCH = 16
SEQ = 2048
DEPTH = 2

ROPE_THETA = 500000.0
NORM_EPS = 1e-6
Q_BLOCK = 128

MLA_HEADS = 8
MLA_NOPE = 64
MLA_ROPE = 32
MLA_V = 64
MLA_Q_LORA = 256
MLA_KV_LORA = 128
CONV_DIM = 512
CONV_WIDTH = 3
DSA_HEADS = 8
DSA_HEAD_DIM = 64
DSA_ROT = DSA_HEAD_DIM // 4
IDX_HEADS = 8
IDX_DIM = 64
IDX_ROT = IDX_DIM // 4
TOPK_MAX = 256
N_BRANCH = 3
BRANCH_WIDTH = 512
IN_SIZES = (MLA_Q_LORA, MLA_KV_LORA + MLA_ROPE, 3 * CONV_DIM, DSA_HEADS * DSA_HEAD_DIM,
            2 * DSA_HEAD_DIM, IDX_HEADS * IDX_DIM, IDX_DIM, IDX_HEADS, N_BRANCH * D_MODEL)
IN_COLS = sum(IN_SIZES)
N_EXPERTS = 32
TOP_K = 4
D_EXPERT = D_MODEL
SWIGLU_ALPHA = 1.702
SWIGLU_LIMIT = 7.0
MOE_BLOCK = 512

kernel_name = "hybrid_mla_conv_dsa_moe_adaln"


def rmsnorm(x, g):
    xf = x.astype(jnp.float32)
    y = xf * lax.rsqrt(jnp.mean(xf * xf, axis=-1, keepdims=True) + NORM_EPS)
    return (y * g.astype(jnp.float32)).astype(x.dtype)


def rope(x, pos, n_rot):
    half = n_rot // 2
    inv_freq = jnp.exp(-math.log(ROPE_THETA) * jnp.arange(half, dtype=jnp.float32) * (2.0 / n_rot))
    ang = pos.astype(jnp.float32)[..., None] * inv_freq
    if x.ndim == 4:
        ang = ang[:, :, None, :]
    cos = jnp.cos(ang).astype(x.dtype)
    sin = jnp.sin(ang).astype(x.dtype)
    x1 = x[..., :half]
    x2 = x[..., half:n_rot]
    return jnp.concatenate([x1 * cos - x2 * sin, x2 * cos + x1 * sin, x[..., n_rot:]], axis=-1)


def split_cols(a, sizes):
    out, start = [], 0
    for n in sizes:
        out.append(a[..., start:start + n])
        start += n
    return out


def to_blocks(a, nb):
    return a.reshape((a.shape[0], nb, Q_BLOCK) + a.shape[2:]).swapaxes(0, 1)


def from_blocks(a):
    a = a.swapaxes(0, 1)
    return a.reshape((a.shape[0], a.shape[1] * a.shape[2]) + a.shape[3:])


def blocked_causal_attention(q, k, v, scale):
    S = q.shape[1]
    nb = S // Q_BLOCK
    kpos = jnp.arange(S)

    def one(args):
        q_b, start = args
        qpos = start + jnp.arange(Q_BLOCK)
        s = jnp.einsum('bqhd,bkhd->bhqk', q_b, k).astype(jnp.float32) * scale
        s = jnp.where((kpos[None, :] <= qpos[:, None])[None, None], s, -jnp.inf)
        p = jax.nn.softmax(s, axis=-1).astype(v.dtype)
        return jnp.einsum('bhqk,bkhd->bqhd', p, v)

    out = lax.map(one, (to_blocks(q, nb), jnp.arange(nb) * Q_BLOCK))
    return from_blocks(out)


def indexer_sparse_attention(q, k, v, qi, ki, wi):
    S = q.shape[1]
    nb = S // Q_BLOCK
    topk = min(TOPK_MAX, S // 4)
    kpos = jnp.arange(S)
    gather = jax.vmap(lambda kb, ib: kb[ib])
    scale = DSA_HEAD_DIM ** -0.5

    def one(args):
        q_b, qi_b, wi_b, start = args
        qpos = start + jnp.arange(Q_BLOCK)
        logits = jnp.einsum('bqhd,bkd->bqhk', qi_b, ki).astype(jnp.float32) * (IDX_DIM ** -0.5)
        score = jnp.einsum('bqhk,bqh->bqk', jax.nn.relu(logits), wi_b.astype(jnp.float32)) * (IDX_HEADS ** -0.5)
        score = jnp.where((kpos[None, :] <= qpos[:, None])[None], score, -jnp.inf)
        _, sel = lax.top_k(score, topk)
        valid = sel <= qpos[None, :, None]
        ks = gather(k, sel)
        vs = gather(v, sel)
        s = jnp.einsum('bqhd,bqkd->bqhk', q_b, ks).astype(jnp.float32) * scale
        s = jnp.where(valid[:, :, None, :], s, -jnp.inf)
        p = jax.nn.softmax(s, axis=-1).astype(v.dtype)
        return jnp.einsum('bqhk,bqkd->bqhd', p, vs)

    out = lax.map(one, (to_blocks(q, nb), to_blocks(qi, nb), to_blocks(wi, nb), jnp.arange(nb) * Q_BLOCK))
    return from_blocks(out)


def causal_depthwise_conv(u, w):
    return lax.conv_general_dilated(u, w[:, None, :].astype(u.dtype), window_strides=(1,),
                                    padding=[(CONV_WIDTH - 1, 0)],
                                    dimension_numbers=('NWC', 'WIO', 'NWC'),
                                    feature_group_count=u.shape[-1])


def token_mixer(h, pos, w_in, b_gate, q_norm_g, w_uq, kv_norm_g, w_ukv, conv_w, w_branch, w_out):
    B, S, D = h.shape
    proj = h @ w_in
    q_lat, kv_lat, conv_in, dq, dkv, iq, ik, iw, gate_logits = split_cols(proj, IN_SIZES)

    cq = rmsnorm(q_lat, q_norm_g)
    qa = rope((cq @ w_uq).reshape(B, S, MLA_HEADS, MLA_ROPE + MLA_NOPE), pos, MLA_ROPE)
    ckv = rmsnorm(kv_lat[..., :MLA_KV_LORA], kv_norm_g)
    k_pe = rope(kv_lat[..., MLA_KV_LORA:], pos, MLA_ROPE)
    kv = (ckv @ w_ukv).reshape(B, S, MLA_HEADS, MLA_NOPE + MLA_V)
    ka = jnp.concatenate([jnp.broadcast_to(k_pe[:, :, None, :], (B, S, MLA_HEADS, MLA_ROPE)),
                          kv[..., :MLA_NOPE]], axis=-1)
    o_a = blocked_causal_attention(qa, ka, kv[..., MLA_NOPE:], (MLA_NOPE + MLA_ROPE) ** -0.5)
    o_a = o_a.reshape(B, S, BRANCH_WIDTH)

    g_b, g_c, hc = split_cols(conv_in, (CONV_DIM, CONV_DIM, CONV_DIM))
    o_b = g_b * causal_depthwise_conv(g_c * hc, conv_w)

    qc = rope(dq.reshape(B, S, DSA_HEADS, DSA_HEAD_DIM), pos, DSA_ROT)
    kc = rope(dkv[..., :DSA_HEAD_DIM], pos, DSA_ROT)
    vc = dkv[..., DSA_HEAD_DIM:]
    qi = rope(iq.reshape(B, S, IDX_HEADS, IDX_DIM), pos, IDX_ROT)
    ki = rope(ik, pos, IDX_ROT)
    o_c = indexer_sparse_attention(qc, kc, vc, qi, ki, iw).reshape(B, S, BRANCH_WIDTH)

    o = jnp.stack([o_a, o_b, o_c], axis=2)
    y = jnp.einsum('bsnw,nwd->bsnd', o, w_branch)
    g = jax.nn.sigmoid(gate_logits + b_gate).reshape(B, S, N_BRANCH, D)
    return jnp.sum(g * y, axis=2) @ w_out


def moe(xf, router_w, router_b, w1, b1, w2, b2):
    T, D = xf.shape
    logits = (xf @ router_w + router_b).astype(jnp.float32)
    top_vals, top_idx = lax.top_k(logits, TOP_K)
    gates = jax.nn.softmax(top_vals, axis=-1)
    N = T * TOP_K
    n_blocks = (N + N_EXPERTS * (MOE_BLOCK - 1) + MOE_BLOCK - 1) // MOE_BLOCK
    P = n_blocks * MOE_BLOCK
    slot_expert = top_idx.reshape(N)
    slot_token = jnp.arange(N, dtype=jnp.int32) // TOP_K
    order = jnp.argsort(slot_expert)
    e_sorted = slot_expert[order]
    counts = jnp.bincount(slot_expert, length=N_EXPERTS)
    padded = (counts + MOE_BLOCK - 1) // MOE_BLOCK * MOE_BLOCK
    starts = jnp.cumsum(counts) - counts
    pends = jnp.cumsum(padded)
    pstarts = pends - padded
    dest = pstarts[e_sorted] + (jnp.arange(N, dtype=jnp.int32) - starts[e_sorted])
    tok_pad = jnp.full((P,), T, dtype=jnp.int32).at[dest].set(slot_token[order])
    gate_pad = jnp.zeros((P,), jnp.float32).at[dest].set(gates.reshape(N)[order])
    block_expert = jnp.minimum(jnp.searchsorted(pends, jnp.arange(n_blocks) * MOE_BLOCK, side='right'),
                               N_EXPERTS - 1)
    x_pad = jnp.concatenate([xf, jnp.zeros((1, D), xf.dtype)], axis=0)
    xb = x_pad[tok_pad].reshape(n_blocks, MOE_BLOCK, D)

    def expert_block(args):
        xblk, e = args
        hgu = xblk @ w1[e] + b1[e]
        gate = jnp.minimum(hgu[..., 0::2], SWIGLU_LIMIT)
        up = jnp.clip(hgu[..., 1::2], -SWIGLU_LIMIT, SWIGLU_LIMIT)
        glu = gate * jax.nn.sigmoid(gate * SWIGLU_ALPHA)
        return ((up + 1.0) * glu) @ w2[e] + b2[e]

    yb = lax.map(expert_block, (xb, block_expert)).reshape(P, D)
    out = jnp.zeros((T + 1, D), xf.dtype).at[tok_pad].add(yb * gate_pad[:, None].astype(xf.dtype))
    return out[:T]


def setup_inputs(seed: int = 0) -> dict:
    key = jax.random.key(seed)
    ks = jax.random.split(key, 32)
    L, D, E, F = DEPTH, D_MODEL, N_EXPERTS, D_EXPERT

    def nrm(k, shape, scale):
        return jax.random.normal(k, shape, jnp.float32) * scale

    x = nrm(ks[0], (BATCH, SEQ, D), 1.0)
    c = nrm(ks[1], (BATCH, D), 1.0)
    positions = (jnp.arange(SEQ, dtype=jnp.int32)[None, :]
                 + jax.random.randint(ks[2], (BATCH, 1), 0, 4096, dtype=jnp.int32))
    return {
        "x": x,
        "c": c,
        "positions": positions,
        "norm1_g": 1.0 + nrm(ks[3], (L, D), 0.05),
        "norm2_g": 1.0 + nrm(ks[4], (L, D), 0.05),
        "w_ada": nrm(ks[5], (L, D, 6 * D), 0.5 * D ** -0.5),
        "b_ada": nrm(ks[6], (L, 6 * D), 0.02),
        "w_in": nrm(ks[7], (L, D, IN_COLS), D ** -0.5),
        "b_gate": nrm(ks[8], (L, N_BRANCH * D), 0.02),
        "mla_q_norm": 1.0 + nrm(ks[9], (L, MLA_Q_LORA), 0.05),
        "mla_w_uq": nrm(ks[10], (L, MLA_Q_LORA, MLA_HEADS * (MLA_ROPE + MLA_NOPE)), MLA_Q_LORA ** -0.5),
        "mla_kv_norm": 1.0 + nrm(ks[11], (L, MLA_KV_LORA), 0.05),
        "mla_w_ukv": nrm(ks[12], (L, MLA_KV_LORA, MLA_HEADS * (MLA_NOPE + MLA_V)), MLA_KV_LORA ** -0.5),
        "conv_w": nrm(ks[13], (L, CONV_WIDTH, CONV_DIM), CONV_WIDTH ** -0.5),
        "w_branch": nrm(ks[14], (L, N_BRANCH, BRANCH_WIDTH, D), BRANCH_WIDTH ** -0.5),
        "w_out": nrm(ks[15], (L, D, D), D ** -0.5),
        "router_w": nrm(ks[16], (L, D, E), D ** -0.5),
        "router_b": nrm(ks[17], (L, E), 0.01),
        "exp_w1": nrm(ks[18], (L, E, D, 2 * F), D ** -0.5),
        "exp_b1": nrm(ks[19], (L, E, 2 * F), 0.01),
        "exp_w2": nrm(ks[20], (L, E, F, D), F ** -0.5),
        "exp_b2": nrm(ks[21], (L, E, D), 0.01),
        "final_g": 1.0 + nrm(ks[22], (D,), 0.05),
    }


def reference(x, c, positions, norm1_g, norm2_g, w_ada, b_ada, w_in, b_gate, mla_q_norm, mla_w_uq,
              mla_kv_norm, mla_w_ukv, conv_w, w_branch, w_out, router_w, router_b, exp_w1, exp_b1,
              exp_w2, exp_b2, final_g):
    B, S, D = x.shape
    c_act = jax.nn.silu(c)
    for l in range(DEPTH):
        mod = (c_act @ w_ada[l] + b_ada[l])[:, None, :]
        sh1, sc1, g1, sh2, sc2, g2 = jnp.split(mod, 6, axis=-1)
        h = rmsnorm(x, norm1_g[l]) * (1.0 + sc1) + sh1
        x = x + g1 * token_mixer(h, positions, w_in[l], b_gate[l], mla_q_norm[l], mla_w_uq[l],
                                 mla_kv_norm[l], mla_w_ukv[l], conv_w[l], w_branch[l], w_out[l])
        h = rmsnorm(x, norm2_g[l]) * (1.0 + sc2) + sh2
        y = moe(h.reshape(B * S, D), router_w[l], router_b[l], exp_w1[l], exp_b1[l], exp_w2[l], exp_b2[l])
        x = x + g2 * y.reshape(B, S, D)
    return rmsnorm(x, final_g)
```

```python
import math
import types
import numpy as np
from contextlib import ExitStack
import concourse.bass as bass
import concourse.mybir as mybir
from concourse.bass_utils import run_bass_kernel_spmd

F32 = mybir.dt.float32
BF16 = mybir.dt.bfloat16
I32 = mybir.dt.int32
AF = mybir.ActivationFunctionType
ALU = mybir.AluOpType
AX = mybir.AxisListType

S = 2048
D = 1024
NT = 16
NCH = 4
IN_COLS = 6248
NEXP = 32
BIG = 1.0e30


class Buf:
    __slots__ = ("name", "w", "r", "excl")

    def __init__(self, name="", excl=False):
        self.name = name
        self.w = None
        self.r = []
        self.excl = excl


class Prog:
    def __init__(self, nc, n_dma_sems=40):
        self.nc = nc
        self.ops = []
        self.n_dma_sems = n_dma_sems

    @staticmethod
    def _freeze(fn, depth=0):
        if not isinstance(fn, types.FunctionType) or fn.__closure__ is None or depth > 3:
            return fn
        cells = []
        for c in fn.__closure__:
            try:
                v = c.cell_contents
            except ValueError:
                cells.append(c)
                continue
            if isinstance(v, types.FunctionType):
                v = Prog._freeze(v, depth + 1)
            cells.append(types.CellType(v))
        g = types.FunctionType(fn.__code__, fn.__globals__, fn.__name__, fn.__defaults__, tuple(cells))
        g.__kwdefaults__ = fn.__kwdefaults__
        return g

    def add(self, eng, fn, reads=(), writes=(), dma=False):
        self.ops.append((eng, Prog._freeze(fn), tuple(reads), tuple(writes), dma))

    def pe(self, fn, reads=(), writes=()):
        self.add("pe", fn, reads, writes)

    def act(self, fn, reads=(), writes=()):
        self.add("act", fn, reads, writes)

    def dve(self, fn, reads=(), writes=()):
        self.add("dve", fn, reads, writes)

    def pool(self, fn, reads=(), writes=()):
        self.add("pool", fn, reads, writes)

    def dma(self, q, fn, reads=(), writes=()):
        self.add(q, fn, reads, writes, dma=True)

    def barrier(self):
        self.ops.append(("BAR", None, (), (), False))

    def emit(self, final_bufs=()):
        nc = self.nc
        ops = self.ops
        n = len(ops)
        deps = [None] * n
        signal = [False] * n
        last_on = {}
        dmas_since = []
        bar_deps = {}
        for i, (eng, fn, reads, writes, dma) in enumerate(ops):
            if eng == "BAR":
                bd = set(last_on.values()) | set(dmas_since)
                for j in bd:
                    signal[j] = True
                bar_deps[i] = bd
                dmas_since = []
                continue
            d = set()
            for b in reads:
                if b.w is not None:
                    d.add(b.w)
                if b.excl:
                    d.update(j for j in b.r if ops[j][0] != eng)
            for b in writes:
                if b.w is not None:
                    d.add(b.w)
                d.update(b.r)
            d.discard(i)
            keep = set()
            for j in d:
                jeng, _, jr, jw, jdma = ops[j]
                if not dma and not jdma and jeng == eng:
                    if eng == "pe":
                        continue
                keep.add(j)
            deps[i] = keep
            for j in keep:
                signal[j] = True
            for b in reads:
                if not dma:
                    b.r = [j for j in b.r if ops[j][4] or ops[j][0] != eng]
                b.r.append(i)
            for b in writes:
                b.w = i
                b.r = []
            if dma:
                dmas_since.append(i)
            else:
                last_on[eng] = i
        final_deps = set()
        for b in final_bufs:
            if b.w is not None:
                final_deps.add(b.w)
                signal[b.w] = True

        with ExitStack() as es:
            SEM_LIMIT = 2000
            tot = {e: 0 for e in ("pe", "act", "dve", "pool")}
            for i, (eng, fn, reads, writes, dma) in enumerate(ops):
                if eng in tot and not dma and signal[i]:
                    tot[eng] += 1
            csem = {e: [es.enter_context(nc.semaphore("cs_%s%d" % (e, k))) for k in range(tot[e] // SEM_LIMIT + 1)]
                    for e in tot}
            dsem = [es.enter_context(nc.semaphore("ds%d" % k)) for k in range(self.n_dma_sems)]
            ccount = {e: 0 for e in csem}
            dcount = [0] * self.n_dma_sems
            token = [None] * n
            prevtok = [None] * n
            nd = 0
            nsw = 0
            nhw = 0
            half = self.n_dma_sems // 2
            for i, (eng, fn, reads, writes, dma) in enumerate(ops):
                if eng == "BAR":
                    continue
                if dma:
                    if eng == "pool":
                        k = nsw % half
                        nsw += 1
                    else:
                        k = half + nhw % (self.n_dma_sems - half)
                        nhw += 1
                    nd += 1
                    if dcount[k] > 0:
                        prevtok[i] = (dsem[k], dcount[k])
                    dcount[k] += 16
                    token[i] = (dsem[k], dcount[k])
                elif signal[i]:
                    c = ccount[eng]
                    ccount[eng] += 1
                    token[i] = (csem[eng][c // SEM_LIMIT], c % SEM_LIMIT + 1)
            self.stats = {"n_ops": n, "n_dma": nd, "ccount": dict(ccount)}

            def do_waits(e, waited, toks):
                best = {}
                for (s, v) in toks:
                    if best.get(s.num, (None, 0))[1] < v:
                        best[s.num] = (s, v)
                for num, (s, v) in best.items():
                    if waited.get(num, 0) < v:
                        e.wait_ge(s, v)
                        waited[num] = v

            def emit_engine(ename, e):
                waited = {}
                for i, (eng, fn, reads, writes, dma) in enumerate(ops):
                    if eng == "BAR":
                        do_waits(e, waited, [token[j] for j in bar_deps[i]])
                        continue
                    if eng != ename:
                        continue
                    toks = [token[j] for j in deps[i]]
                    if prevtok[i] is not None:
                        toks.append(prevtok[i])
                    do_waits(e, waited, toks)
                    ins = fn(e)
                    if token[i] is not None:
                        s, v = token[i]
                        ins.then_inc(s, 16 if dma else 1)
                if ename == "sp":
                    do_waits(e, waited, [token[j] for j in final_deps])

            with nc.Block() as block:
                @block.tensor
                def _(e):
                    emit_engine("pe", e)

                @block.scalar
                def _(e):
                    emit_engine("act", e)

                @block.vector
                def _(e):
                    emit_engine("dve", e)

                @block.gpsimd
                def _(e):
                    emit_engine("pool", e)

                @block.sync
                def _(e):
                    emit_engine("sp", e)


class Ring:
    def __init__(self, items):
        self.items = items
        self.i = 0

    def get(self):
        it = self.items[self.i % len(self.items)]
        self.i += 1
        return it


def build(nseq, depth=2, phases=None, dbg=False, nlayers=None):
    nc = bass.Bass("TRN2", target_bir_lowering=False)
    P = Prog(nc)
    root = ExitStack()
    ALL_PHASES = ("prologue", "norm1", "mla", "conv", "d1", "d2", "merge", "moe", "d1_score", "d1_bis", "d1_mask", "d1p_q", "d1p_k", "d1p_w")
    run_phases = set(ALL_PHASES if phases is None else phases)

    def phase(name):
        if name in run_phases:
            with ExitStack() as es_:
                yield es_

    def din(name, shape, dt=F32):
        return nc.dram_tensor(name, list(shape), dt, kind="ExternalInput").ap()

    def dscr(name, shape, dt=BF16):
        if dbg:
            return nc.dram_tensor(name, list(shape), dt, kind="ExternalOutput").ap()
        return nc.dram_tensor(name, list(shape), dt).ap()

    _cnt = [0]

    def sb(es, name, shape, dt=F32):
        _cnt[0] += 1
        return es.enter_context(nc.sbuf_tensor("s%d_%s" % (_cnt[0], name), list(shape), dt))

    x_d = din("x", [nseq, S, D])
    cT_d = din("cT", [128, 8, nseq])
    pos_d = din("pos", [nseq, S], I32)
    w_ada_d = din("w_ada", [depth, D, 6 * D])
    b_adaT_d = din("b_adaT", [depth, 128, 48])
    n1g_d = din("n1g", [depth, 128, 8])
    n2g_d = din("n2g", [depth, 128, 8])
    fg_d = din("fg", [128, 8])
    w_in_d = din("w_in", [depth, D, IN_COLS])
    w_inr_d = din("w_in_rot", [depth, D, 336])
    b_gateT_d = din("b_gateT", [depth, 128, 24])
    qng_d = din("qng", [depth, 128, 2])
    w_uq_d = din("w_uq", [depth, 256, 768])
    w_uqr_d = din("w_uq_rot", [depth, 256, 256])
    kvng_d = din("kvng", [depth, 128, 1])
    w_ukv_d = din("w_ukv", [depth, 128, 1024])
    conv_wT_d = din("conv_wT", [depth, 128, 4, 3])
    w_br_d = din("w_branch", [depth, 3, 512, D])
    w_out_d = din("w_out", [depth, D, D])
    rw_d = din("router_w", [depth, D, NEXP])
    rb_d = din("router_b", [depth, 1, NEXP])
    w1_d = din("exp_w1", [depth, NEXP, D, 2 * D])
    b1T_d = din("exp_b1T", [depth, NEXP, 128, 16])
    w2_d = din("exp_w2", [depth, NEXP, D, D])
    b2_d = din("exp_b2", [depth, NEXP, D])
    ident_d = din("ident", [128, 128])
    sel_d = din("sel", [NEXP, NEXP, 128])
    ropec_d = din("ropec", [32, 4])
    tri_d = din("tri", [128, 128])
    ntri_d = din("ntri", [128, 128])
    out_d = nc.dram_tensor("out", [nseq, S, D], F32, kind="ExternalOutput").ap()
    oa_s = dscr("oa_s", [8, 64, S])
    ob_s = dscr("ob_s", [4, 128, S])
    oc_s = dscr("oc_s", [8, 64, S])
    mask_s = dscr("mask_s", [NT, 128, S])
    dbg_out = {}
    if dbg:
        dbg_out["hT"] = nc.dram_tensor("dbg_hT", [128, 8, S], BF16, kind="ExternalOutput").ap()
        dbg_out["x1"] = nc.dram_tensor("dbg_x1", [128, 8, S], F32, kind="ExternalOutput").ap()
        dbg_out["x2"] = nc.dram_tensor("dbg_x2", [128, 8, S], F32, kind="ExternalOutput").ap()

    xT = sb(root, "xT", [128, 8, S], F32)
    hT = sb(root, "hT", [128, 8, S], BF16)
    ident = sb(root, "ident", [128, 128], F32)
    identb = sb(root, "identb", [128, 128], BF16)
    ones_f = sb(root, "ones_f", [128, 128], F32)
    ones_b = sb(root, "ones_b", [128, 128], BF16)
    tri_b = sb(root, "tri_b", [128, 128], BF16)
    ntri = sb(root, "ntri", [128, 128], F32)
    ropec = sb(root, "ropec", [32, 4], F32)
    cos32 = sb(root, "cos32", [32, S], BF16)
    sin32 = sb(root, "sin32", [32, S], BF16)
    cos16 = sb(root, "cos16", [32, S], BF16)
    sin16 = sb(root, "sin16", [32, S], BF16)
    modc = {}
    for l in range(depth):
        for b in range(nseq):
            for nm in ("gs1", "sh1", "g1", "gs2", "sh2", "g2"):
                modc[(l, b, nm)] = sb(root, "m_%s_%d_%d" % (nm, l, b), [128, 8], F32)
    fg = sb(root, "fg", [128, 8], F32)
    B_xT = [[Buf("xT%d_%d" % (c, t)) for t in range(NCH)] for c in range(8)]
    B_hT = [[Buf("hT%d_%d" % (c, t)) for t in range(NCH)] for c in range(8)]
    B_const = Buf("const")
    B_rope = Buf("rope")
    B_mod = Buf("mod")
    B_oa, B_ob, B_oc = Buf("oa"), Buf("ob"), Buf("oc")
    B_out = Buf("out")
    B_mask = Buf("mask")

    ps_t = [root.enter_context(nc.psum_tensor("ps%d" % i, [128, 512], F32)) for i in range(8)]
    ps_b = [Buf("ps%d" % i, excl=True) for i in range(8)]
    PSA = Ring([(ps_t[i], ps_b[i]) for i in range(0, 4)])
    PSB = Ring([(ps_t[i], ps_b[i]) for i in range(4, 6)])
    PSC = Ring([(ps_t[i], ps_b[i]) for i in range(6, 8)])
    PS8 = Ring([(ps_t[i], ps_b[i]) for i in range(8)])

    def tcs(t):
        return slice(t * 512, (t + 1) * 512)

    def mm(out, lhsT, rhs, start, stop, reads, writes):
        P.pe(lambda e: e.matmul(out, lhsT, rhs, start=start, stop=stop), reads=reads, writes=writes)

    P.dma("sp", lambda e: e.dma_start(out=ident[:], in_=ident_d[:, :]), writes=[B_const])
    P.dma("sp", lambda e: e.dma_start(out=ntri[:], in_=ntri_d[:, :]), writes=[B_const])
    P.dma("sp", lambda e: e.dma_start(out=ropec[:], in_=ropec_d[:, :]), writes=[B_const])
    P.dma("sp", lambda e: e.dma_start(out=fg[:], in_=fg_d[:, :]), writes=[B_const])
    P.dma("pool", lambda e: e.dma_start(out=tri_b[:], in_=tri_d[:, :]), writes=[B_const])
    P.dma("pool", lambda e: e.dma_start(out=identb[:], in_=ident_d[:, :]), writes=[B_const])
    P.dve(lambda e: e.memset(ones_f[:], 1.0), writes=[B_const])
    P.dve(lambda e: e.memset(ones_b[:], 1.0), writes=[B_const])

    for es in phase("prologue"):
        cact = sb(es, "cact", [128, 8, nseq], F32)
        modT = sb(es, "modT", [128, 48, nseq], F32)
        badaT = sb(es, "badaT", [128, 48], F32)
        ngt = sb(es, "ngt", [128, 8], F32)
        wa = [sb(es, "wa%d" % i, [128, 8, 512], F32) for i in range(2)]
        wab = [Buf("wa%d" % i) for i in range(2)]
        B_c, B_modT, B_bada, B_ng = Buf(), Buf(), Buf(), Buf()
        P.dma("sp", lambda e: e.dma_start(out=cact[:], in_=cT_d[:, :, :]), writes=[B_c])
        P.act(lambda e: e.activation(out=cact[:], in_=cact[:], func=AF.Silu), reads=[B_c], writes=[B_c])
        for l in range(depth):
            P.dma("sp", lambda e, l=l: e.dma_start(out=badaT[:], in_=b_adaT_d[l]), writes=[B_bada])
            for g in range(12):
                w_t, w_b = wa[g % 2], wab[g % 2]
                src = w_ada_d[l, :, g * 512:(g + 1) * 512].rearrange("(kc p) n -> p kc n", p=128)
                P.dma("sp", lambda e, w_t=w_t, src=src: e.dma_start(out=w_t[:], in_=src), writes=[w_b])
                pt, pb = PSA.get()
                for j in range(4):
                    for kc in range(8):
                        mm(pt[:, j * nseq:(j + 1) * nseq], w_t[:, kc, j * 128:(j + 1) * 128], cact[:, kc, :],
                           kc == 0, kc == 7, [w_b, B_c], [pb])
                P.dve(lambda e, pt=pt, g=g: e.tensor_copy(
                    modT[:, g * 4:(g + 1) * 4, :], pt[:, 0:4 * nseq].rearrange("p (j b) -> p j b", j=4)),
                    reads=[pb], writes=[B_modT])
            for b in range(nseq):
                P.dve(lambda e, b=b: e.tensor_tensor(modT[:, :, b], modT[:, :, b], badaT[:], ALU.add),
                      reads=[B_modT, B_bada], writes=[B_modT])
            for (nm_gs, nm_sh, nm_g, base, ng_d) in (("gs1", "sh1", "g1", 0, n1g_d), ("gs2", "sh2", "g2", 24, n2g_d)):
                P.dma("sp", lambda e, ng_d=ng_d, l=l: e.dma_start(out=ngt[:], in_=ng_d[l]), writes=[B_ng])
                for b in range(nseq):
                    gs, sh, gg = modc[(l, b, nm_gs)], modc[(l, b, nm_sh)], modc[(l, b, nm_g)]
                    P.dve(lambda e, sh=sh, b=b, base=base: e.tensor_copy(sh[:], modT[:, base:base + 8, b]),
                          reads=[B_modT], writes=[B_mod])
                    P.dve(lambda e, gs=gs, b=b, base=base: e.scalar_tensor_tensor(
                        out=gs[:], in0=modT[:, base + 8:base + 16, b], scalar=1.0, in1=ngt[:],
                        op0=ALU.add, op1=ALU.mult), reads=[B_modT, B_ng], writes=[B_mod])
                    P.dve(lambda e, gg=gg, b=b, base=base: e.tensor_copy(gg[:], modT[:, base + 16:base + 24, b]),
                          reads=[B_modT], writes=[B_mod])
        P.barrier()

    def norm_mod(es_name, gs, sh, t, dst_fn, dst_bufs_fn, tmp, extra=None):
        sq_r, rs_t, rs_b = tmp
        pt, pb = PSA.get()
        for c in range(8):
            sq, sqb = sq_r.get()
            P.act(lambda e, sq=sq, c=c: e.activation(out=sq[:], in_=xT[:, c, tcs(t)], func=AF.Square),
                  reads=[B_xT[c][t]], writes=[sqb])
            mm(pt[:, :], ones_f[:], sq[:], c == 0, c == 7, [sqb, B_const], [pb])
        P.act(lambda e, pt=pt: e.activation(out=rs_t[:], in_=pt[:, :], func=AF.Sqrt, scale=1.0 / D, bias=1e-6),
              reads=[pb], writes=[rs_b])
        P.dve(lambda e: e.reciprocal(rs_t[:], rs_t[:]), reads=[rs_b], writes=[rs_b])
        for c in range(8):
            sq, sqb = sq_r.get()
            P.dve(lambda e, sq=sq, c=c: e.tensor_tensor(sq[:], xT[:, c, tcs(t)], rs_t[:], ALU.mult),
                  reads=[B_xT[c][t], rs_b], writes=[sqb])
            P.act(lambda e, sq=sq, c=c: e.activation(out=sq[:], in_=sq[:], func=AF.Identity,
                                                     scale=gs[:, c:c + 1], bias=sh[:, c:c + 1]),
                  reads=[sqb, B_mod], writes=[sqb])
            if extra is not None:
                extra(c, sq, sqb)
            P.dve(lambda e, sq=sq, c=c: e.tensor_copy(dst_fn(c), sq[:]), reads=[sqb], writes=dst_bufs_fn(c))

    def load_w(q, dst, src, buf):
        P.dma(q, lambda e: e.dma_start(out=dst, in_=src), writes=[buf])

    def rope_tables(es, b):
        posi = sb(es, "posi", [32, S], I32)
        ang = sb(es, "ang", [32, S], F32)
        t1 = sb(es, "rt1", [32, S], F32)
        t2 = sb(es, "rt2", [32, S], F32)
        ki = sb(es, "rki", [32, S], I32)
        Bp, Ba, B1, B2, Bk = Buf(), Buf(), Buf(), Buf(), Buf()
        P.dma("sp", lambda e: e.dma_start(out=posi[:], in_=pos_d[b, :].partition_broadcast(32)), writes=[Bp])
        C1 = 6.28125
        C2 = 2.0 * math.pi - C1
        for (n, icol, scol, cos_t, sin_t) in ((32, 0, 1, cos32, sin32), (32, 2, 3, cos16, sin16)):
            P.dve(lambda e, n=n: e.tensor_copy(ang[0:n, :], posi[0:n, :]), reads=[Bp], writes=[Ba])
            P.dve(lambda e, n=n, icol=icol: e.tensor_scalar(ang[0:n, :], ang[0:n, :], ropec[0:n, icol:icol + 1], None,
                                                            ALU.mult), reads=[Ba, B_const], writes=[Ba])
            for (shift, dst, signed) in ((0.5 * math.pi, cos_t, False), (0.0, sin_t, True)):
                P.dve(lambda e, n=n, shift=shift: e.tensor_scalar(t1[0:n, :], ang[0:n, :], shift, None, ALU.add),
                      reads=[Ba], writes=[B1])
                P.dve(lambda e, n=n: e.tensor_scalar(t2[0:n, :], t1[0:n, :], 1.0 / (2.0 * math.pi), None, ALU.mult),
                      reads=[B1], writes=[B2])
                P.dve(lambda e, n=n: e.tensor_copy(ki[0:n, :], t2[0:n, :]), reads=[B2], writes=[Bk])
                P.dve(lambda e, n=n: e.tensor_copy(t2[0:n, :], ki[0:n, :]), reads=[Bk], writes=[B2])
                P.dve(lambda e, n=n: e.scalar_tensor_tensor(out=t1[0:n, :], in0=t2[0:n, :], scalar=-C1, in1=t1[0:n, :],
                                                            op0=ALU.mult, op1=ALU.add), reads=[B1, B2], writes=[B1])
                P.dve(lambda e, n=n: e.scalar_tensor_tensor(out=t1[0:n, :], in0=t2[0:n, :], scalar=-C2, in1=t1[0:n, :],
                                                            op0=ALU.mult, op1=ALU.add), reads=[B1, B2], writes=[B1])
                P.dve(lambda e, n=n: e.tensor_scalar(t2[0:n, :], t1[0:n, :], math.pi, -2.0 * math.pi, ALU.is_gt, ALU.mult),
                      reads=[B1], writes=[B2])
                P.dve(lambda e, n=n: e.tensor_tensor(t1[0:n, :], t1[0:n, :], t2[0:n, :], ALU.add),
                      reads=[B1, B2], writes=[B1])
                P.dve(lambda e, n=n: e.tensor_scalar(t1[0:n, :], t1[0:n, :], -math.pi, math.pi, ALU.max, ALU.min),
                      reads=[B1], writes=[B1])
                P.act(lambda e, n=n, dst=dst: e.activation(out=dst[0:n, :], in_=t1[0:n, :], func=AF.Sin),
                      reads=[B1], writes=[B_rope])
                if signed:
                    P.dve(lambda e, n=n, dst=dst, scol=scol: e.tensor_scalar(
                        dst[0:n, :], dst[0:n, :], ropec[0:n, scol:scol + 1], None, ALU.mult),
                        reads=[B_rope, B_const], writes=[B_rope])

    for b in range(nseq):
        with ExitStack() as es:
            xin = [sb(es, "xin%d" % i, [128, D], F32) for i in range(2)]
            xinb = [Buf() for _ in range(2)]
            for i in range(NT):
                xi, xb = xin[i % 2], xinb[i % 2]
                P.dma("sp", lambda e, xi=xi, i=i: e.dma_start(out=xi[:], in_=x_d[b, i * 128:(i + 1) * 128, :]),
                      writes=[xb])
                for g in range(2):
                    pt, pb = PSA.get()
                    for j in range(4):
                        c = g * 4 + j
                        mm(pt[:, j * 128:(j + 1) * 128], xi[:, c * 128:(c + 1) * 128], ident[:], True, True,
                           [xb, B_const], [pb])
                    P.dve(lambda e, pt=pt, g=g, i=i: e.tensor_copy(
                        xT[:, g * 4:(g + 1) * 4, i * 128:(i + 1) * 128],
                        pt[:, :].rearrange("p (c t) -> p c t", c=4)),
                        reads=[pb], writes=[B_xT[c2][i // 4] for c2 in range(g * 4, g * 4 + 4)])
            rope_tables(es, b)
            P.barrier()

        for l in range(depth if nlayers is None else nlayers):
            gs1, sh1, g1c = modc[(l, b, "gs1")], modc[(l, b, "sh1")], modc[(l, b, "g1")]
            gs2, sh2, g2c = modc[(l, b, "gs2")], modc[(l, b, "sh2")], modc[(l, b, "g2")]
            allh = lambda t: [B_hT[c][t] for c in range(8)]

            def wsrc(c0, n, l=l):
                return w_in_d[l, :, c0:c0 + n].rearrange("(kc p) n -> p kc n", p=128)

            def wrsrc(c0, n, l=l):
                return w_inr_d[l, :, c0:c0 + n].rearrange("(kc p) n -> p kc n", p=128)

            def proj(W, wb, c0, M, t, pool=PSA):
                pt, pb = pool.get()
                for kc in range(8):
                    mm(pt[0:M, :], W[:, kc, c0:c0 + M], hT[:, kc, tcs(t)], kc == 0, kc == 7,
                       [wb, B_hT[kc][t]], [pb])
                return pt, pb

            def rope_comb(dst, dstb, A, Ab, Bm, Bb, n, cos_t, sin_t, t, tmp_r):
                t1, t1b = tmp_r.get()
                t2, t2b = tmp_r.get()
                P.dve(lambda e: e.tensor_tensor(t1[0:n, :], A[0:n, :], cos_t[0:n, tcs(t)], ALU.mult),
                      reads=[Ab, B_rope], writes=[t1b])
                P.dve(lambda e: e.tensor_tensor(t2[0:n, :], Bm[0:n, :], sin_t[0:n, tcs(t)], ALU.mult),
                      reads=[Bb, B_rope], writes=[t2b])
                P.dve(lambda e: e.tensor_tensor(dst, t1[0:n, :], t2[0:n, :], ALU.add),
                      reads=[t1b, t2b], writes=[dstb])

            for es in phase("norm1"):
                sq_r = Ring([(sb(es, "sq%d" % i, [128, 512], F32), Buf()) for i in range(3)])
                rs_t, rs_b = sb(es, "rs", [128, 512], F32), Buf()
                for t in range(NCH):
                    norm_mod("n1", gs1, sh1, t, lambda c, t=t: hT[:, c, tcs(t)], lambda c, t=t: [B_hT[c][t]],
                             (sq_r, rs_t, rs_b))
                P.barrier()

            if dbg and l == 0 and b == 0:
                P.dma("sp", lambda e: e.dma_start(out=dbg_out["hT"][:, :, :], in_=hT[:]),
                      reads=[bb for r in B_hT for bb in r], writes=[B_out])
                P.barrier()
            for es in phase("mla"):
                wq, wkv, wkr = sb(es, "wq", [128, 8, 256], BF16), sb(es, "wkv", [128, 8, 160], BF16), sb(es, "wkr", [128, 8, 32], BF16)
                wuq, wuqr = sb(es, "wuq", [128, 2, 768], BF16), sb(es, "wuqr", [128, 2, 256], BF16)
                wukv = sb(es, "wukv", [128, 1024], BF16)
                qng, kvng = sb(es, "qng", [128, 2], F32), sb(es, "kvng", [128, 1], F32)
                Bw = Buf()
                load_w("pool", wq[:], wsrc(0, 256), Bw)
                load_w("pool", wkv[:], wsrc(256, 160), Bw)
                load_w("pool", wkr[:], wrsrc(0, 32), Bw)
                load_w("pool", wuq[:], w_uq_d[l].rearrange("(kc p) n -> p kc n", p=128), Bw)
                load_w("pool", wuqr[:], w_uqr_d[l].rearrange("(kc p) n -> p kc n", p=128), Bw)
                load_w("pool", wukv[:], w_ukv_d[l], Bw)
                load_w("sp", qng[:], qng_d[l], Bw)
                load_w("sp", kvng[:], kvng_d[l], Bw)
                cqT, ckvT, kpeT = sb(es, "cqT", [128, 2, S], BF16), sb(es, "ckvT", [128, S], BF16), sb(es, "kpeT", [32, S], BF16)
                Bcq, Bckv, Bkpe = Buf(), Buf(), Buf()
                raw_r = Ring([(sb(es, "raw%d" % i, [128, 512], F32), Buf()) for i in range(4)])
                tmp_r = Ring([(sb(es, "tmp%d" % i, [128, 512], F32), Buf()) for i in range(4)])
                rstd, rstdb = sb(es, "rstd", [128, 512], F32), Buf()

                def lat_norm(raws, gcol, dst_fn, dstb, nfeat):
                    pt, pb = PSA.get()
                    for i, (rw_t, rw_b) in enumerate(raws):
                        sq, sqb = tmp_r.get()
                        P.act(lambda e, sq=sq, rw_t=rw_t: e.activation(out=sq[:], in_=rw_t[:], func=AF.Square),
                              reads=[rw_b], writes=[sqb])
                        mm(pt[:, :], ones_f[:], sq[:], i == 0, i == len(raws) - 1, [sqb, B_const], [pb])
                    P.act(lambda e, pt=pt: e.activation(out=rstd[:], in_=pt[:, :], func=AF.Sqrt, scale=1.0 / nfeat, bias=1e-6),
                          reads=[pb], writes=[rstdb])
                    P.dve(lambda e: e.reciprocal(rstd[:], rstd[:]), reads=[rstdb], writes=[rstdb])
                    for i, (rw_t, rw_b) in enumerate(raws):
                        P.dve(lambda e, i=i, rw_t=rw_t: e.scalar_tensor_tensor(
                            out=dst_fn(i), in0=rw_t[:], scalar=gcol[:, i:i + 1], in1=rstd[:], op0=ALU.mult, op1=ALU.mult),
                            reads=[rw_b, rstdb, Bw], writes=[dstb])

                for t in range(NCH):
                    raws = []
                    for j in range(2):
                        pt, pb = proj(wq, Bw, j * 128, 128, t)
                        rw_t, rw_b = raw_r.get()
                        P.act(lambda e, rw_t=rw_t, pt=pt: e.activation(out=rw_t[:], in_=pt[:, :], func=AF.Identity),
                              reads=[pb], writes=[rw_b])
                        raws.append((rw_t, rw_b))
                    lat_norm(raws, qng, lambda i, t=t: cqT[:, i, tcs(t)], Bcq, 256.0)
                    pt, pb = proj(wkv, Bw, 0, 128, t)
                    rw_t, rw_b = raw_r.get()
                    P.act(lambda e, rw_t=rw_t, pt=pt: e.activation(out=rw_t[:], in_=pt[:, :], func=AF.Identity),
                          reads=[pb], writes=[rw_b])
                    lat_norm([(rw_t, rw_b)], kvng, lambda i, t=t: ckvT[:, tcs(t)], Bckv, 128.0)
                    A, Ab = proj(wkv, Bw, 128, 32, t)
                    Bm, Bb = proj(wkr, Bw, 0, 32, t)
                    rope_comb(kpeT[0:32, tcs(t)], Bkpe, A, Ab, Bm, Bb, 32, cos32, sin32, t, tmp_r)

                qr = [sb(es, "qr%d" % i, [32, S], BF16) for i in range(2)]
                qn = [sb(es, "qn%d" % i, [64, S], BF16) for i in range(2)]
                kn = [sb(es, "kn%d" % i, [64, S], BF16) for i in range(2)]
                Vh = [sb(es, "Vh%d" % i, [128, NT, 64], BF16) for i in range(2)]
                hb = [[Buf() for _ in range(4)] for _ in range(2)]
                PT_r = Ring([(sb(es, "PT%d" % i, [128, 512], BF16), Buf()) for i in range(3)])
                rD, rDb = sb(es, "rD", [64, 512], F32), Buf()
                o_r = Ring([(sb(es, "o%d" % i, [64, 512], BF16), Buf()) for i in range(2)])
                sc_a = 96.0 ** -0.5
                for h in range(8):
                    s2 = h % 2
                    Bqr, Bqn, Bkn, BV = hb[s2]
                    for t in range(NCH):
                        def cproj(W, c0, M, t=t):
                            pt, pb = PSA.get()
                            for kc in range(2):
                                mm(pt[0:M, :], W[:, kc, c0:c0 + M], cqT[:, kc, tcs(t)], kc == 0, kc == 1, [Bw, Bcq], [pb])
                            return pt, pb
                        A, Ab = cproj(wuq, h * 96, 32)
                        Bm, Bb = cproj(wuqr, h * 32, 32)
                        rope_comb(qr[s2][0:32, tcs(t)], Bqr, A, Ab, Bm, Bb, 32, cos32, sin32, t, tmp_r)
                        pt, pb = cproj(wuq, h * 96 + 32, 64)
                        P.act(lambda e, pt=pt, t=t, s2=s2: e.activation(out=qn[s2][0:64, tcs(t)], in_=pt[0:64, :], func=AF.Identity),
                              reads=[pb], writes=[Bqn])
                        pt, pb = PSA.get()
                        mm(pt[0:64, :], wukv[:, h * 128:h * 128 + 64], ckvT[:, tcs(t)], True, True, [Bw, Bckv], [pb])
                        P.act(lambda e, pt=pt, t=t, s2=s2: e.activation(out=kn[s2][0:64, tcs(t)], in_=pt[0:64, :], func=AF.Identity),
                              reads=[pb], writes=[Bkn])
                    for g in range(2):
                        pt, pb = PSA.get()
                        for j in range(8):
                            i = g * 8 + j
                            mm(pt[:, j * 64:(j + 1) * 64], ckvT[:, i * 128:(i + 1) * 128], wukv[:, h * 128 + 64:h * 128 + 128],
                               True, True, [Bw, Bckv], [pb])
                        P.dve(lambda e, pt=pt, g=g, s2=s2: e.tensor_copy(Vh[s2][:, g * 8:(g + 1) * 8, :],
                                                                        pt[:, :].rearrange("p (j d) -> p j d", j=8)),
                              reads=[pb], writes=[BV])
                    for c in range(NCH):
                        O, Ob = PSB.get()
                        Dn, Db = PSC.get()
                        nk = 4 * c + 4
                        for kt in range(nk):
                            j = kt - 4 * c
                            q0 = max(j, 0) * 128
                            qs = slice(c * 512 + q0, (c + 1) * 512)
                            ks = slice(kt * 128, (kt + 1) * 128)
                            pt, pb = PSA.get()
                            mm(pt[:, q0:512], kpeT[0:32, ks], qr[s2][0:32, qs], True, False, [Bkpe, Bqr], [pb])
                            mm(pt[:, q0:512], kn[s2][0:64, ks], qn[s2][0:64, qs], False, True, [Bkn, Bqn], [pb])
                            PT, PTb = PT_r.get()
                            P.act(lambda e, PT=PT, pt=pt, q0=q0: e.activation(out=PT[:, q0:512], in_=pt[:, q0:512], func=AF.Exp, scale=sc_a),
                                  reads=[pb], writes=[PTb])
                            if j >= 0:
                                P.dve(lambda e, PT=PT, q0=q0: e.tensor_tensor(PT[:, q0:q0 + 128], PT[:, q0:q0 + 128], tri_b[:], ALU.mult),
                                      reads=[PTb, B_const], writes=[PTb])
                            mm(O[0:64, q0:512], Vh[s2][:, kt, :], PT[:, q0:512], kt == 0, kt == nk - 1, [BV, PTb], [Ob])
                            mm(Dn[0:64, q0:512], ones_b[:, 0:64], PT[:, q0:512], kt == 0, kt == nk - 1, [B_const, PTb], [Db])
                        P.dve(lambda e, Dn=Dn: e.reciprocal(rD[:], Dn[0:64, :]), reads=[Db], writes=[rDb])
                        o_t, o_b = o_r.get()
                        P.dve(lambda e, O=O, o_t=o_t: e.tensor_tensor(o_t[:], O[0:64, :], rD[:], ALU.mult),
                              reads=[Ob, rDb], writes=[o_b])
                        P.dma("sp", lambda e, o_t=o_t, h=h, c=c: e.dma_start(out=oa_s[h, :, tcs(c)], in_=o_t[:]),
                              reads=[o_b], writes=[B_oa])
                P.barrier()

            for es in phase("conv"):
                cw = sb(es, "cw", [128, 4, 3], F32)
                Bcw = Buf()
                load_w("sp", cw[:], conv_wT_d[l], Bcw)
                wc_r = Ring([(sb(es, "wc%d" % i, [128, 8, 3, 128], BF16), Buf()) for i in range(2)])
                u, Bu = sb(es, "u", [128, S + 2], F32), Buf()
                y, By = sb(es, "y", [128, S], F32), Buf()
                gb, Bgb = sb(es, "gb", [128, S], BF16), Buf()
                ob_r = Ring([(sb(es, "obt%d" % i, [128, S], BF16), Buf()) for i in range(2)])
                tmp_r = Ring([(sb(es, "ctmp%d" % i, [128, 512], F32), Buf()) for i in range(2)])
                P.dve(lambda e: e.memset(u[:, 0:2], 0.0), writes=[Bu])
                for j in range(4):
                    wc, wcb = wc_r.get()
                    for k in range(3):
                        load_w("pool", wc[:, :, k, :], wsrc(416 + k * 512 + j * 128, 128), wcb)
                    for t in range(NCH):
                        def cp(k, t=t, wc=wc, wcb=wcb):
                            pt, pb = PSA.get()
                            for kc in range(8):
                                mm(pt[:, :], wc[:, kc, k, :], hT[:, kc, tcs(t)], kc == 0, kc == 7, [wcb, B_hT[kc][t]], [pb])
                            return pt, pb
                        pgc, pgcb = cp(1)
                        phc, phcb = cp(2)
                        pgb, pgbb = cp(0)
                        tm, tmb = tmp_r.get()
                        P.act(lambda e, tm=tm, pgc=pgc: e.activation(out=tm[:], in_=pgc[:, :], func=AF.Identity), reads=[pgcb], writes=[tmb])
                        P.dve(lambda e, tm=tm, phc=phc, t=t: e.tensor_tensor(u[:, 2 + t * 512:2 + (t + 1) * 512], tm[:], phc[:, :], ALU.mult),
                              reads=[tmb, phcb], writes=[Bu])
                        P.act(lambda e, pgb=pgb, t=t: e.activation(out=gb[:, tcs(t)], in_=pgb[:, :], func=AF.Identity), reads=[pgbb], writes=[Bgb])
                    P.dve(lambda e, j=j: e.tensor_scalar(y[:], u[:, 2:S + 2], cw[:, j, 2:3], None, ALU.mult), reads=[Bu, Bcw], writes=[By])
                    P.dve(lambda e, j=j: e.scalar_tensor_tensor(out=y[:], in0=u[:, 1:S + 1], scalar=cw[:, j, 1:2], in1=y[:], op0=ALU.mult, op1=ALU.add),
                          reads=[Bu, Bcw, By], writes=[By])
                    P.dve(lambda e, j=j: e.scalar_tensor_tensor(out=y[:], in0=u[:, 0:S], scalar=cw[:, j, 0:1], in1=y[:], op0=ALU.mult, op1=ALU.add),
                          reads=[Bu, Bcw, By], writes=[By])
                    ot, otb = ob_r.get()
                    P.dve(lambda e, ot=ot: e.tensor_tensor(ot[:], y[:], gb[:], ALU.mult), reads=[By, Bgb], writes=[otb])
                    P.dma("sp", lambda e, ot=ot, j=j: e.dma_start(out=ob_s[j], in_=ot[:]), reads=[otb], writes=[B_ob])
                P.barrier()

            for es in phase("d1"):
                qiT, kiT = sb(es, "qiT", [64, 8, S], BF16), sb(es, "kiT", [64, S], BF16)
                iw = sb(es, "iw", [128, NT, 8], F32)
                Bqi, Bki, Biw = Buf(), Buf(), Buf()
                with ExitStack() as es2:
                    wiq, wiqr = sb(es2, "wiq", [128, 8, 512], BF16), sb(es2, "wiqr", [128, 8, 144], BF16)
                    wik, wikr = sb(es2, "wik", [128, 8, 128], BF16), sb(es2, "wikr", [128, 8, 128], BF16)
                    tmp_r = Ring([(sb(es2, "dtmp%d" % i, [128, 512], F32), Buf()) for i in range(4)])
                    Bw = Buf()
                    load_w("pool", wiq[:], wsrc(2592, 512), Bw)
                    load_w("pool", wiqr[:], wrsrc(176, 144), Bw)
                    load_w("pool", wik[:], wsrc(3104, 128), Bw)
                    load_w("pool", wikr[:], wrsrc(208, 128), Bw)
                    for t in range(NCH):
                        for h in range(8 if "d1p_q" in run_phases else 0):
                            A, Ab = proj(wiq, Bw, h * 64, 64, t)
                            Bm, Bb = proj(wiqr, Bw, h * 16, 32, t)
                            P.act(lambda e, A=A, h=h, t=t: e.activation(out=qiT[0:64, h, tcs(t)], in_=A[0:64, :], func=AF.Identity),
                                  reads=[Ab], writes=[Ab, Bqi])
                            rope_comb(qiT[0:32, h, tcs(t)], Bqi, A, Ab, Bm, Bb, 32, cos16, sin16, t, tmp_r)
                        if "d1p_k" in run_phases:
                            A, Ab = proj(wik, Bw, 0, 64, t)
                            Bm, Bb = proj(wikr, Bw, 96, 32, t)
                            P.act(lambda e, A=A, t=t: e.activation(out=kiT[0:64, tcs(t)], in_=A[0:64, :], func=AF.Identity), reads=[Ab], writes=[Ab, Bki])
                            rope_comb(kiT[0:32, tcs(t)], Bki, A, Ab, Bm, Bb, 32, cos16, sin16, t, tmp_r)
                        for jj in range(4 if "d1p_w" in run_phases else 0):
                            i = t * 4 + jj
                            pt, pb = PSA.get()
                            for kc in range(8):
                                mm(pt[:, 0:64], hT[:, kc, i * 128:(i + 1) * 128], wik[:, kc, 64:128], kc == 0, kc == 7, [Bw, B_hT[kc][t]], [pb])
                            P.dve(lambda e, pt=pt, i=i: e.tensor_copy(iw[:, i, :], pt[:, 0:8]), reads=[pb], writes=[Biw])
                    P.barrier()
                sc, Bsc = sb(es, "sc", [128, S], F32), Buf()
                rl_r = Ring([(sb(es, "rl%d" % i, [128, 512], BF16), Buf()) for i in range(3)])
                dg, Bdg = sb(es, "dg", [128, 8, 128], BF16), Buf()
                mk, Bmk = sb(es, "mk", [128, S], BF16), Buf()
                mT_r = Ring([(sb(es, "mT%d" % i, [128, 4, 128], BF16), Buf()) for i in range(2)])
                st = sb(es, "st", [128, 64], F32)
                Bst = Buf()
                NIT = 18
                ACC = [PSB.get(), PSB.get(), PSC.get(), PSC.get()]
                for qt in range(NT if "d1_score" in run_phases else 0):
                    nk = (qt + 1) * 128
                    nkc = (nk + 511) // 512
                    for h in range(8):
                        P.dve(lambda e, h=h, qt=qt: e.tensor_scalar(dg[:, h, :], identb[:], iw[:, qt, h:h + 1], None, ALU.mult),
                              reads=[B_const, Biw], writes=[Bdg])
                    for h in range(8):
                        for kk in range(nkc):
                            w = min(512, nk - kk * 512)
                            pt, pb = PSA.get()
                            mm(pt[:, 0:w], qiT[0:64, h, qt * 128:(qt + 1) * 128], kiT[0:64, kk * 512:kk * 512 + w], True, True, [Bqi, Bki], [pb])
                            rl, rlb = rl_r.get()
                            P.act(lambda e, rl=rl, pt=pt, w=w: e.activation(out=rl[:, 0:w], in_=pt[:, 0:w], func=AF.Relu), reads=[pb], writes=[rlb])
                            at, ab = ACC[kk]
                            mm(at[:, 0:w], dg[:, h, :], rl[:, 0:w], h == 0, h == 7, [Bdg, rlb], [ab])
                    for kk in range(nkc):
                        w = min(512, nk - kk * 512)
                        at, ab = ACC[kk]
                        P.act(lambda e, at=at, kk=kk, w=w: e.activation(out=sc[:, kk * 512:kk * 512 + w], in_=at[:, 0:w], func=AF.Identity),
                              reads=[ab], writes=[Bsc])
                    if "d1_bis" not in run_phases:
                        continue
                    if nk <= 256:
                        P.dve(lambda e: e.memset(st[:, 0:1], -1.0e29), writes=[Bst])
                        P.dve(lambda e, qt=qt: e.tensor_tensor(sc[:, qt * 128:(qt + 1) * 128], sc[:, qt * 128:(qt + 1) * 128], ntri[:], ALU.add),
                              reads=[Bsc, B_const], writes=[Bsc])
                    else:
                        P.dve(lambda e, nk=nk: e.tensor_reduce(out=st[:, 1:2], in_=sc[:, 0:nk], axis=AX.X, op=ALU.min), reads=[Bsc], writes=[Bst])
                        P.dve(lambda e, nk=nk: e.tensor_reduce(out=st[:, 2:3], in_=sc[:, 0:nk], axis=AX.X, op=ALU.max), reads=[Bsc], writes=[Bst])
                        P.dve(lambda e, qt=qt: e.tensor_tensor(sc[:, qt * 128:(qt + 1) * 128], sc[:, qt * 128:(qt + 1) * 128], ntri[:], ALU.add),
                              reads=[Bsc, B_const], writes=[Bsc])
                        P.dve(lambda e: e.tensor_tensor(st[:, 3:4], st[:, 2:3], st[:, 1:2], ALU.subtract), reads=[Bst], writes=[Bst])
                        P.dve(lambda e: e.tensor_scalar(st[:, 3:4], st[:, 3:4], 1.0001, 1e-6, ALU.mult, ALU.add), reads=[Bst], writes=[Bst])
                        P.dve(lambda e: e.tensor_copy(st[:, 0:1], st[:, 1:2]), reads=[Bst], writes=[Bst])
                        for i in range(NIT):
                            P.dve(lambda e, i=i: e.tensor_scalar(st[:, 8 + i:9 + i], st[:, 3:4], 2.0 ** -(i + 1), None, ALU.mult), reads=[Bst], writes=[Bst])
                        for i in range(NIT):
                            P.dve(lambda e, i=i: e.tensor_tensor(st[:, 4:5], st[:, 0:1], st[:, 8 + i:9 + i], ALU.add), reads=[Bst], writes=[Bst])
                            P.dve(lambda e, nk=nk: e.tensor_scalar(mk[:, 0:nk], sc[:, 0:nk], st[:, 4:5], None, ALU.is_ge, ALU.add, accum_out=st[:, 5:6]),
                                  reads=[Bsc, Bst], writes=[Bmk, Bst])
                            P.dve(lambda e, i=i: e.tensor_scalar(st[:, 6:7], st[:, 5:6], 255.5, st[:, 8 + i:9 + i], ALU.is_ge, ALU.mult), reads=[Bst], writes=[Bst])
                            P.dve(lambda e: e.tensor_tensor(st[:, 0:1], st[:, 0:1], st[:, 6:7], ALU.add), reads=[Bst], writes=[Bst])
                    if "d1_mask" not in run_phases:
                        continue
                    P.dve(lambda e, nk=nk: e.tensor_scalar(mk[:, 0:nk], sc[:, 0:nk], st[:, 0:1], None, ALU.is_ge), reads=[Bsc, Bst], writes=[Bmk])
                    for g in range((qt + 4) // 4):
                        k0 = g * 4
                        nb = min(4, qt + 1 - k0)
                        pt, pb = PSA.get()
                        for jj in range(nb):
                            kt = k0 + jj
                            mm(pt[:, jj * 128:(jj + 1) * 128], mk[:, kt * 128:(kt + 1) * 128], identb[:], True, True, [Bmk, B_const], [pb])
                        mT, mTb = mT_r.get()
                        P.act(lambda e, mT=mT, pt=pt, nb=nb: e.activation(out=mT[:, 0:nb, :], in_=pt[:, 0:nb * 128].rearrange("p (k q) -> p k q", k=nb), func=AF.Identity),
                              reads=[pb], writes=[mTb])
                        P.dma("sp", lambda e, mT=mT, k0=k0, nb=nb, qt=qt: e.dma_start(
                            out=mask_s[k0:k0 + nb, :, qt * 128:(qt + 1) * 128].rearrange("k p q -> p k q"), in_=mT[:, 0:nb, :]),
                            reads=[mTb], writes=[B_mask])
                P.barrier()

            for es in phase("d2"):
                qcT, kcT = sb(es, "qcT", [64, 8, S], BF16), sb(es, "kcT", [64, S], BF16)
                Vc = sb(es, "Vc", [128, NT, 64], BF16)
                Bqc, Bkc, BVc = Buf(), Buf(), Buf()
                with ExitStack() as es2:
                    wdq, wdqr = sb(es2, "wdq", [128, 8, 512], BF16), sb(es2, "wdqr", [128, 8, 144], BF16)
                    wkc, wkcr = sb(es2, "wkc", [128, 8, 128], BF16), sb(es2, "wkcr", [128, 8, 128], BF16)
                    tmp_r = Ring([(sb(es2, "etmp%d" % i, [128, 512], F32), Buf()) for i in range(4)])
                    Bw = Buf()
                    load_w("pool", wdq[:], wsrc(1952, 512), Bw)
                    load_w("pool", wdqr[:], wrsrc(32, 144), Bw)
                    load_w("pool", wkc[:], wsrc(2464, 128), Bw)
                    load_w("pool", wkcr[:], wrsrc(128, 128), Bw)
                    for t in range(NCH):
                        for h in range(8):
                            A, Ab = proj(wdq, Bw, h * 64, 64, t)
                            Bm, Bb = proj(wdqr, Bw, h * 16, 32, t)
                            P.act(lambda e, A=A, h=h, t=t: e.activation(out=qcT[0:64, h, tcs(t)], in_=A[0:64, :], func=AF.Identity), reads=[Ab], writes=[Ab, Bqc])
                            rope_comb(qcT[0:32, h, tcs(t)], Bqc, A, Ab, Bm, Bb, 32, cos16, sin16, t, tmp_r)
                        A, Ab = proj(wkc, Bw, 0, 64, t)
                        Bm, Bb = proj(wkcr, Bw, 32, 32, t)
                        P.act(lambda e, A=A, t=t: e.activation(out=kcT[0:64, tcs(t)], in_=A[0:64, :], func=AF.Identity), reads=[Ab], writes=[Ab, Bkc])
                        rope_comb(kcT[0:32, tcs(t)], Bkc, A, Ab, Bm, Bb, 32, cos16, sin16, t, tmp_r)
                        pt, pb = PSA.get()
                        for jj in range(4):
                            i = t * 4 + jj
                            for kc in range(8):
                                mm(pt[:, jj * 64:(jj + 1) * 64], hT[:, kc, i * 128:(i + 1) * 128], wkc[:, kc, 64:128], kc == 0, kc == 7, [Bw, B_hT[kc][t]], [pb])
                        P.dve(lambda e, pt=pt, t=t: e.tensor_copy(Vc[:, t * 4:(t + 1) * 4, :], pt[:, 0:256].rearrange("p (j d) -> p j d", j=4)),
                              reads=[pb], writes=[BVc])
                    P.barrier()
                mTc, BmTc = sb(es, "mTc", [128, NT, 512], BF16), Buf()
                PT_r = Ring([(sb(es, "PTc%d" % i, [128, 512], BF16), Buf()) for i in range(3)])
                rD, rDb = sb(es, "rDc", [64, 512], F32), Buf()
                o_r = Ring([(sb(es, "oc%d" % i, [64, 512], BF16), Buf()) for i in range(2)])
                sc_c = 64.0 ** -0.5
                for c in range(NCH):
                    nk = 4 * c + 4
                    for kt in range(nk):
                        q0 = max(kt - 4 * c, 0) * 128
                        P.dma("sp", lambda e, kt=kt, q0=q0, c=c: e.dma_start(out=mTc[:, kt, q0:512], in_=mask_s[kt, :, c * 512 + q0:(c + 1) * 512]),
                              reads=[B_mask], writes=[BmTc])
                    for h in range(8):
                        O, Ob = PSB.get()
                        Dn, Db = PSC.get()
                        for kt in range(nk):
                            q0 = max(kt - 4 * c, 0) * 128
                            qs = slice(c * 512 + q0, (c + 1) * 512)
                            ks = slice(kt * 128, (kt + 1) * 128)
                            pt, pb = PSA.get()
                            mm(pt[:, q0:512], kcT[0:64, ks], qcT[0:64, h, qs], True, True, [Bkc, Bqc], [pb])
                            PT, PTb = PT_r.get()
                            P.act(lambda e, PT=PT, pt=pt, q0=q0: e.activation(out=PT[:, q0:512], in_=pt[:, q0:512], func=AF.Exp, scale=sc_c), reads=[pb], writes=[PTb])
                            P.dve(lambda e, PT=PT, q0=q0, kt=kt: e.tensor_tensor(PT[:, q0:512], PT[:, q0:512], mTc[:, kt, q0:512], ALU.mult),
                                  reads=[PTb, BmTc], writes=[PTb])
                            mm(O[0:64, q0:512], Vc[:, kt, :], PT[:, q0:512], kt == 0, kt == nk - 1, [BVc, PTb], [Ob])
                            mm(Dn[0:64, q0:512], ones_b[:, 0:64], PT[:, q0:512], kt == 0, kt == nk - 1, [B_const, PTb], [Db])
                        P.dve(lambda e, Dn=Dn: e.reciprocal(rD[:], Dn[0:64, :]), reads=[Db], writes=[rDb])
                        o_t, o_b = o_r.get()
                        P.dve(lambda e, O=O, o_t=o_t: e.tensor_tensor(o_t[:], O[0:64, :], rD[:], ALU.mult), reads=[Ob, rDb], writes=[o_b])
                        P.dma("sp", lambda e, o_t=o_t, h=h, c=c: e.dma_start(out=oc_s[h, :, tcs(c)], in_=o_t[:]), reads=[o_b], writes=[B_oc])
                P.barrier()

            for es in phase("merge"):
                wo, Bwo = sb(es, "wo", [128, 8, D], BF16), Buf()
                load_w("pool", wo[:], w_out_d[l].rearrange("(kc p) n -> p kc n", p=128), Bwo)
                bg, Bbg = sb(es, "bg", [128, 24], F32), Buf()
                load_w("sp", bg[:], b_gateT_d[l], Bbg)
                oa_t, ob_t, oc_t = sb(es, "oa_t", [64, 8, 512], BF16), sb(es, "ob_t", [128, 4, 512], BF16), sb(es, "oc_t", [64, 8, 512], BF16)
                Bo = Buf()
                wg_r = Ring([(sb(es, "wg%d" % i, [128, 8, 3, 128], BF16), sb(es, "wba%d" % i, [64, 8, 128], BF16),
                              sb(es, "wbb%d" % i, [128, 4, 128], BF16), sb(es, "wbc%d" % i, [64, 8, 128], BF16), Buf()) for i in range(2)])
                g_r = Ring([(sb(es, "g%d" % i, [128, 512], BF16), Buf()) for i in range(2)])
                a3 = [(sb(es, "a3%d" % i, [128, 512], F32), Buf()) for i in range(3)]
                mg, Bmg = sb(es, "mg", [128, 8, 512], BF16), Buf()
                for t in range(NCH):
                    P.dma("sp", lambda e, t=t: e.dma_start(out=oa_t[:], in_=oa_s[:, :, tcs(t)].rearrange("h p t -> p h t")), reads=[B_oa], writes=[Bo])
                    P.dma("sp", lambda e, t=t: e.dma_start(out=ob_t[:], in_=ob_s[:, :, tcs(t)].rearrange("h p t -> p h t")), reads=[B_ob], writes=[Bo])
                    P.dma("sp", lambda e, t=t: e.dma_start(out=oc_t[:], in_=oc_s[:, :, tcs(t)].rearrange("h p t -> p h t")), reads=[B_oc], writes=[Bo])
                    for dc in range(8):
                        wg, wba, wbb, wbc, Bwg = wg_r.get()
                        for n3 in range(3):
                            load_w("pool", wg[:, :, n3, :], wsrc(3176 + n3 * 1024 + dc * 128, 128), Bwg)
                        load_w("pool", wba[:], w_br_d[l, 0, :, dc * 128:(dc + 1) * 128].rearrange("(h p) n -> p h n", p=64), Bwg)
                        load_w("pool", wbb[:], w_br_d[l, 1, :, dc * 128:(dc + 1) * 128].rearrange("(h p) n -> p h n", p=128), Bwg)
                        load_w("pool", wbc[:], w_br_d[l, 2, :, dc * 128:(dc + 1) * 128].rearrange("(h p) n -> p h n", p=64), Bwg)
                        for n3 in range(3):
                            py, pyb = PSA.get()
                            if n3 == 1:
                                for k in range(4):
                                    mm(py[:, :], wbb[:, k, :], ob_t[:, k, :], k == 0, k == 3, [Bwg, Bo], [pyb])
                            else:
                                wb_, o_ = (wba, oa_t) if n3 == 0 else (wbc, oc_t)
                                for k in range(8):
                                    mm(py[:, :], wb_[0:64, k, :], o_[0:64, k, :], k == 0, k == 7, [Bwg, Bo], [pyb])
                            pg, pgb = PSA.get()
                            for kc in range(8):
                                mm(pg[:, :], wg[:, kc, n3, :], hT[:, kc, tcs(t)], kc == 0, kc == 7, [Bwg, B_hT[kc][t]], [pgb])
                            g_t, g_b = g_r.get()
                            P.act(lambda e, g_t=g_t, pg=pg, n3=n3, dc=dc: e.activation(out=g_t[:], in_=pg[:, :], func=AF.Sigmoid,
                                                                                     bias=bg[:, n3 * 8 + dc:n3 * 8 + dc + 1]),
                                  reads=[pgb, Bbg], writes=[g_b])
                            P.dve(lambda e, n3=n3, py=py, g_t=g_t: e.tensor_tensor(a3[n3][0][:], py[:, :], g_t[:], ALU.mult),
                                  reads=[pyb, g_b], writes=[a3[n3][1]])
                        P.dve(lambda e: e.tensor_tensor(a3[0][0][:], a3[0][0][:], a3[1][0][:], ALU.add), reads=[a3[0][1], a3[1][1]], writes=[a3[0][1]])
                        P.dve(lambda e, dc=dc: e.tensor_tensor(mg[:, dc, :], a3[0][0][:], a3[2][0][:], ALU.add), reads=[a3[0][1], a3[2][1]], writes=[Bmg])
                    for dc2 in range(8):
                        po, pob = PSA.get()
                        for dc in range(8):
                            mm(po[:, :], wo[:, dc, dc2 * 128:(dc2 + 1) * 128], mg[:, dc, :], dc == 0, dc == 7, [Bwo, Bmg], [pob])
                        P.dve(lambda e, po=po, dc2=dc2, t=t: e.scalar_tensor_tensor(out=xT[:, dc2, tcs(t)], in0=po[:, :], scalar=g1c[:, dc2:dc2 + 1],
                                                                                  in1=xT[:, dc2, tcs(t)], op0=ALU.mult, op1=ALU.add),
                              reads=[pob, B_mod, B_xT[dc2][t]], writes=[B_xT[dc2][t]])
                P.barrier()

            if dbg and l == 0 and b == 0:
                P.dma("sp", lambda e: e.dma_start(out=dbg_out["x1"][:, :, :], in_=xT[:]),
                      reads=[bb for r in B_xT for bb in r], writes=[B_out])
                P.barrier()
            for es in phase("moe"):
                rw, Brw = sb(es, "rw", [128, 8, NEXP], F32), Buf()
                rb, b2t = sb(es, "rb", [1, NEXP], F32), sb(es, "b2t", [NEXP, D], F32)
                load_w("sp", rw[:], rw_d[l].rearrange("(kc p) n -> p kc n", p=128), Brw)
                load_w("sp", rb[:], rb_d[l], Brw)
                load_w("sp", b2t[:], b2_d[l], Brw)
                w1, b1 = sb(es, "w1", [128, 8, 2 * D], BF16), sb(es, "b1", [128, 16], F32)
                Bw1 = [Buf(), Buf()]
                Bb1 = Buf()
                w2, Bw2 = sb(es, "w2", [128, 8, D], BF16), Buf()
                gl, Bgl = sb(es, "gl", [128, 8, 512], BF16), [Buf() for _ in range(8)]
                gwT, BgwT = sb(es, "gwT", [NEXP, S], F32), [Buf() for _ in range(NCH)]
                gwb, Bgwb = sb(es, "gwb", [128, 512], F32), Buf()
                with ExitStack() as es2:
                    sq_r = Ring([(sb(es2, "msq%d" % i, [128, 512], F32), Buf()) for i in range(2)])
                    rs_t, rs_b = sb(es2, "mrs", [128, 512], F32), Buf()
                    lg, Blg = sb(es2, "lg", [128, NEXP], F32), Buf()
                    ex = sb(es2, "ex", [128, NEXP], F32)
                    mk4 = sb(es2, "mk4", [128, NEXP], F32)
                    t8 = sb(es2, "t8", [128, 16], F32)
                    RL = [PSB.get(), PSB.get(), PSC.get(), PSC.get()]
                    for t in range(NCH):
                        def router_extra(c, sq, sqb, t=t):
                            for jj in range(4):
                                rt, rtb = RL[jj]
                                mm(rt[:, 0:NEXP], sq[:, jj * 128:(jj + 1) * 128], rw[:, c, :], c == 0, False, [sqb, Brw], [rtb])
                        norm_mod("n2", gs2, sh2, t, lambda c, t=t: hT[:, c, tcs(t)], lambda c, t=t: [B_hT[c][t]],
                                 (sq_r, rs_t, rs_b), extra=router_extra)
                        for jj in range(4):
                            rt, rtb = RL[jj]
                            mm(rt[:, 0:NEXP], ones_f[0:1, :], rb[0:1, :], False, True, [B_const, Brw], [rtb])
                            P.dve(lambda e, rt=rt: e.tensor_copy(lg[:], rt[:, 0:NEXP]), reads=[rtb], writes=[Blg])
                            P.dve(lambda e: e.max(out=t8[:, 0:8], in_=lg[:]), reads=[Blg], writes=[Blg])
                            P.dve(lambda e: e.tensor_scalar(t8[:, 8:9], t8[:, 0:1], -1.0, None, ALU.mult), reads=[Blg], writes=[Blg])
                            P.act(lambda e: e.activation(out=ex[:], in_=lg[:], func=AF.Exp, bias=t8[:, 8:9]), reads=[Blg], writes=[Blg])
                            P.dve(lambda e: e.tensor_scalar(mk4[:], lg[:], t8[:, 3:4], None, ALU.is_ge), reads=[Blg], writes=[Blg])
                            P.dve(lambda e: e.tensor_tensor(ex[:], ex[:], mk4[:], ALU.mult), reads=[Blg], writes=[Blg])
                            P.dve(lambda e: e.tensor_reduce(out=t8[:, 9:10], in_=ex[:], axis=AX.X, op=ALU.add), reads=[Blg], writes=[Blg])
                            P.dve(lambda e: e.reciprocal(t8[:, 10:11], t8[:, 9:10]), reads=[Blg], writes=[Blg])
                            P.dve(lambda e: e.tensor_scalar(ex[:], ex[:], t8[:, 10:11], None, ALU.mult), reads=[Blg], writes=[Blg])
                            pt, pb = PSA.get()
                            mm(pt[0:NEXP, 0:128], ex[:], ident[:], True, True, [Blg, B_const], [pb])
                            i = t * 4 + jj
                            P.dve(lambda e, pt=pt, i=i: e.tensor_copy(gwT[:, i * 128:(i + 1) * 128], pt[0:NEXP, 0:128]), reads=[pb], writes=[BgwT[t]])
                    P.barrier()
                et_r = Ring([(sb(es, "et%d" % i, [128, 512], F32), Buf()) for i in range(6)])
                for ei in range(NEXP):
                    for hf in range(2):
                        load_w("pool", w1[:, :, hf * D:(hf + 1) * D], w1_d[l, ei, :, hf * D:(hf + 1) * D].rearrange("(kc p) n -> p kc n", p=128), Bw1[hf])
                    load_w("sp", b1[:], b1T_d[l, ei], Bb1)
                    load_w("pool", w2[:], w2_d[l, ei].rearrange("(kc p) n -> p kc n", p=128), Bw2)
                    for t in range(NCH):
                        pt, pb = PS8.get()
                        mm(pt[:, :], ident[0:NEXP, ei:ei + 1].to_broadcast([NEXP, 128]), gwT[:, tcs(t)], True, True, [B_const, BgwT[t]], [pb])
                        P.act(lambda e, pt=pt: e.activation(out=gwb[:], in_=pt[:, :], func=AF.Identity), reads=[pb], writes=[Bgwb])
                        for fc in range(8):
                            bw = Bw1[fc // 4]
                            pg, pgb = PS8.get()
                            for kc in range(8):
                                mm(pg[:, :], w1[:, kc, fc * 256:(fc + 1) * 256:2], hT[:, kc, tcs(t)], kc == 0, kc == 7, [bw, B_hT[kc][t]], [pgb])
                            pu, pub = PS8.get()
                            for kc in range(8):
                                mm(pu[:, :], w1[:, kc, fc * 256 + 1:(fc + 1) * 256:2], hT[:, kc, tcs(t)], kc == 0, kc == 7, [bw, B_hT[kc][t]], [pub])
                            gt, gtb = et_r.get()
                            sg, sgb = et_r.get()
                            ut, utb = et_r.get()
                            P.dve(lambda e, gt=gt, pg=pg, fc=fc: e.tensor_scalar(gt[:], pg[:, :], b1[:, fc:fc + 1], 7.0, ALU.add, ALU.min),
                                  reads=[pgb, Bb1], writes=[gtb])
                            P.act(lambda e, sg=sg, gt=gt: e.activation(out=sg[:], in_=gt[:], func=AF.Sigmoid, scale=1.702), reads=[gtb], writes=[sgb])
                            P.dve(lambda e, ut=ut, pu=pu, fc=fc: e.tensor_scalar(ut[:], pu[:, :], b1[:, 8 + fc:9 + fc], 7.0, ALU.add, ALU.min),
                                  reads=[pub, Bb1], writes=[utb])
                            P.pool(lambda e, ut=ut: e.tensor_scalar(ut[:], ut[:], -7.0, 1.0, ALU.max, ALU.add), reads=[utb], writes=[utb])
                            P.pool(lambda e, gt=gt, sg=sg: e.tensor_tensor(gt[:], gt[:], sg[:], ALU.mult), reads=[gtb, sgb], writes=[gtb])
                            P.dve(lambda e, gt=gt, ut=ut: e.tensor_tensor(gt[:], gt[:], ut[:], ALU.mult), reads=[gtb, utb], writes=[gtb])
                            P.dve(lambda e, gt=gt, fc=fc: e.tensor_tensor(gl[:, fc, :], gt[:], gwb[:], ALU.mult), reads=[gtb, Bgwb], writes=[Bgl[fc]])
                        for dc in range(8):
                            py, pyb = PS8.get()
                            for fc in range(8):
                                mm(py[:, :], w2[:, fc, dc * 128:(dc + 1) * 128], gl[:, fc, :], fc == 0, fc == 7, [Bw2, Bgl[fc]], [pyb])
                            P.dve(lambda e, py=py, dc=dc, t=t: e.scalar_tensor_tensor(out=xT[:, dc, tcs(t)], in0=py[:, :], scalar=g2c[:, dc:dc + 1],
                                                                                    in1=xT[:, dc, tcs(t)], op0=ALU.mult, op1=ALU.add),
                                  reads=[pyb, B_mod, B_xT[dc][t]], writes=[B_xT[dc][t]])
                for t in range(NCH):
                    for dc in range(8):
                        py, pyb = PS8.get()
                        mm(py[:, :], b2t[:, dc * 128:(dc + 1) * 128], gwT[:, tcs(t)], True, True, [Brw, BgwT[t]], [pyb])
                        P.dve(lambda e, py=py, dc=dc, t=t: e.scalar_tensor_tensor(out=xT[:, dc, tcs(t)], in0=py[:, :], scalar=g2c[:, dc:dc + 1],
                                                                                in1=xT[:, dc, tcs(t)], op0=ALU.mult, op1=ALU.add),
                              reads=[pyb, B_mod, B_xT[dc][t]], writes=[B_xT[dc][t]])
                P.barrier()

            if dbg and l == 0 and b == 0:
                P.dma("sp", lambda e: e.dma_start(out=dbg_out["x2"][:, :, :], in_=xT[:]),
                      reads=[bb for r in B_xT for bb in r], writes=[B_out])
                P.barrier()
        with ExitStack() as es:
            sq_r = Ring([(sb(es, "fsq%d" % i, [128, 512], F32), Buf()) for i in range(3)])
            rs_t, rs_b = sb(es, "frs", [128, 512], F32), Buf()
            fin = sb(es, "fin", [128, 8, 512], F32)
            Bfin = [Buf() for _ in range(8)]
            zero8 = sb(es, "zero8", [128, 8], F32)
            Bz = Buf()
            P.dve(lambda e: e.memset(zero8[:], 0.0), writes=[B_mod])
            orow_r = Ring([(sb(es, "orow%d" % i, [128, D], F32), Buf()) for i in range(2)])
            for t in range(NCH):
                norm_mod("nf", fg, zero8, t, lambda c: fin[:, c, :], lambda c: [Bfin[c]], (sq_r, rs_t, rs_b))
                for jj in range(4):
                    orow, orb = orow_r.get()
                    for g in range(2):
                        pt, pb = PSA.get()
                        for k in range(4):
                            c = g * 4 + k
                            mm(pt[:, k * 128:(k + 1) * 128], fin[:, c, jj * 128:(jj + 1) * 128], ident[:], True, True, [Bfin[c], B_const], [pb])
                        P.act(lambda e, orow=orow, pt=pt, g=g: e.activation(out=orow[:, g * 512:(g + 1) * 512], in_=pt[:, :], func=AF.Identity), reads=[pb], writes=[orb])
                    i = t * 4 + jj
                    P.dma("sp", lambda e, orow=orow, i=i: e.dma_start(out=out_d[b, i * 128:(i + 1) * 128, :], in_=orow[:]), reads=[orb], writes=[B_out])
            P.barrier()

    P.emit(final_bufs=[B_out])
    root.close()
    return nc, P


def _rot_cols(c0, n_rot):
    h = n_rot // 2
    return list(range(c0 + h, c0 + n_rot)) + list(range(c0, c0 + h))


def _prep_inputs(inp, b0, nseq):
    f = lambda a: np.ascontiguousarray(a, dtype=np.float32)
    L = inp["w_in"].shape[0]

    def colT(v, nch):
        v = np.asarray(v)
        return f(v.reshape(v.shape[:-1] + (nch, 128)).swapaxes(-1, -2))

    m = {}
    m["x"] = f(inp["x"][b0:b0 + nseq])
    m["cT"] = f(np.asarray(inp["c"])[b0:b0 + nseq].reshape(nseq, 8, 128).transpose(2, 1, 0))
    m["pos"] = np.ascontiguousarray(np.asarray(inp["positions"])[b0:b0 + nseq], dtype=np.int32)
    m["w_ada"] = f(inp["w_ada"])
    m["b_adaT"] = colT(inp["b_ada"], 48)
    m["n1g"] = colT(inp["norm1_g"], 8)
    m["n2g"] = colT(inp["norm2_g"], 8)
    m["fg"] = colT(inp["final_g"], 8)
    w_in = np.asarray(inp["w_in"])
    m["w_in"] = f(w_in)
    rot = _rot_cols(384, 32)
    for h in range(8):
        rot += _rot_cols(1952 + h * 64, 16)
    rot += _rot_cols(2464, 16)
    for h in range(8):
        rot += _rot_cols(2592 + h * 64, 16)
    rot += _rot_cols(3104, 16)
    rot += rot[:16]
    m["w_in_rot"] = f(w_in[:, :, rot])
    m["b_gateT"] = colT(inp["b_gate"], 24)
    m["qng"] = colT(inp["mla_q_norm"], 2)
    w_uq = np.asarray(inp["mla_w_uq"])
    m["w_uq"] = f(w_uq)
    rq = []
    for h in range(8):
        rq += _rot_cols(h * 96, 32)
    m["w_uq_rot"] = f(w_uq[:, :, rq])
    m["kvng"] = colT(inp["mla_kv_norm"], 1)
    m["w_ukv"] = f(inp["mla_w_ukv"])
    cw = np.asarray(inp["conv_w"])
    m["conv_wT"] = f(cw.reshape(L, 3, 4, 128).transpose(0, 3, 2, 1))
    m["w_branch"] = f(inp["w_branch"])
    m["w_out"] = f(inp["w_out"])
    m["router_w"] = f(inp["router_w"])
    m["router_b"] = f(np.asarray(inp["router_b"]).reshape(L, 1, NEXP))
    m["exp_w1"] = f(inp["exp_w1"])
    b1 = np.asarray(inp["exp_b1"])
    b1g = b1[:, :, 0::2].reshape(L, NEXP, 8, 128).swapaxes(-1, -2)
    b1u = b1[:, :, 1::2].reshape(L, NEXP, 8, 128).swapaxes(-1, -2)
    m["exp_b1T"] = f(np.concatenate([b1g, b1u], axis=-1))
    m["exp_w2"] = f(inp["exp_w2"])
    m["exp_b2"] = f(inp["exp_b2"])
    return m


_CONST = None


def _consts():
    global _CONST
    if _CONST is None:
        c = {}
        c["ident"] = np.eye(128, dtype=np.float32)
        sel = np.zeros((NEXP, NEXP, 128), np.float32)
        for e in range(NEXP):
            sel[e, e, :] = 1.0
        c["sel"] = sel
        rc = np.zeros((32, 4), np.float32)
        i16 = np.arange(16, dtype=np.float32)
        invf32 = np.exp(-math.log(500000.0) * i16 * (2.0 / 32)).astype(np.float32)
        i8 = np.arange(8, dtype=np.float32)
        invf16 = np.exp(-math.log(500000.0) * i8 * (2.0 / 16)).astype(np.float32)
        rc[:, 0] = np.concatenate([invf32, invf32])
        rc[:, 1] = np.concatenate([-np.ones(16), np.ones(16)])
        rc[0:16, 2] = np.concatenate([invf16, invf16])
        rc[0:16, 3] = np.concatenate([-np.ones(8), np.ones(8)])
        c["ropec"] = rc
        k = np.arange(128)
        c["tri"] = (k[:, None] <= k[None, :]).astype(np.float32)
        c["ntri"] = np.where(k[None, :] <= k[:, None], 0.0, -BIG).astype(np.float32)
        _CONST = c
    return _CONST


_NC_CACHE = {}


def kernel(**inputs):
    n_cores = 8
    B = np.asarray(inputs["x"]).shape[0]
    nseq = B // n_cores
    if nseq not in _NC_CACHE:
        _NC_CACHE[nseq] = build(nseq)[0]
    nc = _NC_CACHE[nseq]
    consts = _consts()
    in_maps = []
    for core in range(n_cores):
        m = _prep_inputs(inputs, core * nseq, nseq)
        m.update(consts)
        in_maps.append(m)
    res = run_bass_kernel_spmd(nc, in_maps, core_ids=list(range(n_cores)))
    out = np.concatenate([np.asarray(r["out"]) for r in res.results], axis=0)
    return out.astype(np.float32)
```

```python
import math
import types
import numpy as np
from contextlib import ExitStack
import concourse.bass as bass
import concourse.mybir as mybir
from concourse.bass_utils import run_bass_kernel_spmd

F32 = mybir.dt.float32
BF16 = mybir.dt.bfloat16
I32 = mybir.dt.int32
AF = mybir.ActivationFunctionType
ALU = mybir.AluOpType
AX = mybir.AxisListType

S = 2048
D = 1024
NT = 16
NCH = 4
IN_COLS = 6248
NEXP = 32
BIG = 1.0e30


class Buf:
    __slots__ = ("name", "w", "r", "excl")

    def __init__(self, name="", excl=False):
        self.name = name
        self.w = None
        self.r = []
        self.excl = excl


class Prog:
    def __init__(self, nc, n_dma_sems=40):
        self.nc = nc
        self.ops = []
        self.n_dma_sems = n_dma_sems

    @staticmethod
    def _freeze(fn, depth=0):
        if not isinstance(fn, types.FunctionType) or fn.__closure__ is None or depth > 3:
            return fn
        cells = []
        for c in fn.__closure__:
            try:
                v = c.cell_contents
            except ValueError:
                cells.append(c)
                continue
            if isinstance(v, types.FunctionType):
                v = Prog._freeze(v, depth + 1)
            cells.append(types.CellType(v))
        g = types.FunctionType(fn.__code__, fn.__globals__, fn.__name__, fn.__defaults__, tuple(cells))
        g.__kwdefaults__ = fn.__kwdefaults__
        return g

    def add(self, eng, fn, reads=(), writes=(), dma=False):
        self.ops.append((eng, Prog._freeze(fn), tuple(reads), tuple(writes), dma))

    def pe(self, fn, reads=(), writes=()):
        self.add("pe", fn, reads, writes)

    def act(self, fn, reads=(), writes=()):
        self.add("act", fn, reads, writes)

    def dve(self, fn, reads=(), writes=()):
        self.add("dve", fn, reads, writes)

    def pool(self, fn, reads=(), writes=()):
        self.add("pool", fn, reads, writes)

    def dma(self, q, fn, reads=(), writes=()):
        self.add(q, fn, reads, writes, dma=True)

    def barrier(self):
        self.ops.append(("BAR", None, (), (), False))

    def emit(self, final_bufs=()):
        nc = self.nc
        ops = self.ops
        n = len(ops)
        deps = [None] * n
        signal = [False] * n
        last_on = {}
        dmas_since = []
        bar_deps = {}
        for i, (eng, fn, reads, writes, dma) in enumerate(ops):
            if eng == "BAR":
                bd = set(last_on.values()) | set(dmas_since)
                for j in bd:
                    signal[j] = True
                bar_deps[i] = bd
                dmas_since = []
                continue
            d = set()
            for b in reads:
                if b.w is not None:
                    d.add(b.w)
                if b.excl:
                    d.update(j for j in b.r if ops[j][0] != eng)
            for b in writes:
                if b.w is not None:
                    d.add(b.w)
                d.update(b.r)
            d.discard(i)
            keep = set()
            for j in d:
                jeng, _, jr, jw, jdma = ops[j]
                if not dma and not jdma and jeng == eng:
                    if eng == "pe":
                        continue
                keep.add(j)
            deps[i] = keep
            for j in keep:
                signal[j] = True
            for b in reads:
                if not dma:
                    b.r = [j for j in b.r if ops[j][4] or ops[j][0] != eng]
                b.r.append(i)
            for b in writes:
                b.w = i
                b.r = []
            if dma:
                dmas_since.append(i)
            else:
                last_on[eng] = i
        final_deps = set()
        for b in final_bufs:
            if b.w is not None:
                final_deps.add(b.w)
                signal[b.w] = True

        with ExitStack() as es:
            SEM_LIMIT = 2000
            tot = {e: 0 for e in ("pe", "act", "dve", "pool")}
            for i, (eng, fn, reads, writes, dma) in enumerate(ops):
                if eng in tot and not dma and signal[i]:
                    tot[eng] += 1
            csem = {e: [es.enter_context(nc.semaphore("cs_%s%d" % (e, k))) for k in range(tot[e] // SEM_LIMIT + 1)]
                    for e in tot}
            dsem = [es.enter_context(nc.semaphore("ds%d" % k)) for k in range(self.n_dma_sems)]
            ccount = {e: 0 for e in csem}
            dcount = [0] * self.n_dma_sems
            token = [None] * n
            prevtok = [None] * n
            nd = 0
            nsw = 0
            nhw = 0
            half = self.n_dma_sems // 2
            for i, (eng, fn, reads, writes, dma) in enumerate(ops):
                if eng == "BAR":
                    continue
                if dma:
                    if eng == "pool":
                        k = nsw % half
                        nsw += 1
                    else:
                        k = half + nhw % (self.n_dma_sems - half)
                        nhw += 1
                    nd += 1
                    if dcount[k] > 0:
                        prevtok[i] = (dsem[k], dcount[k])
                    dcount[k] += 16
                    token[i] = (dsem[k], dcount[k])
                elif signal[i]:
                    c = ccount[eng]
                    ccount[eng] += 1
                    token[i] = (csem[eng][c // SEM_LIMIT], c % SEM_LIMIT + 1)
            self.stats = {"n_ops": n, "n_dma": nd, "ccount": dict(ccount)}

            def do_waits(e, waited, toks):
                best = {}
                for (s, v) in toks:
                    if best.get(s.num, (None, 0))[1] < v:
                        best[s.num] = (s, v)
                for num, (s, v) in best.items():
                    if waited.get(num, 0) < v:
                        e.wait_ge(s, v)
                        waited[num] = v

            def emit_engine(ename, e):
                waited = {}
                for i, (eng, fn, reads, writes, dma) in enumerate(ops):
                    if eng == "BAR":
                        do_waits(e, waited, [token[j] for j in bar_deps[i]])
                        continue
                    if eng != ename:
                        continue
                    toks = [token[j] for j in deps[i]]
                    if prevtok[i] is not None:
                        toks.append(prevtok[i])
                    do_waits(e, waited, toks)
                    ins = fn(e)
                    if token[i] is not None:
                        s, v = token[i]
                        ins.then_inc(s, 16 if dma else 1)
                if ename == "sp":
                    do_waits(e, waited, [token[j] for j in final_deps])

            with nc.Block() as block:
                @block.tensor
                def _(e):
                    emit_engine("pe", e)

                @block.scalar
                def _(e):
                    emit_engine("act", e)

                @block.vector
                def _(e):
                    emit_engine("dve", e)

                @block.gpsimd
                def _(e):
                    emit_engine("pool", e)

                @block.sync
                def _(e):
                    emit_engine("sp", e)


class Ring:
    def __init__(self, items):
        self.items = items
        self.i = 0

    def get(self):
        it = self.items[self.i % len(self.items)]
        self.i += 1
        return it


def build(nseq, depth=2, phases=None, dbg=False, nlayers=None):
    nc = bass.Bass("TRN2", target_bir_lowering=False)
    P = Prog(nc)
    root = ExitStack()
    ALL_PHASES = ("prologue", "norm1", "mla", "conv", "d1", "d2", "merge", "moe", "d1_score", "d1_bis", "d1_mask", "d1p_q", "d1p_k", "d1p_w")
    run_phases = set(ALL_PHASES if phases is None else phases)

    def phase(name):
        if name in run_phases:
            with ExitStack() as es_:
                yield es_

    def din(name, shape, dt=F32):
        return nc.dram_tensor(name, list(shape), dt, kind="ExternalInput").ap()

    def dscr(name, shape, dt=BF16):
        if dbg:
            return nc.dram_tensor(name, list(shape), dt, kind="ExternalOutput").ap()
        return nc.dram_tensor(name, list(shape), dt).ap()

    _cnt = [0]

    def sb(es, name, shape, dt=F32):
        _cnt[0] += 1
        return es.enter_context(nc.sbuf_tensor("s%d_%s" % (_cnt[0], name), list(shape), dt))

    x_d = din("x", [nseq, S, D])
    cT_d = din("cT", [128, 8, nseq])
    pos_d = din("pos", [nseq, S], I32)
    w_ada_d = din("w_ada", [depth, D, 6 * D])
    b_adaT_d = din("b_adaT", [depth, 128, 48])
    n1g_d = din("n1g", [depth, 128, 8])
    n2g_d = din("n2g", [depth, 128, 8])
    fg_d = din("fg", [128, 8])
    w_in_d = din("w_in", [depth, D, IN_COLS])
    w_inr_d = din("w_in_rot", [depth, D, 336])
    b_gateT_d = din("b_gateT", [depth, 128, 24])
    qng_d = din("qng", [depth, 128, 2])
    w_uq_d = din("w_uq", [depth, 256, 768])
    w_uqr_d = din("w_uq_rot", [depth, 256, 256])
    kvng_d = din("kvng", [depth, 128, 1])
    w_ukv_d = din("w_ukv", [depth, 128, 1024])
    conv_wT_d = din("conv_wT", [depth, 128, 4, 3])
    w_br_d = din("w_branch", [depth, 3, 512, D])
    w_out_d = din("w_out", [depth, D, D])
    rw_d = din("router_w", [depth, D, NEXP])
    rb_d = din("router_b", [depth, 1, NEXP])
    w1_d = din("exp_w1", [depth, NEXP, D, 2 * D])
    b1T_d = din("exp_b1T", [depth, NEXP, 128, 16])
    w2_d = din("exp_w2", [depth, NEXP, D, D])
    b2_d = din("exp_b2", [depth, NEXP, D])
    ident_d = din("ident", [128, 128])
    sel_d = din("sel", [NEXP, NEXP, 128])
    ropec_d = din("ropec", [32, 4])
    tri_d = din("tri", [128, 128])
    ntri_d = din("ntri", [128, 128])
    out_d = nc.dram_tensor("out", [nseq, S, D], F32, kind="ExternalOutput").ap()
    oa_s = dscr("oa_s", [8, 64, S])
    ob_s = dscr("ob_s", [4, 128, S])
    oc_s = dscr("oc_s", [8, 64, S])
    mask_s = dscr("mask_s", [NT, 128, S])
    dbg_out = {}
    if dbg:
        dbg_out["hT"] = nc.dram_tensor("dbg_hT", [128, 8, S], BF16, kind="ExternalOutput").ap()
        dbg_out["x1"] = nc.dram_tensor("dbg_x1", [128, 8, S], F32, kind="ExternalOutput").ap()
        dbg_out["x2"] = nc.dram_tensor("dbg_x2", [128, 8, S], F32, kind="ExternalOutput").ap()

    xT = sb(root, "xT", [128, 8, S], F32)
    hT = sb(root, "hT", [128, 8, S], BF16)
    ident = sb(root, "ident", [128, 128], F32)
    identb = sb(root, "identb", [128, 128], BF16)
    ones_f = sb(root, "ones_f", [128, 128], F32)
    ones_b = sb(root, "ones_b", [128, 128], BF16)
    tri_b = sb(root, "tri_b", [128, 128], BF16)
    ntri = sb(root, "ntri", [128, 128], F32)
    ropec = sb(root, "ropec", [32, 4], F32)
    cos32 = sb(root, "cos32", [32, S], BF16)
    sin32 = sb(root, "sin32", [32, S], BF16)
    cos16 = sb(root, "cos16", [32, S], BF16)
    sin16 = sb(root, "sin16", [32, S], BF16)
    modc = {}
    for l in range(depth):
        for b in range(nseq):
            for nm in ("gs1", "sh1", "g1", "gs2", "sh2", "g2"):
                modc[(l, b, nm)] = sb(root, "m_%s_%d_%d" % (nm, l, b), [128, 8], F32)
    fg = sb(root, "fg", [128, 8], F32)
    B_xT = [[Buf("xT%d_%d" % (c, t)) for t in range(NCH)] for c in range(8)]
    B_hT = [[Buf("hT%d_%d" % (c, t)) for t in range(NCH)] for c in range(8)]
    B_const = Buf("const")
    B_rope = Buf("rope")
    B_mod = Buf("mod")
    B_oa, B_ob, B_oc = Buf("oa"), Buf("ob"), Buf("oc")
    B_out = Buf("out")
    B_mask = Buf("mask")

    ps_t = [root.enter_context(nc.psum_tensor("ps%d" % i, [128, 512], F32)) for i in range(8)]
    ps_b = [Buf("ps%d" % i, excl=True) for i in range(8)]
    PSA = Ring([(ps_t[i], ps_b[i]) for i in range(0, 4)])
    PSB = Ring([(ps_t[i], ps_b[i]) for i in range(4, 6)])
    PSC = Ring([(ps_t[i], ps_b[i]) for i in range(6, 8)])

    def tcs(t):
        return slice(t * 512, (t + 1) * 512)

    def mm(out, lhsT, rhs, start, stop, reads, writes):
        P.pe(lambda e: e.matmul(out, lhsT, rhs, start=start, stop=stop), reads=reads, writes=writes)

    P.dma("sp", lambda e: e.dma_start(out=ident[:], in_=ident_d[:, :]), writes=[B_const])
    P.dma("sp", lambda e: e.dma_start(out=ntri[:], in_=ntri_d[:, :]), writes=[B_const])
    P.dma("sp", lambda e: e.dma_start(out=ropec[:], in_=ropec_d[:, :]), writes=[B_const])
    P.dma("sp", lambda e: e.dma_start(out=fg[:], in_=fg_d[:, :]), writes=[B_const])
    P.dma("pool", lambda e: e.dma_start(out=tri_b[:], in_=tri_d[:, :]), writes=[B_const])
    P.dma("pool", lambda e: e.dma_start(out=identb[:], in_=ident_d[:, :]), writes=[B_const])
    P.dve(lambda e: e.memset(ones_f[:], 1.0), writes=[B_const])
    P.dve(lambda e: e.memset(ones_b[:], 1.0), writes=[B_const])

    for es in phase("prologue"):
        cact = sb(es, "cact", [128, 8, nseq], F32)
        modT = sb(es, "modT", [128, 48, nseq], F32)
        badaT = sb(es, "badaT", [128, 48], F32)
        ngt = sb(es, "ngt", [128, 8], F32)
        wa = [sb(es, "wa%d" % i, [128, 8, 512], F32) for i in range(2)]
        wab = [Buf("wa%d" % i) for i in range(2)]
        B_c, B_modT, B_bada, B_ng = Buf(), Buf(), Buf(), Buf()
        P.dma("sp", lambda e: e.dma_start(out=cact[:], in_=cT_d[:, :, :]), writes=[B_c])
        P.act(lambda e: e.activation(out=cact[:], in_=cact[:], func=AF.Silu), reads=[B_c], writes=[B_c])
        for l in range(depth):
            P.dma("sp", lambda e, l=l: e.dma_start(out=badaT[:], in_=b_adaT_d[l]), writes=[B_bada])
            for g in range(12):
                w_t, w_b = wa[g % 2], wab[g % 2]
                src = w_ada_d[l, :, g * 512:(g + 1) * 512].rearrange("(kc p) n -> p kc n", p=128)
                P.dma("sp", lambda e, w_t=w_t, src=src: e.dma_start(out=w_t[:], in_=src), writes=[w_b])
                pt, pb = PSA.get()
                for j in range(4):
                    for kc in range(8):
                        mm(pt[:, j * nseq:(j + 1) * nseq], w_t[:, kc, j * 128:(j + 1) * 128], cact[:, kc, :],
                           kc == 0, kc == 7, [w_b, B_c], [pb])
                P.dve(lambda e, pt=pt, g=g: e.tensor_copy(
                    modT[:, g * 4:(g + 1) * 4, :], pt[:, 0:4 * nseq].rearrange("p (j b) -> p j b", j=4)),
                    reads=[pb], writes=[B_modT])
            for b in range(nseq):
                P.dve(lambda e, b=b: e.tensor_tensor(modT[:, :, b], modT[:, :, b], badaT[:], ALU.add),
                      reads=[B_modT, B_bada], writes=[B_modT])
            for (nm_gs, nm_sh, nm_g, base, ng_d) in (("gs1", "sh1", "g1", 0, n1g_d), ("gs2", "sh2", "g2", 24, n2g_d)):
                P.dma("sp", lambda e, ng_d=ng_d, l=l: e.dma_start(out=ngt[:], in_=ng_d[l]), writes=[B_ng])
                for b in range(nseq):
                    gs, sh, gg = modc[(l, b, nm_gs)], modc[(l, b, nm_sh)], modc[(l, b, nm_g)]
                    P.dve(lambda e, sh=sh, b=b, base=base: e.tensor_copy(sh[:], modT[:, base:base + 8, b]),
                          reads=[B_modT], writes=[B_mod])
                    P.dve(lambda e, gs=gs, b=b, base=base: e.scalar_tensor_tensor(
                        out=gs[:], in0=modT[:, base + 8:base + 16, b], scalar=1.0, in1=ngt[:],
                        op0=ALU.add, op1=ALU.mult), reads=[B_modT, B_ng], writes=[B_mod])
                    P.dve(lambda e, gg=gg, b=b, base=base: e.tensor_copy(gg[:], modT[:, base + 16:base + 24, b]),
                          reads=[B_modT], writes=[B_mod])
        P.barrier()

    def norm_mod(es_name, gs, sh, t, dst_fn, dst_bufs_fn, tmp, extra=None):
        sq_r, rs_t, rs_b = tmp
        pt, pb = PSA.get()
        for c in range(8):
            sq, sqb = sq_r.get()
            P.act(lambda e, sq=sq, c=c: e.activation(out=sq[:], in_=xT[:, c, tcs(t)], func=AF.Square),
                  reads=[B_xT[c][t]], writes=[sqb])
            mm(pt[:, :], ones_f[:], sq[:], c == 0, c == 7, [sqb, B_const], [pb])
        P.act(lambda e, pt=pt: e.activation(out=rs_t[:], in_=pt[:, :], func=AF.Sqrt, scale=1.0 / D, bias=1e-6),
              reads=[pb], writes=[rs_b])
        P.dve(lambda e: e.reciprocal(rs_t[:], rs_t[:]), reads=[rs_b], writes=[rs_b])
        for c in range(8):
            sq, sqb = sq_r.get()
            P.dve(lambda e, sq=sq, c=c: e.tensor_tensor(sq[:], xT[:, c, tcs(t)], rs_t[:], ALU.mult),
                  reads=[B_xT[c][t], rs_b], writes=[sqb])
            P.act(lambda e, sq=sq, c=c: e.activation(out=sq[:], in_=sq[:], func=AF.Identity,
                                                     scale=gs[:, c:c + 1], bias=sh[:, c:c + 1]),
                  reads=[sqb, B_mod], writes=[sqb])
            if extra is not None:
                extra(c, sq, sqb)
            P.dve(lambda e, sq=sq, c=c: e.tensor_copy(dst_fn(c), sq[:]), reads=[sqb], writes=dst_bufs_fn(c))

    def load_w(q, dst, src, buf):
        P.dma(q, lambda e: e.dma_start(out=dst, in_=src), writes=[buf])

    def rope_tables(es, b):
        posi = sb(es, "posi", [32, S], I32)
        ang = sb(es, "ang", [32, S], F32)
        t1 = sb(es, "rt1", [32, S], F32)
        t2 = sb(es, "rt2", [32, S], F32)
        ki = sb(es, "rki", [32, S], I32)
        Bp, Ba, B1, B2, Bk = Buf(), Buf(), Buf(), Buf(), Buf()
        P.dma("sp", lambda e: e.dma_start(out=posi[:], in_=pos_d[b, :].partition_broadcast(32)), writes=[Bp])
        C1 = 6.28125
        C2 = 2.0 * math.pi - C1
        for (n, icol, scol, cos_t, sin_t) in ((32, 0, 1, cos32, sin32), (32, 2, 3, cos16, sin16)):
            P.dve(lambda e, n=n: e.tensor_copy(ang[0:n, :], posi[0:n, :]), reads=[Bp], writes=[Ba])
            P.dve(lambda e, n=n, icol=icol: e.tensor_scalar(ang[0:n, :], ang[0:n, :], ropec[0:n, icol:icol + 1], None,
                                                            ALU.mult), reads=[Ba, B_const], writes=[Ba])
            for (shift, dst, signed) in ((0.5 * math.pi, cos_t, False), (0.0, sin_t, True)):
                P.dve(lambda e, n=n, shift=shift: e.tensor_scalar(t1[0:n, :], ang[0:n, :], shift, None, ALU.add),
                      reads=[Ba], writes=[B1])
                P.dve(lambda e, n=n: e.tensor_scalar(t2[0:n, :], t1[0:n, :], 1.0 / (2.0 * math.pi), None, ALU.mult),
                      reads=[B1], writes=[B2])
                P.dve(lambda e, n=n: e.tensor_copy(ki[0:n, :], t2[0:n, :]), reads=[B2], writes=[Bk])
                P.dve(lambda e, n=n: e.tensor_copy(t2[0:n, :], ki[0:n, :]), reads=[Bk], writes=[B2])
                P.dve(lambda e, n=n: e.scalar_tensor_tensor(out=t1[0:n, :], in0=t2[0:n, :], scalar=-C1, in1=t1[0:n, :],
                                                            op0=ALU.mult, op1=ALU.add), reads=[B1, B2], writes=[B1])
                P.dve(lambda e, n=n: e.scalar_tensor_tensor(out=t1[0:n, :], in0=t2[0:n, :], scalar=-C2, in1=t1[0:n, :],
                                                            op0=ALU.mult, op1=ALU.add), reads=[B1, B2], writes=[B1])
                P.dve(lambda e, n=n: e.tensor_scalar(t2[0:n, :], t1[0:n, :], math.pi, -2.0 * math.pi, ALU.is_gt, ALU.mult),
                      reads=[B1], writes=[B2])
                P.dve(lambda e, n=n: e.tensor_tensor(t1[0:n, :], t1[0:n, :], t2[0:n, :], ALU.add),
                      reads=[B1, B2], writes=[B1])
                P.dve(lambda e, n=n: e.tensor_scalar(t1[0:n, :], t1[0:n, :], -math.pi, math.pi, ALU.max, ALU.min),
                      reads=[B1], writes=[B1])
                P.act(lambda e, n=n, dst=dst: e.activation(out=dst[0:n, :], in_=t1[0:n, :], func=AF.Sin),
                      reads=[B1], writes=[B_rope])
                if signed:
                    P.dve(lambda e, n=n, dst=dst, scol=scol: e.tensor_scalar(
                        dst[0:n, :], dst[0:n, :], ropec[0:n, scol:scol + 1], None, ALU.mult),
                        reads=[B_rope, B_const], writes=[B_rope])

    for b in range(nseq):
        with ExitStack() as es:
            xin = [sb(es, "xin%d" % i, [128, D], F32) for i in range(2)]
            xinb = [Buf() for _ in range(2)]
            for i in range(NT):
                xi, xb = xin[i % 2], xinb[i % 2]
                P.dma("sp", lambda e, xi=xi, i=i: e.dma_start(out=xi[:], in_=x_d[b, i * 128:(i + 1) * 128, :]),
                      writes=[xb])
                for g in range(2):
                    pt, pb = PSA.get()
                    for j in range(4):
                        c = g * 4 + j
                        mm(pt[:, j * 128:(j + 1) * 128], xi[:, c * 128:(c + 1) * 128], ident[:], True, True,
                           [xb, B_const], [pb])
                    P.dve(lambda e, pt=pt, g=g, i=i: e.tensor_copy(
                        xT[:, g * 4:(g + 1) * 4, i * 128:(i + 1) * 128],
                        pt[:, :].rearrange("p (c t) -> p c t", c=4)),
                        reads=[pb], writes=[B_xT[c2][i // 4] for c2 in range(g * 4, g * 4 + 4)])
            rope_tables(es, b)
            P.barrier()

        for l in range(depth if nlayers is None else nlayers):
            gs1, sh1, g1c = modc[(l, b, "gs1")], modc[(l, b, "sh1")], modc[(l, b, "g1")]
            gs2, sh2, g2c = modc[(l, b, "gs2")], modc[(l, b, "sh2")], modc[(l, b, "g2")]
            allh = lambda t: [B_hT[c][t] for c in range(8)]

            def wsrc(c0, n, l=l):
                return w_in_d[l, :, c0:c0 + n].rearrange("(kc p) n -> p kc n", p=128)

            def wrsrc(c0, n, l=l):
                return w_inr_d[l, :, c0:c0 + n].rearrange("(kc p) n -> p kc n", p=128)

            def proj(W, wb, c0, M, t, pool=PSA):
                pt, pb = pool.get()
                for kc in range(8):
                    mm(pt[0:M, :], W[:, kc, c0:c0 + M], hT[:, kc, tcs(t)], kc == 0, kc == 7,
                       [wb, B_hT[kc][t]], [pb])
                return pt, pb

            def rope_comb(dst, dstb, A, Ab, Bm, Bb, n, cos_t, sin_t, t, tmp_r):
                t1, t1b = tmp_r.get()
                t2, t2b = tmp_r.get()
                P.dve(lambda e: e.tensor_tensor(t1[0:n, :], A[0:n, :], cos_t[0:n, tcs(t)], ALU.mult),
                      reads=[Ab, B_rope], writes=[t1b])
                P.dve(lambda e: e.tensor_tensor(t2[0:n, :], Bm[0:n, :], sin_t[0:n, tcs(t)], ALU.mult),
                      reads=[Bb, B_rope], writes=[t2b])
                P.dve(lambda e: e.tensor_tensor(dst, t1[0:n, :], t2[0:n, :], ALU.add),
                      reads=[t1b, t2b], writes=[dstb])

            for es in phase("norm1"):
                sq_r = Ring([(sb(es, "sq%d" % i, [128, 512], F32), Buf()) for i in range(3)])
                rs_t, rs_b = sb(es, "rs", [128, 512], F32), Buf()
                for t in range(NCH):
                    norm_mod("n1", gs1, sh1, t, lambda c, t=t: hT[:, c, tcs(t)], lambda c, t=t: [B_hT[c][t]],
                             (sq_r, rs_t, rs_b))
                P.barrier()

            if dbg and l == 0 and b == 0:
                P.dma("sp", lambda e: e.dma_start(out=dbg_out["hT"][:, :, :], in_=hT[:]),
                      reads=[bb for r in B_hT for bb in r], writes=[B_out])
                P.barrier()
            for es in phase("mla"):
                wq, wkv, wkr = sb(es, "wq", [128, 8, 256], BF16), sb(es, "wkv", [128, 8, 160], BF16), sb(es, "wkr", [128, 8, 32], BF16)
                wuq, wuqr = sb(es, "wuq", [128, 2, 768], BF16), sb(es, "wuqr", [128, 2, 256], BF16)
                wukv = sb(es, "wukv", [128, 1024], BF16)
                qng, kvng = sb(es, "qng", [128, 2], F32), sb(es, "kvng", [128, 1], F32)
                Bw = Buf()
                load_w("pool", wq[:], wsrc(0, 256), Bw)
                load_w("pool", wkv[:], wsrc(256, 160), Bw)
                load_w("pool", wkr[:], wrsrc(0, 32), Bw)
                load_w("pool", wuq[:], w_uq_d[l].rearrange("(kc p) n -> p kc n", p=128), Bw)
                load_w("pool", wuqr[:], w_uqr_d[l].rearrange("(kc p) n -> p kc n", p=128), Bw)
                load_w("pool", wukv[:], w_ukv_d[l], Bw)
                load_w("sp", qng[:], qng_d[l], Bw)
                load_w("sp", kvng[:], kvng_d[l], Bw)
                cqT, ckvT, kpeT = sb(es, "cqT", [128, 2, S], BF16), sb(es, "ckvT", [128, S], BF16), sb(es, "kpeT", [32, S], BF16)
                Bcq, Bckv, Bkpe = Buf(), Buf(), Buf()
                raw_r = Ring([(sb(es, "raw%d" % i, [128, 512], F32), Buf()) for i in range(4)])
                tmp_r = Ring([(sb(es, "tmp%d" % i, [128, 512], F32), Buf()) for i in range(4)])
                rstd, rstdb = sb(es, "rstd", [128, 512], F32), Buf()

                def lat_norm(raws, gcol, dst_fn, dstb, nfeat):
                    pt, pb = PSA.get()
                    for i, (rw_t, rw_b) in enumerate(raws):
                        sq, sqb = tmp_r.get()
                        P.act(lambda e, sq=sq, rw_t=rw_t: e.activation(out=sq[:], in_=rw_t[:], func=AF.Square),
                              reads=[rw_b], writes=[sqb])
                        mm(pt[:, :], ones_f[:], sq[:], i == 0, i == len(raws) - 1, [sqb, B_const], [pb])
                    P.act(lambda e, pt=pt: e.activation(out=rstd[:], in_=pt[:, :], func=AF.Sqrt, scale=1.0 / nfeat, bias=1e-6),
                          reads=[pb], writes=[rstdb])
                    P.dve(lambda e: e.reciprocal(rstd[:], rstd[:]), reads=[rstdb], writes=[rstdb])
                    for i, (rw_t, rw_b) in enumerate(raws):
                        P.dve(lambda e, i=i, rw_t=rw_t: e.scalar_tensor_tensor(
                            out=dst_fn(i), in0=rw_t[:], scalar=gcol[:, i:i + 1], in1=rstd[:], op0=ALU.mult, op1=ALU.mult),
                            reads=[rw_b, rstdb, Bw], writes=[dstb])

                for t in range(NCH):
                    raws = []
                    for j in range(2):
                        pt, pb = proj(wq, Bw, j * 128, 128, t)
                        rw_t, rw_b = raw_r.get()
                        P.act(lambda e, rw_t=rw_t, pt=pt: e.activation(out=rw_t[:], in_=pt[:, :], func=AF.Identity),
                              reads=[pb], writes=[rw_b])
                        raws.append((rw_t, rw_b))
                    lat_norm(raws, qng, lambda i, t=t: cqT[:, i, tcs(t)], Bcq, 256.0)
                    pt, pb = proj(wkv, Bw, 0, 128, t)
                    rw_t, rw_b = raw_r.get()
                    P.act(lambda e, rw_t=rw_t, pt=pt: e.activation(out=rw_t[:], in_=pt[:, :], func=AF.Identity),
                          reads=[pb], writes=[rw_b])
                    lat_norm([(rw_t, rw_b)], kvng, lambda i, t=t: ckvT[:, tcs(t)], Bckv, 128.0)
                    A, Ab = proj(wkv, Bw, 128, 32, t)
                    Bm, Bb = proj(wkr, Bw, 0, 32, t)
                    rope_comb(kpeT[0:32, tcs(t)], Bkpe, A, Ab, Bm, Bb, 32, cos32, sin32, t, tmp_r)

                qr = [sb(es, "qr%d" % i, [32, S], BF16) for i in range(2)]
                qn = [sb(es, "qn%d" % i, [64, S], BF16) for i in range(2)]
                kn = [sb(es, "kn%d" % i, [64, S], BF16) for i in range(2)]
                Vh = [sb(es, "Vh%d" % i, [128, NT, 64], BF16) for i in range(2)]
                hb = [[Buf() for _ in range(4)] for _ in range(2)]
                PT_r = Ring([(sb(es, "PT%d" % i, [128, 512], BF16), Buf()) for i in range(3)])
                rD, rDb = sb(es, "rD", [64, 512], F32), Buf()
                o_r = Ring([(sb(es, "o%d" % i, [64, 512], BF16), Buf()) for i in range(2)])
                sc_a = 96.0 ** -0.5
                for h in range(8):
                    s2 = h % 2
                    Bqr, Bqn, Bkn, BV = hb[s2]
                    for t in range(NCH):
                        def cproj(W, c0, M, t=t):
                            pt, pb = PSA.get()
                            for kc in range(2):
                                mm(pt[0:M, :], W[:, kc, c0:c0 + M], cqT[:, kc, tcs(t)], kc == 0, kc == 1, [Bw, Bcq], [pb])
                            return pt, pb
                        A, Ab = cproj(wuq, h * 96, 32)
                        Bm, Bb = cproj(wuqr, h * 32, 32)
                        rope_comb(qr[s2][0:32, tcs(t)], Bqr, A, Ab, Bm, Bb, 32, cos32, sin32, t, tmp_r)
                        pt, pb = cproj(wuq, h * 96 + 32, 64)
                        P.act(lambda e, pt=pt, t=t, s2=s2: e.activation(out=qn[s2][0:64, tcs(t)], in_=pt[0:64, :], func=AF.Identity),
                              reads=[pb], writes=[Bqn])
                        pt, pb = PSA.get()
                        mm(pt[0:64, :], wukv[:, h * 128:h * 128 + 64], ckvT[:, tcs(t)], True, True, [Bw, Bckv], [pb])
                        P.act(lambda e, pt=pt, t=t, s2=s2: e.activation(out=kn[s2][0:64, tcs(t)], in_=pt[0:64, :], func=AF.Identity),
                              reads=[pb], writes=[Bkn])
                    for g in range(2):
                        pt, pb = PSA.get()
                        for j in range(8):
                            i = g * 8 + j
                            mm(pt[:, j * 64:(j + 1) * 64], ckvT[:, i * 128:(i + 1) * 128], wukv[:, h * 128 + 64:h * 128 + 128],
                               True, True, [Bw, Bckv], [pb])
                        P.dve(lambda e, pt=pt, g=g, s2=s2: e.tensor_copy(Vh[s2][:, g * 8:(g + 1) * 8, :],
                                                                        pt[:, :].rearrange("p (j d) -> p j d", j=8)),
                              reads=[pb], writes=[BV])
                    for c in range(NCH):
                        O, Ob = PSB.get()
                        Dn, Db = PSC.get()
                        nk = 4 * c + 4
                        for kt in range(nk):
                            j = kt - 4 * c
                            q0 = max(j, 0) * 128
                            qs = slice(c * 512 + q0, (c + 1) * 512)
                            ks = slice(kt * 128, (kt + 1) * 128)
                            pt, pb = PSA.get()
                            mm(pt[:, q0:512], kpeT[0:32, ks], qr[s2][0:32, qs], True, False, [Bkpe, Bqr], [pb])
                            mm(pt[:, q0:512], kn[s2][0:64, ks], qn[s2][0:64, qs], False, True, [Bkn, Bqn], [pb])
                            PT, PTb = PT_r.get()
                            P.act(lambda e, PT=PT, pt=pt, q0=q0: e.activation(out=PT[:, q0:512], in_=pt[:, q0:512], func=AF.Exp, scale=sc_a),
                                  reads=[pb], writes=[PTb])
                            if j >= 0:
                                P.dve(lambda e, PT=PT, q0=q0: e.tensor_tensor(PT[:, q0:q0 + 128], PT[:, q0:q0 + 128], tri_b[:], ALU.mult),
                                      reads=[PTb, B_const], writes=[PTb])
                            mm(O[0:64, q0:512], Vh[s2][:, kt, :], PT[:, q0:512], kt == 0, kt == nk - 1, [BV, PTb], [Ob])
                            mm(Dn[0:64, q0:512], ones_b[:, 0:64], PT[:, q0:512], kt == 0, kt == nk - 1, [B_const, PTb], [Db])
                        P.dve(lambda e, Dn=Dn: e.reciprocal(rD[:], Dn[0:64, :]), reads=[Db], writes=[rDb])
                        o_t, o_b = o_r.get()
                        P.dve(lambda e, O=O, o_t=o_t: e.tensor_tensor(o_t[:], O[0:64, :], rD[:], ALU.mult),
                              reads=[Ob, rDb], writes=[o_b])
                        P.dma("sp", lambda e, o_t=o_t, h=h, c=c: e.dma_start(out=oa_s[h, :, tcs(c)], in_=o_t[:]),
                              reads=[o_b], writes=[B_oa])
                P.barrier()

            for es in phase("conv"):
                cw = sb(es, "cw", [128, 4, 3], F32)
                Bcw = Buf()
                load_w("sp", cw[:], conv_wT_d[l], Bcw)
                wc_r = Ring([(sb(es, "wc%d" % i, [128, 8, 3, 128], BF16), Buf()) for i in range(2)])
                u, Bu = sb(es, "u", [128, S + 2], F32), Buf()
                y, By = sb(es, "y", [128, S], F32), Buf()
                gb, Bgb = sb(es, "gb", [128, S], BF16), Buf()
                ob_r = Ring([(sb(es, "obt%d" % i, [128, S], BF16), Buf()) for i in range(2)])
                tmp_r = Ring([(sb(es, "ctmp%d" % i, [128, 512], F32), Buf()) for i in range(2)])
                P.dve(lambda e: e.memset(u[:, 0:2], 0.0), writes=[Bu])
                for j in range(4):
                    wc, wcb = wc_r.get()
                    for k in range(3):
                        load_w("pool", wc[:, :, k, :], wsrc(416 + k * 512 + j * 128, 128), wcb)
                    for t in range(NCH):
                        def cp(k, t=t, wc=wc, wcb=wcb):
                            pt, pb = PSA.get()
                            for kc in range(8):
                                mm(pt[:, :], wc[:, kc, k, :], hT[:, kc, tcs(t)], kc == 0, kc == 7, [wcb, B_hT[kc][t]], [pb])
                            return pt, pb
                        pgc, pgcb = cp(1)
                        phc, phcb = cp(2)
                        pgb, pgbb = cp(0)
                        tm, tmb = tmp_r.get()
                        P.act(lambda e, tm=tm, pgc=pgc: e.activation(out=tm[:], in_=pgc[:, :], func=AF.Identity), reads=[pgcb], writes=[tmb])
                        P.dve(lambda e, tm=tm, phc=phc, t=t: e.tensor_tensor(u[:, 2 + t * 512:2 + (t + 1) * 512], tm[:], phc[:, :], ALU.mult),
                              reads=[tmb, phcb], writes=[Bu])
                        P.act(lambda e, pgb=pgb, t=t: e.activation(out=gb[:, tcs(t)], in_=pgb[:, :], func=AF.Identity), reads=[pgbb], writes=[Bgb])
                    P.dve(lambda e, j=j: e.tensor_scalar(y[:], u[:, 2:S + 2], cw[:, j, 2:3], None, ALU.mult), reads=[Bu, Bcw], writes=[By])
                    P.dve(lambda e, j=j: e.scalar_tensor_tensor(out=y[:], in0=u[:, 1:S + 1], scalar=cw[:, j, 1:2], in1=y[:], op0=ALU.mult, op1=ALU.add),
                          reads=[Bu, Bcw, By], writes=[By])
                    P.dve(lambda e, j=j: e.scalar_tensor_tensor(out=y[:], in0=u[:, 0:S], scalar=cw[:, j, 0:1], in1=y[:], op0=ALU.mult, op1=ALU.add),
                          reads=[Bu, Bcw, By], writes=[By])
                    ot, otb = ob_r.get()
                    P.dve(lambda e, ot=ot: e.tensor_tensor(ot[:], y[:], gb[:], ALU.mult), reads=[By, Bgb], writes=[otb])
                    P.dma("sp", lambda e, ot=ot, j=j: e.dma_start(out=ob_s[j], in_=ot[:]), reads=[otb], writes=[B_ob])
                P.barrier()

            for es in phase("d1"):
                qiT, kiT = sb(es, "qiT", [64, 8, S], BF16), sb(es, "kiT", [64, S], BF16)
                iw = sb(es, "iw", [128, NT, 8], F32)
                Bqi, Bki, Biw = Buf(), Buf(), Buf()
                with ExitStack() as es2:
                    wiq, wiqr = sb(es2, "wiq", [128, 8, 512], BF16), sb(es2, "wiqr", [128, 8, 144], BF16)
                    wik, wikr = sb(es2, "wik", [128, 8, 128], BF16), sb(es2, "wikr", [128, 8, 128], BF16)
                    tmp_r = Ring([(sb(es2, "dtmp%d" % i, [128, 512], F32), Buf()) for i in range(4)])
                    Bw = Buf()
                    load_w("pool", wiq[:], wsrc(2592, 512), Bw)
                    load_w("pool", wiqr[:], wrsrc(176, 144), Bw)
                    load_w("pool", wik[:], wsrc(3104, 128), Bw)
                    load_w("pool", wikr[:], wrsrc(208, 128), Bw)
                    for t in range(NCH):
                        for h in range(8 if "d1p_q" in run_phases else 0):
                            A, Ab = proj(wiq, Bw, h * 64, 64, t)
                            Bm, Bb = proj(wiqr, Bw, h * 16, 32, t)
                            P.act(lambda e, A=A, h=h, t=t: e.activation(out=qiT[0:64, h, tcs(t)], in_=A[0:64, :], func=AF.Identity),
                                  reads=[Ab], writes=[Ab, Bqi])
                            rope_comb(qiT[0:32, h, tcs(t)], Bqi, A, Ab, Bm, Bb, 32, cos16, sin16, t, tmp_r)
                        if "d1p_k" in run_phases:
                            A, Ab = proj(wik, Bw, 0, 64, t)
                            Bm, Bb = proj(wikr, Bw, 96, 32, t)
                            P.act(lambda e, A=A, t=t: e.activation(out=kiT[0:64, tcs(t)], in_=A[0:64, :], func=AF.Identity), reads=[Ab], writes=[Ab, Bki])
                            rope_comb(kiT[0:32, tcs(t)], Bki, A, Ab, Bm, Bb, 32, cos16, sin16, t, tmp_r)
                        for jj in range(4 if "d1p_w" in run_phases else 0):
                            i = t * 4 + jj
                            pt, pb = PSA.get()
                            for kc in range(8):
                                mm(pt[:, 0:64], hT[:, kc, i * 128:(i + 1) * 128], wik[:, kc, 64:128], kc == 0, kc == 7, [Bw, B_hT[kc][t]], [pb])
                            P.dve(lambda e, pt=pt, i=i: e.tensor_copy(iw[:, i, :], pt[:, 0:8]), reads=[pb], writes=[Biw])
                    P.barrier()
                sc, Bsc = sb(es, "sc", [128, S], F32), Buf()
                rl_r = Ring([(sb(es, "rl%d" % i, [128, 512], BF16), Buf()) for i in range(3)])
                dg, Bdg = sb(es, "dg", [128, 8, 128], BF16), Buf()
                mk, Bmk = sb(es, "mk", [128, S], BF16), Buf()
                mT_r = Ring([(sb(es, "mT%d" % i, [128, 4, 128], BF16), Buf()) for i in range(2)])
                st = sb(es, "st", [128, 64], F32)
                Bst = Buf()
                NIT = 18
                ACC = [PSB.get(), PSB.get(), PSC.get(), PSC.get()]
                for qt in range(NT if "d1_score" in run_phases else 0):
                    nk = (qt + 1) * 128
                    nkc = (nk + 511) // 512
                    for h in range(8):
                        P.dve(lambda e, h=h, qt=qt: e.tensor_scalar(dg[:, h, :], identb[:], iw[:, qt, h:h + 1], None, ALU.mult),
                              reads=[B_const, Biw], writes=[Bdg])
                    for h in range(8):
                        for kk in range(nkc):
                            w = min(512, nk - kk * 512)
                            pt, pb = PSA.get()
                            mm(pt[:, 0:w], qiT[0:64, h, qt * 128:(qt + 1) * 128], kiT[0:64, kk * 512:kk * 512 + w], True, True, [Bqi, Bki], [pb])
                            rl, rlb = rl_r.get()
                            P.act(lambda e, rl=rl, pt=pt, w=w: e.activation(out=rl[:, 0:w], in_=pt[:, 0:w], func=AF.Relu), reads=[pb], writes=[rlb])
                            at, ab = ACC[kk]
                            mm(at[:, 0:w], dg[:, h, :], rl[:, 0:w], h == 0, h == 7, [Bdg, rlb], [ab])
                    for kk in range(nkc):
                        w = min(512, nk - kk * 512)
                        at, ab = ACC[kk]
                        P.act(lambda e, at=at, kk=kk, w=w: e.activation(out=sc[:, kk * 512:kk * 512 + w], in_=at[:, 0:w], func=AF.Identity),
                              reads=[ab], writes=[Bsc])
                    if "d1_bis" not in run_phases:
                        continue
                    if nk <= 256:
                        P.dve(lambda e: e.memset(st[:, 0:1], -1.0e29), writes=[Bst])
                        P.dve(lambda e, qt=qt: e.tensor_tensor(sc[:, qt * 128:(qt + 1) * 128], sc[:, qt * 128:(qt + 1) * 128], ntri[:], ALU.add),
                              reads=[Bsc, B_const], writes=[Bsc])
                    else:
                        P.dve(lambda e, nk=nk: e.tensor_reduce(out=st[:, 1:2], in_=sc[:, 0:nk], axis=AX.X, op=ALU.min), reads=[Bsc], writes=[Bst])
                        P.dve(lambda e, nk=nk: e.tensor_reduce(out=st[:, 2:3], in_=sc[:, 0:nk], axis=AX.X, op=ALU.max), reads=[Bsc], writes=[Bst])
                        P.dve(lambda e, qt=qt: e.tensor_tensor(sc[:, qt * 128:(qt + 1) * 128], sc[:, qt * 128:(qt + 1) * 128], ntri[:], ALU.add),
                              reads=[Bsc, B_const], writes=[Bsc])
                        P.dve(lambda e: e.tensor_tensor(st[:, 3:4], st[:, 2:3], st[:, 1:2], ALU.subtract), reads=[Bst], writes=[Bst])
                        P.dve(lambda e: e.tensor_scalar(st[:, 3:4], st[:, 3:4], 1.0001, 1e-6, ALU.mult, ALU.add), reads=[Bst], writes=[Bst])
                        P.dve(lambda e: e.tensor_copy(st[:, 0:1], st[:, 1:2]), reads=[Bst], writes=[Bst])
                        for i in range(NIT):
                            P.dve(lambda e, i=i: e.tensor_scalar(st[:, 8 + i:9 + i], st[:, 3:4], 2.0 ** -(i + 1), None, ALU.mult), reads=[Bst], writes=[Bst])
                        for i in range(NIT):
                            P.dve(lambda e, i=i: e.tensor_tensor(st[:, 4:5], st[:, 0:1], st[:, 8 + i:9 + i], ALU.add), reads=[Bst], writes=[Bst])
                            P.dve(lambda e, nk=nk: e.tensor_scalar(mk[:, 0:nk], sc[:, 0:nk], st[:, 4:5], None, ALU.is_ge, ALU.add, accum_out=st[:, 5:6]),
                                  reads=[Bsc, Bst], writes=[Bmk, Bst])
                            P.dve(lambda e, i=i: e.tensor_scalar(st[:, 6:7], st[:, 5:6], 255.5, st[:, 8 + i:9 + i], ALU.is_ge, ALU.mult), reads=[Bst], writes=[Bst])
                            P.dve(lambda e: e.tensor_tensor(st[:, 0:1], st[:, 0:1], st[:, 6:7], ALU.add), reads=[Bst], writes=[Bst])
                    if "d1_mask" not in run_phases:
                        continue
                    P.dve(lambda e, nk=nk: e.tensor_scalar(mk[:, 0:nk], sc[:, 0:nk], st[:, 0:1], None, ALU.is_ge), reads=[Bsc, Bst], writes=[Bmk])
                    for g in range((qt + 4) // 4):
                        k0 = g * 4
                        nb = min(4, qt + 1 - k0)
                        pt, pb = PSA.get()
                        for jj in range(nb):
                            kt = k0 + jj
                            mm(pt[:, jj * 128:(jj + 1) * 128], mk[:, kt * 128:(kt + 1) * 128], identb[:], True, True, [Bmk, B_const], [pb])
                        mT, mTb = mT_r.get()
                        P.act(lambda e, mT=mT, pt=pt, nb=nb: e.activation(out=mT[:, 0:nb, :], in_=pt[:, 0:nb * 128].rearrange("p (k q) -> p k q", k=nb), func=AF.Identity),
                              reads=[pb], writes=[mTb])
                        P.dma("sp", lambda e, mT=mT, k0=k0, nb=nb, qt=qt: e.dma_start(
                            out=mask_s[k0:k0 + nb, :, qt * 128:(qt + 1) * 128].rearrange("k p q -> p k q"), in_=mT[:, 0:nb, :]),
                            reads=[mTb], writes=[B_mask])
                P.barrier()

            for es in phase("d2"):
                qcT, kcT = sb(es, "qcT", [64, 8, S], BF16), sb(es, "kcT", [64, S], BF16)
                Vc = sb(es, "Vc", [128, NT, 64], BF16)
                Bqc, Bkc, BVc = Buf(), Buf(), Buf()
                with ExitStack() as es2:
                    wdq, wdqr = sb(es2, "wdq", [128, 8, 512], BF16), sb(es2, "wdqr", [128, 8, 144], BF16)
                    wkc, wkcr = sb(es2, "wkc", [128, 8, 128], BF16), sb(es2, "wkcr", [128, 8, 128], BF16)
                    tmp_r = Ring([(sb(es2, "etmp%d" % i, [128, 512], F32), Buf()) for i in range(4)])
                    Bw = Buf()
                    load_w("pool", wdq[:], wsrc(1952, 512), Bw)
                    load_w("pool", wdqr[:], wrsrc(32, 144), Bw)
                    load_w("pool", wkc[:], wsrc(2464, 128), Bw)
                    load_w("pool", wkcr[:], wrsrc(128, 128), Bw)
                    for t in range(NCH):
                        for h in range(8):
                            A, Ab = proj(wdq, Bw, h * 64, 64, t)
                            Bm, Bb = proj(wdqr, Bw, h * 16, 32, t)
                            P.act(lambda e, A=A, h=h, t=t: e.activation(out=qcT[0:64, h, tcs(t)], in_=A[0:64, :], func=AF.Identity), reads=[Ab], writes=[Ab, Bqc])
                            rope_comb(qcT[0:32, h, tcs(t)], Bqc, A, Ab, Bm, Bb, 32, cos16, sin16, t, tmp_r)
                        A, Ab = proj(wkc, Bw, 0, 64, t)
                        Bm, Bb = proj(wkcr, Bw, 32, 32, t)
                        P.act(lambda e, A=A, t=t: e.activation(out=kcT[0:64, tcs(t)], in_=A[0:64, :], func=AF.Identity), reads=[Ab], writes=[Ab, Bkc])
                        rope_comb(kcT[0:32, tcs(t)], Bkc, A, Ab, Bm, Bb, 32, cos16, sin16, t, tmp_r)
                        pt, pb = PSA.get()
                        for jj in range(4):
                            i = t * 4 + jj
                            for kc in range(8):
                                mm(pt[:, jj * 64:(jj + 1) * 64], hT[:, kc, i * 128:(i + 1) * 128], wkc[:, kc, 64:128], kc == 0, kc == 7, [Bw, B_hT[kc][t]], [pb])
                        P.dve(lambda e, pt=pt, t=t: e.tensor_copy(Vc[:, t * 4:(t + 1) * 4, :], pt[:, 0:256].rearrange("p (j d) -> p j d", j=4)),
                              reads=[pb], writes=[BVc])
                    P.barrier()
                mTc, BmTc = sb(es, "mTc", [128, NT, 512], BF16), Buf()
                PT_r = Ring([(sb(es, "PTc%d" % i, [128, 512], BF16), Buf()) for i in range(3)])
                rD, rDb = sb(es, "rDc", [64, 512], F32), Buf()
                o_r = Ring([(sb(es, "oc%d" % i, [64, 512], BF16), Buf()) for i in range(2)])
                sc_c = 64.0 ** -0.5
                for c in range(NCH):
                    nk = 4 * c + 4
                    for kt in range(nk):
                        q0 = max(kt - 4 * c, 0) * 128
                        P.dma("sp", lambda e, kt=kt, q0=q0, c=c: e.dma_start(out=mTc[:, kt, q0:512], in_=mask_s[kt, :, c * 512 + q0:(c + 1) * 512]),
                              reads=[B_mask], writes=[BmTc])
                    for h in range(8):
                        O, Ob = PSB.get()
                        Dn, Db = PSC.get()
                        for kt in range(nk):
                            q0 = max(kt - 4 * c, 0) * 128
                            qs = slice(c * 512 + q0, (c + 1) * 512)
                            ks = slice(kt * 128, (kt + 1) * 128)
                            pt, pb = PSA.get()
                            mm(pt[:, q0:512], kcT[0:64, ks], qcT[0:64, h, qs], True, True, [Bkc, Bqc], [pb])
                            PT, PTb = PT_r.get()
                            P.act(lambda e, PT=PT, pt=pt, q0=q0: e.activation(out=PT[:, q0:512], in_=pt[:, q0:512], func=AF.Exp, scale=sc_c), reads=[pb], writes=[PTb])
                            P.dve(lambda e, PT=PT, q0=q0, kt=kt: e.tensor_tensor(PT[:, q0:512], PT[:, q0:512], mTc[:, kt, q0:512], ALU.mult),
                                  reads=[PTb, BmTc], writes=[PTb])
                            mm(O[0:64, q0:512], Vc[:, kt, :], PT[:, q0:512], kt == 0, kt == nk - 1, [BVc, PTb], [Ob])
                            mm(Dn[0:64, q0:512], ones_b[:, 0:64], PT[:, q0:512], kt == 0, kt == nk - 1, [B_const, PTb], [Db])
                        P.dve(lambda e, Dn=Dn: e.reciprocal(rD[:], Dn[0:64, :]), reads=[Db], writes=[rDb])
                        o_t, o_b = o_r.get()
                        P.dve(lambda e, O=O, o_t=o_t: e.tensor_tensor(o_t[:], O[0:64, :], rD[:], ALU.mult), reads=[Ob, rDb], writes=[o_b])
                        P.dma("sp", lambda e, o_t=o_t, h=h, c=c: e.dma_start(out=oc_s[h, :, tcs(c)], in_=o_t[:]), reads=[o_b], writes=[B_oc])
                P.barrier()

            for es in phase("merge"):
                wo, Bwo = sb(es, "wo", [128, 8, D], BF16), Buf()
                load_w("pool", wo[:], w_out_d[l].rearrange("(kc p) n -> p kc n", p=128), Bwo)
                bg, Bbg = sb(es, "bg", [128, 24], F32), Buf()
                load_w("sp", bg[:], b_gateT_d[l], Bbg)
                oa_t, ob_t, oc_t = sb(es, "oa_t", [64, 8, 512], BF16), sb(es, "ob_t", [128, 4, 512], BF16), sb(es, "oc_t", [64, 8, 512], BF16)
                Bo = Buf()
                wg_r = Ring([(sb(es, "wg%d" % i, [128, 8, 3, 128], BF16), sb(es, "wba%d" % i, [64, 8, 128], BF16),
                              sb(es, "wbb%d" % i, [128, 4, 128], BF16), sb(es, "wbc%d" % i, [64, 8, 128], BF16), Buf()) for i in range(2)])
                g_r = Ring([(sb(es, "g%d" % i, [128, 512], BF16), Buf()) for i in range(2)])
                a3 = [(sb(es, "a3%d" % i, [128, 512], F32), Buf()) for i in range(3)]
                mg, Bmg = sb(es, "mg", [128, 8, 512], BF16), Buf()
                for t in range(NCH):
                    P.dma("sp", lambda e, t=t: e.dma_start(out=oa_t[:], in_=oa_s[:, :, tcs(t)].rearrange("h p t -> p h t")), reads=[B_oa], writes=[Bo])
                    P.dma("sp", lambda e, t=t: e.dma_start(out=ob_t[:], in_=ob_s[:, :, tcs(t)].rearrange("h p t -> p h t")), reads=[B_ob], writes=[Bo])
                    P.dma("sp", lambda e, t=t: e.dma_start(out=oc_t[:], in_=oc_s[:, :, tcs(t)].rearrange("h p t -> p h t")), reads=[B_oc], writes=[Bo])
                    for dc in range(8):
                        wg, wba, wbb, wbc, Bwg = wg_r.get()
                        for n3 in range(3):
                            load_w("pool", wg[:, :, n3, :], wsrc(3176 + n3 * 1024 + dc * 128, 128), Bwg)
                        load_w("pool", wba[:], w_br_d[l, 0, :, dc * 128:(dc + 1) * 128].rearrange("(h p) n -> p h n", p=64), Bwg)
                        load_w("pool", wbb[:], w_br_d[l, 1, :, dc * 128:(dc + 1) * 128].rearrange("(h p) n -> p h n", p=128), Bwg)
                        load_w("pool", wbc[:], w_br_d[l, 2, :, dc * 128:(dc + 1) * 128].rearrange("(h p) n -> p h n", p=64), Bwg)
                        for n3 in range(3):
                            py, pyb = PSA.get()
                            if n3 == 1:
                                for k in range(4):
                                    mm(py[:, :], wbb[:, k, :], ob_t[:, k, :], k == 0, k == 3, [Bwg, Bo], [pyb])
                            else:
                                wb_, o_ = (wba, oa_t) if n3 == 0 else (wbc, oc_t)
                                for k in range(8):
                                    mm(py[:, :], wb_[0:64, k, :], o_[0:64, k, :], k == 0, k == 7, [Bwg, Bo], [pyb])
                            pg, pgb = PSA.get()
                            for kc in range(8):
                                mm(pg[:, :], wg[:, kc, n3, :], hT[:, kc, tcs(t)], kc == 0, kc == 7, [Bwg, B_hT[kc][t]], [pgb])
                            g_t, g_b = g_r.get()
                            P.act(lambda e, g_t=g_t, pg=pg, n3=n3, dc=dc: e.activation(out=g_t[:], in_=pg[:, :], func=AF.Sigmoid,
                                                                                     bias=bg[:, n3 * 8 + dc:n3 * 8 + dc + 1]),
                                  reads=[pgb, Bbg], writes=[g_b])
                            P.dve(lambda e, n3=n3, py=py, g_t=g_t: e.tensor_tensor(a3[n3][0][:], py[:, :], g_t[:], ALU.mult),
                                  reads=[pyb, g_b], writes=[a3[n3][1]])
                        P.dve(lambda e: e.tensor_tensor(a3[0][0][:], a3[0][0][:], a3[1][0][:], ALU.add), reads=[a3[0][1], a3[1][1]], writes=[a3[0][1]])
                        P.dve(lambda e, dc=dc: e.tensor_tensor(mg[:, dc, :], a3[0][0][:], a3[2][0][:], ALU.add), reads=[a3[0][1], a3[2][1]], writes=[Bmg])
                    for dc2 in range(8):
                        po, pob = PSA.get()
                        for dc in range(8):
                            mm(po[:, :], wo[:, dc, dc2 * 128:(dc2 + 1) * 128], mg[:, dc, :], dc == 0, dc == 7, [Bwo, Bmg], [pob])
                        P.dve(lambda e, po=po, dc2=dc2, t=t: e.scalar_tensor_tensor(out=xT[:, dc2, tcs(t)], in0=po[:, :], scalar=g1c[:, dc2:dc2 + 1],
                                                                                  in1=xT[:, dc2, tcs(t)], op0=ALU.mult, op1=ALU.add),
                              reads=[pob, B_mod, B_xT[dc2][t]], writes=[B_xT[dc2][t]])
                P.barrier()

            if dbg and l == 0 and b == 0:
                P.dma("sp", lambda e: e.dma_start(out=dbg_out["x1"][:, :, :], in_=xT[:]),
                      reads=[bb for r in B_xT for bb in r], writes=[B_out])
                P.barrier()
            for es in phase("moe"):
                rw, Brw = sb(es, "rw", [128, 8, NEXP], F32), Buf()
                rb, b2t = sb(es, "rb", [1, NEXP], F32), sb(es, "b2t", [NEXP, D], F32)
                load_w("sp", rw[:], rw_d[l].rearrange("(kc p) n -> p kc n", p=128), Brw)
                load_w("sp", rb[:], rb_d[l], Brw)
                load_w("sp", b2t[:], b2_d[l], Brw)
                w1, b1 = sb(es, "w1", [128, 8, 2 * D], BF16), sb(es, "b1", [128, 16], F32)
                Bw1 = [Buf(), Buf()]
                Bb1 = Buf()
                b1p = sb(es, "b1p", [128, 8], F32)
                w2, Bw2 = sb(es, "w2", [128, 8, D], BF16), Buf()
                gl, Bgl = sb(es, "gl", [128, 8, 512], BF16), [Buf() for _ in range(8)]
                gwT, BgwT = sb(es, "gwT", [NEXP, S], F32), [Buf() for _ in range(NCH)]
                gwb, Bgwb = sb(es, "gwb", [128, 512], F32), Buf()
                with ExitStack() as es2:
                    sq_r = Ring([(sb(es2, "msq%d" % i, [128, 512], F32), Buf()) for i in range(2)])
                    rs_t, rs_b = sb(es2, "mrs", [128, 512], F32), Buf()
                    lg, Blg = sb(es2, "lg", [128, NEXP], F32), Buf()
                    ex = sb(es2, "ex", [128, NEXP], F32)
                    mk4 = sb(es2, "mk4", [128, NEXP], F32)
                    t8 = sb(es2, "t8", [128, 16], F32)
                    RL = [PSB.get(), PSB.get(), PSC.get(), PSC.get()]
                    for t in range(NCH):
                        def router_extra(c, sq, sqb, t=t):
                            for jj in range(4):
                                rt, rtb = RL[jj]
                                mm(rt[:, 0:NEXP], sq[:, jj * 128:(jj + 1) * 128], rw[:, c, :], c == 0, False, [sqb, Brw], [rtb])
                        norm_mod("n2", gs2, sh2, t, lambda c, t=t: hT[:, c, tcs(t)], lambda c, t=t: [B_hT[c][t]],
                                 (sq_r, rs_t, rs_b), extra=router_extra)
                        for jj in range(4):
                            rt, rtb = RL[jj]
                            mm(rt[:, 0:NEXP], ones_f[0:1, :], rb[0:1, :], False, True, [B_const, Brw], [rtb])
                            P.dve(lambda e, rt=rt: e.tensor_copy(lg[:], rt[:, 0:NEXP]), reads=[rtb], writes=[Blg])
                            P.dve(lambda e: e.max(out=t8[:, 0:8], in_=lg[:]), reads=[Blg], writes=[Blg])
                            P.dve(lambda e: e.tensor_scalar(t8[:, 8:9], t8[:, 0:1], -1.0, None, ALU.mult), reads=[Blg], writes=[Blg])
                            P.act(lambda e: e.activation(out=ex[:], in_=lg[:], func=AF.Exp, bias=t8[:, 8:9]), reads=[Blg], writes=[Blg])
                            P.dve(lambda e: e.tensor_scalar(mk4[:], lg[:], t8[:, 3:4], None, ALU.is_ge), reads=[Blg], writes=[Blg])
                            P.dve(lambda e: e.tensor_tensor(ex[:], ex[:], mk4[:], ALU.mult), reads=[Blg], writes=[Blg])
                            P.dve(lambda e: e.tensor_reduce(out=t8[:, 9:10], in_=ex[:], axis=AX.X, op=ALU.add), reads=[Blg], writes=[Blg])
                            P.dve(lambda e: e.reciprocal(t8[:, 10:11], t8[:, 9:10]), reads=[Blg], writes=[Blg])
                            P.dve(lambda e: e.tensor_scalar(ex[:], ex[:], t8[:, 10:11], None, ALU.mult), reads=[Blg], writes=[Blg])
                            pt, pb = PSA.get()
                            mm(pt[0:NEXP, 0:128], ex[:], ident[:], True, True, [Blg, B_const], [pb])
                            i = t * 4 + jj
                            P.dve(lambda e, pt=pt, i=i: e.tensor_copy(gwT[:, i * 128:(i + 1) * 128], pt[0:NEXP, 0:128]), reads=[pb], writes=[BgwT[t]])
                    P.barrier()
                et_r = Ring([(sb(es, "et%d" % i, [128, 512], F32), Buf()) for i in range(6)])
                for ei in range(NEXP):
                    for hf in range(2):
                        load_w("pool", w1[:, :, hf * D:(hf + 1) * D], w1_d[l, ei, :, hf * D:(hf + 1) * D].rearrange("(kc p) n -> p kc n", p=128), Bw1[hf])
                    load_w("sp", b1[:], b1T_d[l, ei], Bb1)
                    P.dve(lambda e: e.tensor_scalar(b1p[:], b1[:, 8:16], 1.0, None, ALU.add), reads=[Bb1], writes=[Bb1])
                    load_w("pool", w2[:], w2_d[l, ei].rearrange("(kc p) n -> p kc n", p=128), Bw2)
                    for t in range(NCH):
                        pt, pb = PSA.get()
                        mm(pt[:, :], ident[0:NEXP, ei:ei + 1].to_broadcast([NEXP, 128]), gwT[:, tcs(t)], True, True, [B_const, BgwT[t]], [pb])
                        P.act(lambda e, pt=pt: e.activation(out=gwb[:], in_=pt[:, :], func=AF.Identity), reads=[pb], writes=[Bgwb])
                        for fc in range(8):
                            bw = Bw1[fc // 4]
                            pg, pgb = PSA.get()
                            for kc in range(8):
                                mm(pg[:, :], w1[:, kc, fc * 256:(fc + 1) * 256:2], hT[:, kc, tcs(t)], kc == 0, kc == 7, [bw, B_hT[kc][t]], [pgb])
                            pu, pub = PSA.get()
                            for kc in range(8):
                                mm(pu[:, :], w1[:, kc, fc * 256 + 1:(fc + 1) * 256:2], hT[:, kc, tcs(t)], kc == 0, kc == 7, [bw, B_hT[kc][t]], [pub])
                            gt, gtb = et_r.get()
                            sg, sgb = et_r.get()
                            ut, utb = et_r.get()
                            P.dve(lambda e, gt=gt, pg=pg, fc=fc: e.tensor_scalar(gt[:], pg[:, :], b1[:, fc:fc + 1], 7.0, ALU.add, ALU.min),
                                  reads=[pgb, Bb1], writes=[gtb])
                            P.act(lambda e, sg=sg, gt=gt: e.activation(out=sg[:], in_=gt[:], func=AF.Sigmoid, scale=1.702), reads=[gtb], writes=[sgb])
                            P.act(lambda e, ut=ut, pu=pu, fc=fc: e.activation(out=ut[:], in_=pu[:, :], func=AF.Identity, bias=b1p[:, fc:fc + 1]),
                                  reads=[pub, Bb1], writes=[utb])
                            P.dve(lambda e, ut=ut: e.tensor_scalar(ut[:], ut[:], 8.0, -6.0, ALU.min, ALU.max), reads=[utb], writes=[utb])
                            P.pool(lambda e, gt=gt, sg=sg: e.tensor_tensor(gt[:], gt[:], sg[:], ALU.mult), reads=[gtb, sgb], writes=[gtb])
                            P.dve(lambda e, gt=gt, ut=ut: e.tensor_tensor(gt[:], gt[:], ut[:], ALU.mult), reads=[gtb, utb], writes=[gtb])
                            P.dve(lambda e, gt=gt, fc=fc: e.tensor_tensor(gl[:, fc, :], gt[:], gwb[:], ALU.mult), reads=[gtb, Bgwb], writes=[Bgl[fc]])
                        for dc in range(8):
                            py, pyb = PSA.get()
                            for fc in range(8):
                                mm(py[:, :], w2[:, fc, dc * 128:(dc + 1) * 128], gl[:, fc, :], fc == 0, fc == 7, [Bw2, Bgl[fc]], [pyb])
                            P.dve(lambda e, py=py, dc=dc, t=t: e.scalar_tensor_tensor(out=xT[:, dc, tcs(t)], in0=py[:, :], scalar=g2c[:, dc:dc + 1],
                                                                                    in1=xT[:, dc, tcs(t)], op0=ALU.mult, op1=ALU.add),
                                  reads=[pyb, B_mod, B_xT[dc][t]], writes=[B_xT[dc][t]])
                for t in range(NCH):
                    for dc in range(8):
                        py, pyb = PSA.get()
                        mm(py[:, :], b2t[:, dc * 128:(dc + 1) * 128], gwT[:, tcs(t)], True, True, [Brw, BgwT[t]], [pyb])
                        P.dve(lambda e, py=py, dc=dc, t=t: e.scalar_tensor_tensor(out=xT[:, dc, tcs(t)], in0=py[:, :], scalar=g2c[:, dc:dc + 1],
                                                                                in1=xT[:, dc, tcs(t)], op0=ALU.mult, op1=ALU.add),
                              reads=[pyb, B_mod, B_xT[dc][t]], writes=[B_xT[dc][t]])
                P.barrier()

            if dbg and l == 0 and b == 0:
                P.dma("sp", lambda e: e.dma_start(out=dbg_out["x2"][:, :, :], in_=xT[:]),
                      reads=[bb for r in B_xT for bb in r], writes=[B_out])
                P.barrier()
        with ExitStack() as es:
            sq_r = Ring([(sb(es, "fsq%d" % i, [128, 512], F32), Buf()) for i in range(3)])
            rs_t, rs_b = sb(es, "frs", [128, 512], F32), Buf()
            fin = sb(es, "fin", [128, 8, 512], F32)
            Bfin = [Buf() for _ in range(8)]
            zero8 = sb(es, "zero8", [128, 8], F32)
            Bz = Buf()
            P.dve(lambda e: e.memset(zero8[:], 0.0), writes=[B_mod])
            orow_r = Ring([(sb(es, "orow%d" % i, [128, D], F32), Buf()) for i in range(2)])
            for t in range(NCH):
                norm_mod("nf", fg, zero8, t, lambda c: fin[:, c, :], lambda c: [Bfin[c]], (sq_r, rs_t, rs_b))
                for jj in range(4):
                    orow, orb = orow_r.get()
                    for g in range(2):
                        pt, pb = PSA.get()
                        for k in range(4):
                            c = g * 4 + k
                            mm(pt[:, k * 128:(k + 1) * 128], fin[:, c, jj * 128:(jj + 1) * 128], ident[:], True, True, [Bfin[c], B_const], [pb])
                        P.act(lambda e, orow=orow, pt=pt, g=g: e.activation(out=orow[:, g * 512:(g + 1) * 512], in_=pt[:, :], func=AF.Identity), reads=[pb], writes=[orb])
                    i = t * 4 + jj
                    P.dma("sp", lambda e, orow=orow, i=i: e.dma_start(out=out_d[b, i * 128:(i + 1) * 128, :], in_=orow[:]), reads=[orb], writes=[B_out])
            P.barrier()

    P.emit(final_bufs=[B_out])
    root.close()
    return nc, P


def _rot_cols(c0, n_rot):
    h = n_rot // 2
    return list(range(c0 + h, c0 + n_rot)) + list(range(c0, c0 + h))


def _prep_inputs(inp, b0, nseq):
    f = lambda a: np.ascontiguousarray(a, dtype=np.float32)
    L = inp["w_in"].shape[0]

    def colT(v, nch):
        v = np.asarray(v)
        return f(v.reshape(v.shape[:-1] + (nch, 128)).swapaxes(-1, -2))

    m = {}
    m["x"] = f(inp["x"][b0:b0 + nseq])
    m["cT"] = f(np.asarray(inp["c"])[b0:b0 + nseq].reshape(nseq, 8, 128).transpose(2, 1, 0))
    m["pos"] = np.ascontiguousarray(np.asarray(inp["positions"])[b0:b0 + nseq], dtype=np.int32)
    m["w_ada"] = f(inp["w_ada"])
    m["b_adaT"] = colT(inp["b_ada"], 48)
    m["n1g"] = colT(inp["norm1_g"], 8)
    m["n2g"] = colT(inp["norm2_g"], 8)
    m["fg"] = colT(inp["final_g"], 8)
    w_in = np.asarray(inp["w_in"])
    m["w_in"] = f(w_in)
    rot = _rot_cols(384, 32)
    for h in range(8):
        rot += _rot_cols(1952 + h * 64, 16)
    rot += _rot_cols(2464, 16)
    for h in range(8):
        rot += _rot_cols(2592 + h * 64, 16)
    rot += _rot_cols(3104, 16)
    rot += rot[:16]
    m["w_in_rot"] = f(w_in[:, :, rot])
    m["b_gateT"] = colT(inp["b_gate"], 24)
    m["qng"] = colT(inp["mla_q_norm"], 2)
    w_uq = np.asarray(inp["mla_w_uq"])
    m["w_uq"] = f(w_uq)
    rq = []
    for h in range(8):
        rq += _rot_cols(h * 96, 32)
    m["w_uq_rot"] = f(w_uq[:, :, rq])
    m["kvng"] = colT(inp["mla_kv_norm"], 1)
    m["w_ukv"] = f(inp["mla_w_ukv"])
    cw = np.asarray(inp["conv_w"])
    m["conv_wT"] = f(cw.reshape(L, 3, 4, 128).transpose(0, 3, 2, 1))
    m["w_branch"] = f(inp["w_branch"])
    m["w_out"] = f(inp["w_out"])
    m["router_w"] = f(inp["router_w"])
    m["router_b"] = f(np.asarray(inp["router_b"]).reshape(L, 1, NEXP))
    m["exp_w1"] = f(inp["exp_w1"])
    b1 = np.asarray(inp["exp_b1"])
    b1g = b1[:, :, 0::2].reshape(L, NEXP, 8, 128).swapaxes(-1, -2)
    b1u = b1[:, :, 1::2].reshape(L, NEXP, 8, 128).swapaxes(-1, -2)
    m["exp_b1T"] = f(np.concatenate([b1g, b1u], axis=-1))
    m["exp_w2"] = f(inp["exp_w2"])
    m["exp_b2"] = f(inp["exp_b2"])
    return m


_CONST = None


def _consts():
    global _CONST
    if _CONST is None:
        c = {}
        c["ident"] = np.eye(128, dtype=np.float32)
        sel = np.zeros((NEXP, NEXP, 128), np.float32)
        for e in range(NEXP):
            sel[e, e, :] = 1.0
        c["sel"] = sel
        rc = np.zeros((32, 4), np.float32)
        i16 = np.arange(16, dtype=np.float32)
        invf32 = np.exp(-math.log(500000.0) * i16 * (2.0 / 32)).astype(np.float32)
        i8 = np.arange(8, dtype=np.float32)
        invf16 = np.exp(-math.log(500000.0) * i8 * (2.0 / 16)).astype(np.float32)
        rc[:, 0] = np.concatenate([invf32, invf32])
        rc[:, 1] = np.concatenate([-np.ones(16), np.ones(16)])
        rc[0:16, 2] = np.concatenate([invf16, invf16])
        rc[0:16, 3] = np.concatenate([-np.ones(8), np.ones(8)])
        c["ropec"] = rc
        k = np.arange(128)
        c["tri"] = (k[:, None] <= k[None, :]).astype(np.float32)
        c["ntri"] = np.where(k[None, :] <= k[:, None], 0.0, -BIG).astype(np.float32)
        _CONST = c
    return _CONST


_NC_CACHE = {}


def kernel(**inputs):
    n_cores = 8
    B = np.asarray(inputs["x"]).shape[0]
    nseq = B // n_cores
    if nseq not in _NC_CACHE:
        _NC_CACHE[nseq] = build(nseq)[0]
    nc = _NC_CACHE[nseq]
    consts = _consts()
    in_maps = []
    for core in range(n_cores):
        m = _prep_inputs(inputs, core * nseq, nseq)
        m.update(consts)
        in_maps.append(m)
    res = run_bass_kernel_spmd(nc, in_maps, core_ids=list(range(n_cores)))
    out = np.concatenate([np.asarray(r["out"]) for r in res.results], axis=0)
    return out.astype(np.float32)
```

```python
import math
import types
import numpy as np
from contextlib import ExitStack
import concourse.bass as bass
import concourse.mybir as mybir
from concourse.bass_utils import run_bass_kernel_spmd

F32 = mybir.dt.float32
BF16 = mybir.dt.bfloat16
I32 = mybir.dt.int32
AF = mybir.ActivationFunctionType
ALU = mybir.AluOpType
AX = mybir.AxisListType

S = 2048
D = 1024
NT = 16
NCH = 4
IN_COLS = 6248
NEXP = 32
BIG = 1.0e30


class Buf:
    __slots__ = ("name", "w", "r", "excl")

    def __init__(self, name="", excl=False):
        self.name = name
        self.w = None
        self.r = []
        self.excl = excl


class Prog:
    def __init__(self, nc, n_dma_sems=40):
        self.nc = nc
        self.ops = []
        self.n_dma_sems = n_dma_sems

    @staticmethod
    def _freeze(fn, depth=0):
        if not isinstance(fn, types.FunctionType) or fn.__closure__ is None or depth > 3:
            return fn
        cells = []
        for c in fn.__closure__:
            try:
                v = c.cell_contents
            except ValueError:
                cells.append(c)
                continue
            if isinstance(v, types.FunctionType):
                v = Prog._freeze(v, depth + 1)
            cells.append(types.CellType(v))
        g = types.FunctionType(fn.__code__, fn.__globals__, fn.__name__, fn.__defaults__, tuple(cells))
        g.__kwdefaults__ = fn.__kwdefaults__
        return g

    def add(self, eng, fn, reads=(), writes=(), dma=False):
        self.ops.append((eng, Prog._freeze(fn), tuple(reads), tuple(writes), dma))

    def pe(self, fn, reads=(), writes=()):
        self.add("pe", fn, reads, writes)

    def act(self, fn, reads=(), writes=()):
        self.add("act", fn, reads, writes)

    def dve(self, fn, reads=(), writes=()):
        self.add("dve", fn, reads, writes)

    def pool(self, fn, reads=(), writes=()):
        self.add("pool", fn, reads, writes)

    def dma(self, q, fn, reads=(), writes=()):
        self.add(q, fn, reads, writes, dma=True)

    def barrier(self):
        self.ops.append(("BAR", None, (), (), False))

    def emit(self, final_bufs=()):
        nc = self.nc
        ops = self.ops
        n = len(ops)
        deps = [None] * n
        signal = [False] * n
        last_on = {}
        dmas_since = []
        bar_deps = {}
        for i, (eng, fn, reads, writes, dma) in enumerate(ops):
            if eng == "BAR":
                bd = set(last_on.values()) | set(dmas_since)
                for j in bd:
                    signal[j] = True
                bar_deps[i] = bd
                dmas_since = []
                continue
            d = set()
            for b in reads:
                if b.w is not None:
                    d.add(b.w)
                if b.excl:
                    d.update(j for j in b.r if ops[j][0] != eng)
            for b in writes:
                if b.w is not None:
                    d.add(b.w)
                d.update(b.r)
            d.discard(i)
            keep = set()
            for j in d:
                jeng, _, jr, jw, jdma = ops[j]
                if not dma and not jdma and jeng == eng:
                    if eng == "pe":
                        continue
                keep.add(j)
            deps[i] = keep
            for j in keep:
                signal[j] = True
            for b in reads:
                if not dma:
                    b.r = [j for j in b.r if ops[j][4] or ops[j][0] != eng]
                b.r.append(i)
            for b in writes:
                b.w = i
                b.r = []
            if dma:
                dmas_since.append(i)
            else:
                last_on[eng] = i
        final_deps = set()
        for b in final_bufs:
            if b.w is not None:
                final_deps.add(b.w)
                signal[b.w] = True

        with ExitStack() as es:
            SEM_LIMIT = 2000
            tot = {e: 0 for e in ("pe", "act", "dve", "pool")}
            for i, (eng, fn, reads, writes, dma) in enumerate(ops):
                if eng in tot and not dma and signal[i]:
                    tot[eng] += 1
            csem = {e: [es.enter_context(nc.semaphore("cs_%s%d" % (e, k))) for k in range(tot[e] // SEM_LIMIT + 1)]
                    for e in tot}
            dsem = [es.enter_context(nc.semaphore("ds%d" % k)) for k in range(self.n_dma_sems)]
            ccount = {e: 0 for e in csem}
            dcount = [0] * self.n_dma_sems
            token = [None] * n
            prevtok = [None] * n
            nd = 0
            nsw = 0
            nhw = 0
            half = self.n_dma_sems // 2
            for i, (eng, fn, reads, writes, dma) in enumerate(ops):
                if eng == "BAR":
                    continue
                if dma:
                    if eng == "pool":
                        k = nsw % half
                        nsw += 1
                    else:
                        k = half + nhw % (self.n_dma_sems - half)
                        nhw += 1
                    nd += 1
                    if dcount[k] > 0:
                        prevtok[i] = (dsem[k], dcount[k])
                    dcount[k] += 16
                    token[i] = (dsem[k], dcount[k])
                elif signal[i]:
                    c = ccount[eng]
                    ccount[eng] += 1
                    token[i] = (csem[eng][c // SEM_LIMIT], c % SEM_LIMIT + 1)
            self.stats = {"n_ops": n, "n_dma": nd, "ccount": dict(ccount)}

            def do_waits(e, waited, toks):
                best = {}
                for (s, v) in toks:
                    if best.get(s.num, (None, 0))[1] < v:
                        best[s.num] = (s, v)
                for num, (s, v) in best.items():
                    if waited.get(num, 0) < v:
                        e.wait_ge(s, v)
                        waited[num] = v

            def emit_engine(ename, e):
                waited = {}
                for i, (eng, fn, reads, writes, dma) in enumerate(ops):
                    if eng == "BAR":
                        do_waits(e, waited, [token[j] for j in bar_deps[i]])
                        continue
                    if eng != ename:
                        continue
                    toks = [token[j] for j in deps[i]]
                    if prevtok[i] is not None:
                        toks.append(prevtok[i])
                    do_waits(e, waited, toks)
                    ins = fn(e)
                    if token[i] is not None:
                        s, v = token[i]
                        ins.then_inc(s, 16 if dma else 1)
                if ename == "sp":
                    do_waits(e, waited, [token[j] for j in final_deps])

            with nc.Block() as block:
                @block.tensor
                def _(e):
                    emit_engine("pe", e)

                @block.scalar
                def _(e):
                    emit_engine("act", e)

                @block.vector
                def _(e):
                    emit_engine("dve", e)

                @block.gpsimd
                def _(e):
                    emit_engine("pool", e)

                @block.sync
                def _(e):
                    emit_engine("sp", e)


class Ring:
    def __init__(self, items):
        self.items = items
        self.i = 0

    def get(self):
        it = self.items[self.i % len(self.items)]
        self.i += 1
        return it


def build(nseq, depth=2, phases=None, dbg=False, nlayers=None):
    nc = bass.Bass("TRN2", target_bir_lowering=False)
    P = Prog(nc)
    root = ExitStack()
    ALL_PHASES = ("prologue", "norm1", "mla", "conv", "d1", "d2", "merge", "moe", "d1_score", "d1_bis", "d1_mask", "d1p_q", "d1p_k", "d1p_w")
    run_phases = set(ALL_PHASES if phases is None else phases)

    def phase(name):
        if name in run_phases:
            with ExitStack() as es_:
                yield es_

    def din(name, shape, dt=F32):
        return nc.dram_tensor(name, list(shape), dt, kind="ExternalInput").ap()

    def dscr(name, shape, dt=BF16):
        if dbg:
            return nc.dram_tensor(name, list(shape), dt, kind="ExternalOutput").ap()
        return nc.dram_tensor(name, list(shape), dt).ap()

    _cnt = [0]

    def sb(es, name, shape, dt=F32):
        _cnt[0] += 1
        return es.enter_context(nc.sbuf_tensor("s%d_%s" % (_cnt[0], name), list(shape), dt))

    x_d = din("x", [nseq, S, D])
    cT_d = din("cT", [128, 8, nseq])
    pos_d = din("pos", [nseq, S], I32)
    w_ada_d = din("w_ada", [depth, D, 6 * D])
    b_adaT_d = din("b_adaT", [depth, 128, 48])
    n1g_d = din("n1g", [depth, 128, 8])
    n2g_d = din("n2g", [depth, 128, 8])
    fg_d = din("fg", [128, 8])
    w_in_d = din("w_in", [depth, D, IN_COLS])
    w_inr_d = din("w_in_rot", [depth, D, 336])
    b_gateT_d = din("b_gateT", [depth, 128, 24])
    qng_d = din("qng", [depth, 128, 2])
    w_uq_d = din("w_uq", [depth, 256, 768])
    w_uqr_d = din("w_uq_rot", [depth, 256, 256])
    kvng_d = din("kvng", [depth, 128, 1])
    w_ukv_d = din("w_ukv", [depth, 128, 1024])
    conv_wT_d = din("conv_wT", [depth, 128, 4, 3])
    w_br_d = din("w_branch", [depth, 3, 512, D])
    w_out_d = din("w_out", [depth, D, D])
    rw_d = din("router_w", [depth, D, NEXP])
    rb_d = din("router_b", [depth, 1, NEXP])
    w1_d = din("exp_w1", [depth, NEXP, D, 2 * D])
    b1T_d = din("exp_b1T", [depth, NEXP, 128, 16])
    w2_d = din("exp_w2", [depth, NEXP, D, D])
    b2_d = din("exp_b2", [depth, NEXP, D])
    ident_d = din("ident", [128, 128])
    sel_d = din("sel", [NEXP, NEXP, 128])
    ropec_d = din("ropec", [32, 4])
    tri_d = din("tri", [128, 128])
    ntri_d = din("ntri", [128, 128])
    out_d = nc.dram_tensor("out", [nseq, S, D], F32, kind="ExternalOutput").ap()
    oa_s = dscr("oa_s", [8, 64, S])
    ob_s = dscr("ob_s", [4, 128, S])
    oc_s = dscr("oc_s", [8, 64, S])
    mask_s = dscr("mask_s", [NT, 128, S])
    dbg_out = {}
    if dbg:
        dbg_out["hT"] = nc.dram_tensor("dbg_hT", [128, 8, S], BF16, kind="ExternalOutput").ap()
        dbg_out["x1"] = nc.dram_tensor("dbg_x1", [128, 8, S], F32, kind="ExternalOutput").ap()
        dbg_out["x2"] = nc.dram_tensor("dbg_x2", [128, 8, S], F32, kind="ExternalOutput").ap()

    xT = sb(root, "xT", [128, 8, S], F32)
    hT = sb(root, "hT", [128, 8, S], BF16)
    ident = sb(root, "ident", [128, 128], F32)
    identb = sb(root, "identb", [128, 128], BF16)
    ones_f = sb(root, "ones_f", [128, 128], F32)
    ones_b = sb(root, "ones_b", [128, 128], BF16)
    tri_b = sb(root, "tri_b", [128, 128], BF16)
    ntri = sb(root, "ntri", [128, 128], F32)
    ropec = sb(root, "ropec", [32, 4], F32)
    cos32 = sb(root, "cos32", [32, S], BF16)
    sin32 = sb(root, "sin32", [32, S], BF16)
    cos16 = sb(root, "cos16", [32, S], BF16)
    sin16 = sb(root, "sin16", [32, S], BF16)
    modc = {}
    for l in range(depth):
        for b in range(nseq):
            for nm in ("gs1", "sh1", "g1", "gs2", "sh2", "g2"):
                modc[(l, b, nm)] = sb(root, "m_%s_%d_%d" % (nm, l, b), [128, 8], F32)
    fg = sb(root, "fg", [128, 8], F32)
    B_xT = [[Buf("xT%d_%d" % (c, t)) for t in range(NCH)] for c in range(8)]
    B_hT = [[Buf("hT%d_%d" % (c, t)) for t in range(NCH)] for c in range(8)]
    B_const = Buf("const")
    B_rope = Buf("rope")
    B_mod = Buf("mod")
    B_oa, B_ob, B_oc = Buf("oa"), Buf("ob"), Buf("oc")
    B_out = Buf("out")
    B_mask = Buf("mask")

    ps_t = [root.enter_context(nc.psum_tensor("ps%d" % i, [128, 512], F32)) for i in range(8)]
    ps_b = [Buf("ps%d" % i, excl=True) for i in range(8)]
    PSA = Ring([(ps_t[i], ps_b[i]) for i in range(0, 4)])
    PSB = Ring([(ps_t[i], ps_b[i]) for i in range(4, 6)])
    PSC = Ring([(ps_t[i], ps_b[i]) for i in range(6, 8)])

    def tcs(t):
        return slice(t * 512, (t + 1) * 512)

    def mm(out, lhsT, rhs, start, stop, reads, writes):
        P.pe(lambda e: e.matmul(out, lhsT, rhs, start=start, stop=stop), reads=reads, writes=writes)

    P.dma("sp", lambda e: e.dma_start(out=ident[:], in_=ident_d[:, :]), writes=[B_const])
    P.dma("sp", lambda e: e.dma_start(out=ntri[:], in_=ntri_d[:, :]), writes=[B_const])
    P.dma("sp", lambda e: e.dma_start(out=ropec[:], in_=ropec_d[:, :]), writes=[B_const])
    P.dma("sp", lambda e: e.dma_start(out=fg[:], in_=fg_d[:, :]), writes=[B_const])
    P.dma("pool", lambda e: e.dma_start(out=tri_b[:], in_=tri_d[:, :]), writes=[B_const])
    P.dma("pool", lambda e: e.dma_start(out=identb[:], in_=ident_d[:, :]), writes=[B_const])
    P.dve(lambda e: e.memset(ones_f[:], 1.0), writes=[B_const])
    P.dve(lambda e: e.memset(ones_b[:], 1.0), writes=[B_const])

    for es in phase("prologue"):
        cact = sb(es, "cact", [128, 8, nseq], F32)
        modT = sb(es, "modT", [128, 48, nseq], F32)
        badaT = sb(es, "badaT", [128, 48], F32)
        ngt = sb(es, "ngt", [128, 8], F32)
        wa = [sb(es, "wa%d" % i, [128, 8, 512], F32) for i in range(2)]
        wab = [Buf("wa%d" % i) for i in range(2)]
        B_c, B_modT, B_bada, B_ng = Buf(), Buf(), Buf(), Buf()
        P.dma("sp", lambda e: e.dma_start(out=cact[:], in_=cT_d[:, :, :]), writes=[B_c])
        P.act(lambda e: e.activation(out=cact[:], in_=cact[:], func=AF.Silu), reads=[B_c], writes=[B_c])
        for l in range(depth):
            P.dma("sp", lambda e, l=l: e.dma_start(out=badaT[:], in_=b_adaT_d[l]), writes=[B_bada])
            for g in range(12):
                w_t, w_b = wa[g % 2], wab[g % 2]
                src = w_ada_d[l, :, g * 512:(g + 1) * 512].rearrange("(kc p) n -> p kc n", p=128)
                P.dma("sp", lambda e, w_t=w_t, src=src: e.dma_start(out=w_t[:], in_=src), writes=[w_b])
                pt, pb = PSA.get()
                for j in range(4):
                    for kc in range(8):
                        mm(pt[:, j * nseq:(j + 1) * nseq], w_t[:, kc, j * 128:(j + 1) * 128], cact[:, kc, :],
                           kc == 0, kc == 7, [w_b, B_c], [pb])
                P.dve(lambda e, pt=pt, g=g: e.tensor_copy(
                    modT[:, g * 4:(g + 1) * 4, :], pt[:, 0:4 * nseq].rearrange("p (j b) -> p j b", j=4)),
                    reads=[pb], writes=[B_modT])
            for b in range(nseq):
                P.dve(lambda e, b=b: e.tensor_tensor(modT[:, :, b], modT[:, :, b], badaT[:], ALU.add),
                      reads=[B_modT, B_bada], writes=[B_modT])
            for (nm_gs, nm_sh, nm_g, base, ng_d) in (("gs1", "sh1", "g1", 0, n1g_d), ("gs2", "sh2", "g2", 24, n2g_d)):
                P.dma("sp", lambda e, ng_d=ng_d, l=l: e.dma_start(out=ngt[:], in_=ng_d[l]), writes=[B_ng])
                for b in range(nseq):
                    gs, sh, gg = modc[(l, b, nm_gs)], modc[(l, b, nm_sh)], modc[(l, b, nm_g)]
                    P.dve(lambda e, sh=sh, b=b, base=base: e.tensor_copy(sh[:], modT[:, base:base + 8, b]),
                          reads=[B_modT], writes=[B_mod])
                    P.dve(lambda e, gs=gs, b=b, base=base: e.scalar_tensor_tensor(
                        out=gs[:], in0=modT[:, base + 8:base + 16, b], scalar=1.0, in1=ngt[:],
                        op0=ALU.add, op1=ALU.mult), reads=[B_modT, B_ng], writes=[B_mod])
                    P.dve(lambda e, gg=gg, b=b, base=base: e.tensor_copy(gg[:], modT[:, base + 16:base + 24, b]),
                          reads=[B_modT], writes=[B_mod])
        P.barrier()

    def norm_mod(es_name, gs, sh, t, dst_fn, dst_bufs_fn, tmp, extra=None):
        sq_r, rs_t, rs_b = tmp
        pt, pb = PSA.get()
        for c in range(8):
            sq, sqb = sq_r.get()
            P.act(lambda e, sq=sq, c=c: e.activation(out=sq[:], in_=xT[:, c, tcs(t)], func=AF.Square),
                  reads=[B_xT[c][t]], writes=[sqb])
            mm(pt[:, :], ones_f[:], sq[:], c == 0, c == 7, [sqb, B_const], [pb])
        P.act(lambda e, pt=pt: e.activation(out=rs_t[:], in_=pt[:, :], func=AF.Sqrt, scale=1.0 / D, bias=1e-6),
              reads=[pb], writes=[rs_b])
        P.dve(lambda e: e.reciprocal(rs_t[:], rs_t[:]), reads=[rs_b], writes=[rs_b])
        for c in range(8):
            sq, sqb = sq_r.get()
            P.dve(lambda e, sq=sq, c=c: e.tensor_tensor(sq[:], xT[:, c, tcs(t)], rs_t[:], ALU.mult),
                  reads=[B_xT[c][t], rs_b], writes=[sqb])
            P.act(lambda e, sq=sq, c=c: e.activation(out=sq[:], in_=sq[:], func=AF.Identity,
                                                     scale=gs[:, c:c + 1], bias=sh[:, c:c + 1]),
                  reads=[sqb, B_mod], writes=[sqb])
            if extra is not None:
                extra(c, sq, sqb)
            P.dve(lambda e, sq=sq, c=c: e.tensor_copy(dst_fn(c), sq[:]), reads=[sqb], writes=dst_bufs_fn(c))

    def load_w(q, dst, src, buf):
        P.dma(q, lambda e: e.dma_start(out=dst, in_=src), writes=[buf])

    def rope_tables(es, b):
        posi = sb(es, "posi", [32, S], I32)
        ang = sb(es, "ang", [32, S], F32)
        t1 = sb(es, "rt1", [32, S], F32)
        t2 = sb(es, "rt2", [32, S], F32)
        ki = sb(es, "rki", [32, S], I32)
        Bp, Ba, B1, B2, Bk = Buf(), Buf(), Buf(), Buf(), Buf()
        P.dma("sp", lambda e: e.dma_start(out=posi[:], in_=pos_d[b, :].partition_broadcast(32)), writes=[Bp])
        C1 = 6.28125
        C2 = 2.0 * math.pi - C1
        for (n, icol, scol, cos_t, sin_t) in ((32, 0, 1, cos32, sin32), (32, 2, 3, cos16, sin16)):
            P.dve(lambda e, n=n: e.tensor_copy(ang[0:n, :], posi[0:n, :]), reads=[Bp], writes=[Ba])
            P.dve(lambda e, n=n, icol=icol: e.tensor_scalar(ang[0:n, :], ang[0:n, :], ropec[0:n, icol:icol + 1], None,
                                                            ALU.mult), reads=[Ba, B_const], writes=[Ba])
            for (shift, dst, signed) in ((0.5 * math.pi, cos_t, False), (0.0, sin_t, True)):
                P.dve(lambda e, n=n, shift=shift: e.tensor_scalar(t1[0:n, :], ang[0:n, :], shift, None, ALU.add),
                      reads=[Ba], writes=[B1])
                P.dve(lambda e, n=n: e.tensor_scalar(t2[0:n, :], t1[0:n, :], 1.0 / (2.0 * math.pi), None, ALU.mult),
                      reads=[B1], writes=[B2])
                P.dve(lambda e, n=n: e.tensor_copy(ki[0:n, :], t2[0:n, :]), reads=[B2], writes=[Bk])
                P.dve(lambda e, n=n: e.tensor_copy(t2[0:n, :], ki[0:n, :]), reads=[Bk], writes=[B2])
                P.dve(lambda e, n=n: e.scalar_tensor_tensor(out=t1[0:n, :], in0=t2[0:n, :], scalar=-C1, in1=t1[0:n, :],
                                                            op0=ALU.mult, op1=ALU.add), reads=[B1, B2], writes=[B1])
                P.dve(lambda e, n=n: e.scalar_tensor_tensor(out=t1[0:n, :], in0=t2[0:n, :], scalar=-C2, in1=t1[0:n, :],
                                                            op0=ALU.mult, op1=ALU.add), reads=[B1, B2], writes=[B1])
                P.dve(lambda e, n=n: e.tensor_scalar(t2[0:n, :], t1[0:n, :], math.pi, -2.0 * math.pi, ALU.is_gt, ALU.mult),
                      reads=[B1], writes=[B2])
                P.dve(lambda e, n=n: e.tensor_tensor(t1[0:n, :], t1[0:n, :], t2[0:n, :], ALU.add),
                      reads=[B1, B2], writes=[B1])
                P.dve(lambda e, n=n: e.tensor_scalar(t1[0:n, :], t1[0:n, :], -math.pi, math.pi, ALU.max, ALU.min),
                      reads=[B1], writes=[B1])
                P.act(lambda e, n=n, dst=dst: e.activation(out=dst[0:n, :], in_=t1[0:n, :], func=AF.Sin),
                      reads=[B1], writes=[B_rope])
                if signed:
                    P.dve(lambda e, n=n, dst=dst, scol=scol: e.tensor_scalar(
                        dst[0:n, :], dst[0:n, :], ropec[0:n, scol:scol + 1], None, ALU.mult),
                        reads=[B_rope, B_const], writes=[B_rope])

    for b in range(nseq):
        with ExitStack() as es:
            xin = [sb(es, "xin%d" % i, [128, D], F32) for i in range(2)]
            xinb = [Buf() for _ in range(2)]
            for i in range(NT):
                xi, xb = xin[i % 2], xinb[i % 2]
                P.dma("sp", lambda e, xi=xi, i=i: e.dma_start(out=xi[:], in_=x_d[b, i * 128:(i + 1) * 128, :]),
                      writes=[xb])
                for g in range(2):
                    pt, pb = PSA.get()
                    for j in range(4):
                        c = g * 4 + j
                        mm(pt[:, j * 128:(j + 1) * 128], xi[:, c * 128:(c + 1) * 128], ident[:], True, True,
                           [xb, B_const], [pb])
                    P.dve(lambda e, pt=pt, g=g, i=i: e.tensor_copy(
                        xT[:, g * 4:(g + 1) * 4, i * 128:(i + 1) * 128],
                        pt[:, :].rearrange("p (c t) -> p c t", c=4)),
                        reads=[pb], writes=[B_xT[c2][i // 4] for c2 in range(g * 4, g * 4 + 4)])
            rope_tables(es, b)
            P.barrier()

        for l in range(depth if nlayers is None else nlayers):
            gs1, sh1, g1c = modc[(l, b, "gs1")], modc[(l, b, "sh1")], modc[(l, b, "g1")]
            gs2, sh2, g2c = modc[(l, b, "gs2")], modc[(l, b, "sh2")], modc[(l, b, "g2")]
            allh = lambda t: [B_hT[c][t] for c in range(8)]

            def wsrc(c0, n, l=l):
                return w_in_d[l, :, c0:c0 + n].rearrange("(kc p) n -> p kc n", p=128)

            def wrsrc(c0, n, l=l):
                return w_inr_d[l, :, c0:c0 + n].rearrange("(kc p) n -> p kc n", p=128)

            def proj(W, wb, c0, M, t, pool=PSA):
                pt, pb = pool.get()
                for kc in range(8):
                    mm(pt[0:M, :], W[:, kc, c0:c0 + M], hT[:, kc, tcs(t)], kc == 0, kc == 7,
                       [wb, B_hT[kc][t]], [pb])
                return pt, pb

            def rope_comb(dst, dstb, A, Ab, Bm, Bb, n, cos_t, sin_t, t, tmp_r):
                t1, t1b = tmp_r.get()
                t2, t2b = tmp_r.get()
                P.dve(lambda e: e.tensor_tensor(t1[0:n, :], A[0:n, :], cos_t[0:n, tcs(t)], ALU.mult),
                      reads=[Ab, B_rope], writes=[t1b])
                P.dve(lambda e: e.tensor_tensor(t2[0:n, :], Bm[0:n, :], sin_t[0:n, tcs(t)], ALU.mult),
                      reads=[Bb, B_rope], writes=[t2b])
                P.dve(lambda e: e.tensor_tensor(dst, t1[0:n, :], t2[0:n, :], ALU.add),
                      reads=[t1b, t2b], writes=[dstb])

            for es in phase("norm1"):
                sq_r = Ring([(sb(es, "sq%d" % i, [128, 512], F32), Buf()) for i in range(3)])
                rs_t, rs_b = sb(es, "rs", [128, 512], F32), Buf()
                for t in range(NCH):
                    norm_mod("n1", gs1, sh1, t, lambda c, t=t: hT[:, c, tcs(t)], lambda c, t=t: [B_hT[c][t]],
                             (sq_r, rs_t, rs_b))
                P.barrier()

            if dbg and l == 0 and b == 0:
                P.dma("sp", lambda e: e.dma_start(out=dbg_out["hT"][:, :, :], in_=hT[:]),
                      reads=[bb for r in B_hT for bb in r], writes=[B_out])
                P.barrier()
            for es in phase("mla"):
                wq, wkv, wkr = sb(es, "wq", [128, 8, 256], BF16), sb(es, "wkv", [128, 8, 160], BF16), sb(es, "wkr", [128, 8, 32], BF16)
                wuq, wuqr = sb(es, "wuq", [128, 2, 768], BF16), sb(es, "wuqr", [128, 2, 256], BF16)
                wukv = sb(es, "wukv", [128, 1024], BF16)
                qng, kvng = sb(es, "qng", [128, 2], F32), sb(es, "kvng", [128, 1], F32)
                Bw = Buf()
                load_w("pool", wq[:], wsrc(0, 256), Bw)
                load_w("pool", wkv[:], wsrc(256, 160), Bw)
                load_w("pool", wkr[:], wrsrc(0, 32), Bw)
                load_w("pool", wuq[:], w_uq_d[l].rearrange("(kc p) n -> p kc n", p=128), Bw)
                load_w("pool", wuqr[:], w_uqr_d[l].rearrange("(kc p) n -> p kc n", p=128), Bw)
                load_w("pool", wukv[:], w_ukv_d[l], Bw)
                load_w("sp", qng[:], qng_d[l], Bw)
                load_w("sp", kvng[:], kvng_d[l], Bw)
                cqT, ckvT, kpeT = sb(es, "cqT", [128, 2, S], BF16), sb(es, "ckvT", [128, S], BF16), sb(es, "kpeT", [32, S], BF16)
                Bcq, Bckv, Bkpe = Buf(), Buf(), Buf()
                raw_r = Ring([(sb(es, "raw%d" % i, [128, 512], F32), Buf()) for i in range(4)])
                tmp_r = Ring([(sb(es, "tmp%d" % i, [128, 512], F32), Buf()) for i in range(4)])
                rstd, rstdb = sb(es, "rstd", [128, 512], F32), Buf()

                def lat_norm(raws, gcol, dst_fn, dstb, nfeat):
                    pt, pb = PSA.get()
                    for i, (rw_t, rw_b) in enumerate(raws):
                        sq, sqb = tmp_r.get()
                        P.act(lambda e, sq=sq, rw_t=rw_t: e.activation(out=sq[:], in_=rw_t[:], func=AF.Square),
                              reads=[rw_b], writes=[sqb])
                        mm(pt[:, :], ones_f[:], sq[:], i == 0, i == len(raws) - 1, [sqb, B_const], [pb])
                    P.act(lambda e, pt=pt: e.activation(out=rstd[:], in_=pt[:, :], func=AF.Sqrt, scale=1.0 / nfeat, bias=1e-6),
                          reads=[pb], writes=[rstdb])
                    P.dve(lambda e: e.reciprocal(rstd[:], rstd[:]), reads=[rstdb], writes=[rstdb])
                    for i, (rw_t, rw_b) in enumerate(raws):
                        P.dve(lambda e, i=i, rw_t=rw_t: e.scalar_tensor_tensor(
                            out=dst_fn(i), in0=rw_t[:], scalar=gcol[:, i:i + 1], in1=rstd[:], op0=ALU.mult, op1=ALU.mult),
                            reads=[rw_b, rstdb, Bw], writes=[dstb])

                for t in range(NCH):
                    raws = []
                    for j in range(2):
                        pt, pb = proj(wq, Bw, j * 128, 128, t)
                        rw_t, rw_b = raw_r.get()
                        P.act(lambda e, rw_t=rw_t, pt=pt: e.activation(out=rw_t[:], in_=pt[:, :], func=AF.Identity),
                              reads=[pb], writes=[rw_b])
                        raws.append((rw_t, rw_b))
                    lat_norm(raws, qng, lambda i, t=t: cqT[:, i, tcs(t)], Bcq, 256.0)
                    pt, pb = proj(wkv, Bw, 0, 128, t)
                    rw_t, rw_b = raw_r.get()
                    P.act(lambda e, rw_t=rw_t, pt=pt: e.activation(out=rw_t[:], in_=pt[:, :], func=AF.Identity),
                          reads=[pb], writes=[rw_b])
                    lat_norm([(rw_t, rw_b)], kvng, lambda i, t=t: ckvT[:, tcs(t)], Bckv, 128.0)
                    A, Ab = proj(wkv, Bw, 128, 32, t)
                    Bm, Bb = proj(wkr, Bw, 0, 32, t)
                    rope_comb(kpeT[0:32, tcs(t)], Bkpe, A, Ab, Bm, Bb, 32, cos32, sin32, t, tmp_r)

                qr = [sb(es, "qr%d" % i, [32, S], BF16) for i in range(2)]
                qn = [sb(es, "qn%d" % i, [64, S], BF16) for i in range(2)]
                kn = [sb(es, "kn%d" % i, [64, S], BF16) for i in range(2)]
                Vh = [sb(es, "Vh%d" % i, [128, NT, 64], BF16) for i in range(2)]
                hb = [[Buf() for _ in range(4)] for _ in range(2)]
                PT_r = Ring([(sb(es, "PT%d" % i, [128, 512], BF16), Buf()) for i in range(3)])
                rD, rDb = sb(es, "rD", [64, 512], F32), Buf()
                o_r = Ring([(sb(es, "o%d" % i, [64, 512], BF16), Buf()) for i in range(2)])
                sc_a = 96.0 ** -0.5
                for h in range(8):
                    s2 = h % 2
                    Bqr, Bqn, Bkn, BV = hb[s2]
                    for t in range(NCH):
                        def cproj(W, c0, M, t=t):
                            pt, pb = PSA.get()
                            for kc in range(2):
                                mm(pt[0:M, :], W[:, kc, c0:c0 + M], cqT[:, kc, tcs(t)], kc == 0, kc == 1, [Bw, Bcq], [pb])
                            return pt, pb
                        A, Ab = cproj(wuq, h * 96, 32)
                        Bm, Bb = cproj(wuqr, h * 32, 32)
                        rope_comb(qr[s2][0:32, tcs(t)], Bqr, A, Ab, Bm, Bb, 32, cos32, sin32, t, tmp_r)
                        pt, pb = cproj(wuq, h * 96 + 32, 64)
                        P.act(lambda e, pt=pt, t=t, s2=s2: e.activation(out=qn[s2][0:64, tcs(t)], in_=pt[0:64, :], func=AF.Identity),
                              reads=[pb], writes=[Bqn])
                        pt, pb = PSA.get()
                        mm(pt[0:64, :], wukv[:, h * 128:h * 128 + 64], ckvT[:, tcs(t)], True, True, [Bw, Bckv], [pb])
                        P.act(lambda e, pt=pt, t=t, s2=s2: e.activation(out=kn[s2][0:64, tcs(t)], in_=pt[0:64, :], func=AF.Identity),
                              reads=[pb], writes=[Bkn])
                    for g in range(2):
                        pt, pb = PSA.get()
                        for j in range(8):
                            i = g * 8 + j
                            mm(pt[:, j * 64:(j + 1) * 64], ckvT[:, i * 128:(i + 1) * 128], wukv[:, h * 128 + 64:h * 128 + 128],
                               True, True, [Bw, Bckv], [pb])
                        P.dve(lambda e, pt=pt, g=g, s2=s2: e.tensor_copy(Vh[s2][:, g * 8:(g + 1) * 8, :],
                                                                        pt[:, :].rearrange("p (j d) -> p j d", j=8)),
                              reads=[pb], writes=[BV])
                    for c in range(NCH):
                        O, Ob = PSB.get()
                        Dn, Db = PSC.get()
                        nk = 4 * c + 4
                        for kt in range(nk):
                            j = kt - 4 * c
                            q0 = max(j, 0) * 128
                            qs = slice(c * 512 + q0, (c + 1) * 512)
                            ks = slice(kt * 128, (kt + 1) * 128)
                            pt, pb = PSA.get()
                            mm(pt[:, q0:512], kpeT[0:32, ks], qr[s2][0:32, qs], True, False, [Bkpe, Bqr], [pb])
                            mm(pt[:, q0:512], kn[s2][0:64, ks], qn[s2][0:64, qs], False, True, [Bkn, Bqn], [pb])
                            PT, PTb = PT_r.get()
                            P.act(lambda e, PT=PT, pt=pt, q0=q0: e.activation(out=PT[:, q0:512], in_=pt[:, q0:512], func=AF.Exp, scale=sc_a),
                                  reads=[pb], writes=[PTb])
                            if j >= 0:
                                P.dve(lambda e, PT=PT, q0=q0: e.tensor_tensor(PT[:, q0:q0 + 128], PT[:, q0:q0 + 128], tri_b[:], ALU.mult),
                                      reads=[PTb, B_const], writes=[PTb])
                            mm(O[0:64, q0:512], Vh[s2][:, kt, :], PT[:, q0:512], kt == 0, kt == nk - 1, [BV, PTb], [Ob])
                            mm(Dn[0:64, q0:512], ones_b[:, 0:64], PT[:, q0:512], kt == 0, kt == nk - 1, [B_const, PTb], [Db])
                        P.dve(lambda e, Dn=Dn: e.reciprocal(rD[:], Dn[0:64, :]), reads=[Db], writes=[rDb])
                        o_t, o_b = o_r.get()
                        P.dve(lambda e, O=O, o_t=o_t: e.tensor_tensor(o_t[:], O[0:64, :], rD[:], ALU.mult),
                              reads=[Ob, rDb], writes=[o_b])
                        P.dma("sp", lambda e, o_t=o_t, h=h, c=c: e.dma_start(out=oa_s[h, :, tcs(c)], in_=o_t[:]),
                              reads=[o_b], writes=[B_oa])
                P.barrier()

            for es in phase("conv"):
                cw = sb(es, "cw", [128, 4, 3], F32)
                Bcw = Buf()
                load_w("sp", cw[:], conv_wT_d[l], Bcw)
                wc_r = Ring([(sb(es, "wc%d" % i, [128, 8, 3, 128], BF16), Buf()) for i in range(2)])
                u, Bu = sb(es, "u", [128, S + 2], F32), Buf()
                y, By = sb(es, "y", [128, S], F32), Buf()
                gb, Bgb = sb(es, "gb", [128, S], BF16), Buf()
                ob_r = Ring([(sb(es, "obt%d" % i, [128, S], BF16), Buf()) for i in range(2)])
                tmp_r = Ring([(sb(es, "ctmp%d" % i, [128, 512], F32), Buf()) for i in range(2)])
                P.dve(lambda e: e.memset(u[:, 0:2], 0.0), writes=[Bu])
                for j in range(4):
                    wc, wcb = wc_r.get()
                    for k in range(3):
                        load_w("pool", wc[:, :, k, :], wsrc(416 + k * 512 + j * 128, 128), wcb)
                    for t in range(NCH):
                        def cp(k, t=t, wc=wc, wcb=wcb):
                            pt, pb = PSA.get()
                            for kc in range(8):
                                mm(pt[:, :], wc[:, kc, k, :], hT[:, kc, tcs(t)], kc == 0, kc == 7, [wcb, B_hT[kc][t]], [pb])
                            return pt, pb
                        pgc, pgcb = cp(1)
                        phc, phcb = cp(2)
                        pgb, pgbb = cp(0)
                        tm, tmb = tmp_r.get()
                        P.act(lambda e, tm=tm, pgc=pgc: e.activation(out=tm[:], in_=pgc[:, :], func=AF.Identity), reads=[pgcb], writes=[tmb])
                        P.dve(lambda e, tm=tm, phc=phc, t=t: e.tensor_tensor(u[:, 2 + t * 512:2 + (t + 1) * 512], tm[:], phc[:, :], ALU.mult),
                              reads=[tmb, phcb], writes=[Bu])
                        P.act(lambda e, pgb=pgb, t=t: e.activation(out=gb[:, tcs(t)], in_=pgb[:, :], func=AF.Identity), reads=[pgbb], writes=[Bgb])
                    P.dve(lambda e, j=j: e.tensor_scalar(y[:], u[:, 2:S + 2], cw[:, j, 2:3], None, ALU.mult), reads=[Bu, Bcw], writes=[By])
                    P.dve(lambda e, j=j: e.scalar_tensor_tensor(out=y[:], in0=u[:, 1:S + 1], scalar=cw[:, j, 1:2], in1=y[:], op0=ALU.mult, op1=ALU.add),
                          reads=[Bu, Bcw, By], writes=[By])
                    P.dve(lambda e, j=j: e.scalar_tensor_tensor(out=y[:], in0=u[:, 0:S], scalar=cw[:, j, 0:1], in1=y[:], op0=ALU.mult, op1=ALU.add),
                          reads=[Bu, Bcw, By], writes=[By])
                    ot, otb = ob_r.get()
                    P.dve(lambda e, ot=ot: e.tensor_tensor(ot[:], y[:], gb[:], ALU.mult), reads=[By, Bgb], writes=[otb])
                    P.dma("sp", lambda e, ot=ot, j=j: e.dma_start(out=ob_s[j], in_=ot[:]), reads=[otb], writes=[B_ob])
                P.barrier()

            for es in phase("d1"):
                qiT, kiT = sb(es, "qiT", [64, 8, S], BF16), sb(es, "kiT", [64, S], BF16)
                iw = sb(es, "iw", [128, NT, 8], F32)
                Bqi, Bki, Biw = Buf(), Buf(), Buf()
                with ExitStack() as es2:
                    wiq, wiqr = sb(es2, "wiq", [128, 8, 512], BF16), sb(es2, "wiqr", [128, 8, 144], BF16)
                    wik, wikr = sb(es2, "wik", [128, 8, 128], BF16), sb(es2, "wikr", [128, 8, 128], BF16)
                    tmp_r = Ring([(sb(es2, "dtmp%d" % i, [128, 512], F32), Buf()) for i in range(4)])
                    Bw = Buf()
                    load_w("pool", wiq[:], wsrc(2592, 512), Bw)
                    load_w("pool", wiqr[:], wrsrc(176, 144), Bw)
                    load_w("pool", wik[:], wsrc(3104, 128), Bw)
                    load_w("pool", wikr[:], wrsrc(208, 128), Bw)
                    for t in range(NCH):
                        for h in range(8 if "d1p_q" in run_phases else 0):
                            A, Ab = proj(wiq, Bw, h * 64, 64, t)
                            Bm, Bb = proj(wiqr, Bw, h * 16, 32, t)
                            P.act(lambda e, A=A, h=h, t=t: e.activation(out=qiT[0:64, h, tcs(t)], in_=A[0:64, :], func=AF.Identity),
                                  reads=[Ab], writes=[Ab, Bqi])
                            rope_comb(qiT[0:32, h, tcs(t)], Bqi, A, Ab, Bm, Bb, 32, cos16, sin16, t, tmp_r)
                        if "d1p_k" in run_phases:
                            A, Ab = proj(wik, Bw, 0, 64, t)
                            Bm, Bb = proj(wikr, Bw, 96, 32, t)
                            P.act(lambda e, A=A, t=t: e.activation(out=kiT[0:64, tcs(t)], in_=A[0:64, :], func=AF.Identity), reads=[Ab], writes=[Ab, Bki])
                            rope_comb(kiT[0:32, tcs(t)], Bki, A, Ab, Bm, Bb, 32, cos16, sin16, t, tmp_r)
                        for jj in range(4 if "d1p_w" in run_phases else 0):
                            i = t * 4 + jj
                            pt, pb = PSA.get()
                            for kc in range(8):
                                mm(pt[:, 0:64], hT[:, kc, i * 128:(i + 1) * 128], wik[:, kc, 64:128], kc == 0, kc == 7, [Bw, B_hT[kc][t]], [pb])
                            P.dve(lambda e, pt=pt, i=i: e.tensor_copy(iw[:, i, :], pt[:, 0:8]), reads=[pb], writes=[Biw])
                    P.barrier()
                sc, Bsc = sb(es, "sc", [128, S], F32), Buf()
                rl_r = Ring([(sb(es, "rl%d" % i, [128, 512], BF16), Buf()) for i in range(3)])
                dg, Bdg = sb(es, "dg", [128, 8, 128], BF16), Buf()
                mk, Bmk = sb(es, "mk", [128, S], BF16), Buf()
                mT_r = Ring([(sb(es, "mT%d" % i, [128, 4, 128], BF16), Buf()) for i in range(2)])
                st = sb(es, "st", [128, 64], F32)
                Bst = Buf()
                NIT = 18
                ACC = [PSB.get(), PSB.get(), PSC.get(), PSC.get()]
                for qt in range(NT if "d1_score" in run_phases else 0):
                    nk = (qt + 1) * 128
                    nkc = (nk + 511) // 512
                    for h in range(8):
                        P.dve(lambda e, h=h, qt=qt: e.tensor_scalar(dg[:, h, :], identb[:], iw[:, qt, h:h + 1], None, ALU.mult),
                              reads=[B_const, Biw], writes=[Bdg])
                    for h in range(8):
                        for kk in range(nkc):
                            w = min(512, nk - kk * 512)
                            pt, pb = PSA.get()
                            mm(pt[:, 0:w], qiT[0:64, h, qt * 128:(qt + 1) * 128], kiT[0:64, kk * 512:kk * 512 + w], True, True, [Bqi, Bki], [pb])
                            rl, rlb = rl_r.get()
                            P.act(lambda e, rl=rl, pt=pt, w=w: e.activation(out=rl[:, 0:w], in_=pt[:, 0:w], func=AF.Relu), reads=[pb], writes=[rlb])
                            at, ab = ACC[kk]
                            mm(at[:, 0:w], dg[:, h, :], rl[:, 0:w], h == 0, h == 7, [Bdg, rlb], [ab])
                    for kk in range(nkc):
                        w = min(512, nk - kk * 512)
                        at, ab = ACC[kk]
                        P.act(lambda e, at=at, kk=kk, w=w: e.activation(out=sc[:, kk * 512:kk * 512 + w], in_=at[:, 0:w], func=AF.Identity),
                              reads=[ab], writes=[Bsc])
                    if "d1_bis" not in run_phases:
                        continue
                    if nk <= 256:
                        P.dve(lambda e: e.memset(st[:, 0:1], -1.0e29), writes=[Bst])
                        P.dve(lambda e, qt=qt: e.tensor_tensor(sc[:, qt * 128:(qt + 1) * 128], sc[:, qt * 128:(qt + 1) * 128], ntri[:], ALU.add),
                              reads=[Bsc, B_const], writes=[Bsc])
                    else:
                        P.dve(lambda e, nk=nk: e.tensor_reduce(out=st[:, 1:2], in_=sc[:, 0:nk], axis=AX.X, op=ALU.min), reads=[Bsc], writes=[Bst])
                        P.dve(lambda e, nk=nk: e.tensor_reduce(out=st[:, 2:3], in_=sc[:, 0:nk], axis=AX.X, op=ALU.max), reads=[Bsc], writes=[Bst])
                        P.dve(lambda e, qt=qt: e.tensor_tensor(sc[:, qt * 128:(qt + 1) * 128], sc[:, qt * 128:(qt + 1) * 128], ntri[:], ALU.add),
                              reads=[Bsc, B_const], writes=[Bsc])
                        P.dve(lambda e: e.tensor_tensor(st[:, 3:4], st[:, 2:3], st[:, 1:2], ALU.subtract), reads=[Bst], writes=[Bst])
                        P.dve(lambda e: e.tensor_scalar(st[:, 3:4], st[:, 3:4], 1.0001, 1e-6, ALU.mult, ALU.add), reads=[Bst], writes=[Bst])
                        P.dve(lambda e: e.tensor_copy(st[:, 0:1], st[:, 1:2]), reads=[Bst], writes=[Bst])
                        for i in range(NIT):
                            P.dve(lambda e, i=i: e.tensor_scalar(st[:, 8 + i:9 + i], st[:, 3:4], 2.0 ** -(i + 1), None, ALU.mult), reads=[Bst], writes=[Bst])
                        for i in range(NIT):
                            P.dve(lambda e, i=i: e.tensor_tensor(st[:, 4:5], st[:, 0:1], st[:, 8 + i:9 + i], ALU.add), reads=[Bst], writes=[Bst])
                            P.dve(lambda e, nk=nk: e.tensor_scalar(mk[:, 0:nk], sc[:, 0:nk], st[:, 4:5], None, ALU.is_ge, ALU.add, accum_out=st[:, 5:6]),
                                  reads=[Bsc, Bst], writes=[Bmk, Bst])
                            P.dve(lambda e, i=i: e.tensor_scalar(st[:, 6:7], st[:, 5:6], 255.5, st[:, 8 + i:9 + i], ALU.is_ge, ALU.mult), reads=[Bst], writes=[Bst])
                            P.dve(lambda e: e.tensor_tensor(st[:, 0:1], st[:, 0:1], st[:, 6:7], ALU.add), reads=[Bst], writes=[Bst])
                    if "d1_mask" not in run_phases:
                        continue
                    P.dve(lambda e, nk=nk: e.tensor_scalar(mk[:, 0:nk], sc[:, 0:nk], st[:, 0:1], None, ALU.is_ge), reads=[Bsc, Bst], writes=[Bmk])
                    for g in range((qt + 4) // 4):
                        k0 = g * 4
                        nb = min(4, qt + 1 - k0)
                        pt, pb = PSA.get()
                        for jj in range(nb):
                            kt = k0 + jj
                            mm(pt[:, jj * 128:(jj + 1) * 128], mk[:, kt * 128:(kt + 1) * 128], identb[:], True, True, [Bmk, B_const], [pb])
                        mT, mTb = mT_r.get()
                        P.act(lambda e, mT=mT, pt=pt, nb=nb: e.activation(out=mT[:, 0:nb, :], in_=pt[:, 0:nb * 128].rearrange("p (k q) -> p k q", k=nb), func=AF.Identity),
                              reads=[pb], writes=[mTb])
                        P.dma("sp", lambda e, mT=mT, k0=k0, nb=nb, qt=qt: e.dma_start(
                            out=mask_s[k0:k0 + nb, :, qt * 128:(qt + 1) * 128].rearrange("k p q -> p k q"), in_=mT[:, 0:nb, :]),
                            reads=[mTb], writes=[B_mask])
                P.barrier()

            for es in phase("d2"):
                qcT, kcT = sb(es, "qcT", [64, 8, S], BF16), sb(es, "kcT", [64, S], BF16)
                Vc = sb(es, "Vc", [128, NT, 64], BF16)
                Bqc, Bkc, BVc = Buf(), Buf(), Buf()
                with ExitStack() as es2:
                    wdq, wdqr = sb(es2, "wdq", [128, 8, 512], BF16), sb(es2, "wdqr", [128, 8, 144], BF16)
                    wkc, wkcr = sb(es2, "wkc", [128, 8, 128], BF16), sb(es2, "wkcr", [128, 8, 128], BF16)
                    tmp_r = Ring([(sb(es2, "etmp%d" % i, [128, 512], F32), Buf()) for i in range(4)])
                    Bw = Buf()
                    load_w("pool", wdq[:], wsrc(1952, 512), Bw)
                    load_w("pool", wdqr[:], wrsrc(32, 144), Bw)
                    load_w("pool", wkc[:], wsrc(2464, 128), Bw)
                    load_w("pool", wkcr[:], wrsrc(128, 128), Bw)
                    for t in range(NCH):
                        for h in range(8):
                            A, Ab = proj(wdq, Bw, h * 64, 64, t)
                            Bm, Bb = proj(wdqr, Bw, h * 16, 32, t)
                            P.act(lambda e, A=A, h=h, t=t: e.activation(out=qcT[0:64, h, tcs(t)], in_=A[0:64, :], func=AF.Identity), reads=[Ab], writes=[Ab, Bqc])
                            rope_comb(qcT[0:32, h, tcs(t)], Bqc, A, Ab, Bm, Bb, 32, cos16, sin16, t, tmp_r)
                        A, Ab = proj(wkc, Bw, 0, 64, t)
                        Bm, Bb = proj(wkcr, Bw, 32, 32, t)
                        P.act(lambda e, A=A, t=t: e.activation(out=kcT[0:64, tcs(t)], in_=A[0:64, :], func=AF.Identity), reads=[Ab], writes=[Ab, Bkc])
                        rope_comb(kcT[0:32, tcs(t)], Bkc, A, Ab, Bm, Bb, 32, cos16, sin16, t, tmp_r)
                        pt, pb = PSA.get()
                        for jj in range(4):
                            i = t * 4 + jj
                            for kc in range(8):
                                mm(pt[:, jj * 64:(jj + 1) * 64], hT[:, kc, i * 128:(i + 1) * 128], wkc[:, kc, 64:128], kc == 0, kc == 7, [Bw, B_hT[kc][t]], [pb])
                        P.dve(lambda e, pt=pt, t=t: e.tensor_copy(Vc[:, t * 4:(t + 1) * 4, :], pt[:, 0:256].rearrange("p (j d) -> p j d", j=4)),
                              reads=[pb], writes=[BVc])
                    P.barrier()
                mTc, BmTc = sb(es, "mTc", [128, NT, 512], BF16), Buf()
                PT_r = Ring([(sb(es, "PTc%d" % i, [128, 512], BF16), Buf()) for i in range(3)])
                rD, rDb = sb(es, "rDc", [64, 512], F32), Buf()
                o_r = Ring([(sb(es, "oc%d" % i, [64, 512], BF16), Buf()) for i in range(2)])
                sc_c = 64.0 ** -0.5
                for c in range(NCH):
                    nk = 4 * c + 4
                    for kt in range(nk):
                        q0 = max(kt - 4 * c, 0) * 128
                        P.dma("sp", lambda e, kt=kt, q0=q0, c=c: e.dma_start(out=mTc[:, kt, q0:512], in_=mask_s[kt, :, c * 512 + q0:(c + 1) * 512]),
                              reads=[B_mask], writes=[BmTc])
                    for h in range(8):
                        O, Ob = PSB.get()
                        Dn, Db = PSC.get()
                        for kt in range(nk):
                            q0 = max(kt - 4 * c, 0) * 128
                            qs = slice(c * 512 + q0, (c + 1) * 512)
                            ks = slice(kt * 128, (kt + 1) * 128)
                            pt, pb = PSA.get()
                            mm(pt[:, q0:512], kcT[0:64, ks], qcT[0:64, h, qs], True, True, [Bkc, Bqc], [pb])
                            PT, PTb = PT_r.get()
                            P.act(lambda e, PT=PT, pt=pt, q0=q0: e.activation(out=PT[:, q0:512], in_=pt[:, q0:512], func=AF.Exp, scale=sc_c), reads=[pb], writes=[PTb])
                            P.dve(lambda e, PT=PT, q0=q0, kt=kt: e.tensor_tensor(PT[:, q0:512], PT[:, q0:512], mTc[:, kt, q0:512], ALU.mult),
                                  reads=[PTb, BmTc], writes=[PTb])
                            mm(O[0:64, q0:512], Vc[:, kt, :], PT[:, q0:512], kt == 0, kt == nk - 1, [BVc, PTb], [Ob])
                            mm(Dn[0:64, q0:512], ones_b[:, 0:64], PT[:, q0:512], kt == 0, kt == nk - 1, [B_const, PTb], [Db])
                        P.dve(lambda e, Dn=Dn: e.reciprocal(rD[:], Dn[0:64, :]), reads=[Db], writes=[rDb])
                        o_t, o_b = o_r.get()
                        P.dve(lambda e, O=O, o_t=o_t: e.tensor_tensor(o_t[:], O[0:64, :], rD[:], ALU.mult), reads=[Ob, rDb], writes=[o_b])
                        P.dma("sp", lambda e, o_t=o_t, h=h, c=c: e.dma_start(out=oc_s[h, :, tcs(c)], in_=o_t[:]), reads=[o_b], writes=[B_oc])
                P.barrier()

            for es in phase("merge"):
                wo, Bwo = sb(es, "wo", [128, 8, D], BF16), Buf()
                load_w("pool", wo[:], w_out_d[l].rearrange("(kc p) n -> p kc n", p=128), Bwo)
                bg, Bbg = sb(es, "bg", [128, 24], F32), Buf()
                load_w("sp", bg[:], b_gateT_d[l], Bbg)
                oa_t, ob_t, oc_t = sb(es, "oa_t", [64, 8, 512], BF16), sb(es, "ob_t", [128, 4, 512], BF16), sb(es, "oc_t", [64, 8, 512], BF16)
                Bo = Buf()
                wg_r = Ring([(sb(es, "wg%d" % i, [128, 8, 3, 128], BF16), sb(es, "wba%d" % i, [64, 8, 128], BF16),
                              sb(es, "wbb%d" % i, [128, 4, 128], BF16), sb(es, "wbc%d" % i, [64, 8, 128], BF16), Buf()) for i in range(2)])
                g_r = Ring([(sb(es, "g%d" % i, [128, 512], BF16), Buf()) for i in range(2)])
                a3 = [(sb(es, "a3%d" % i, [128, 512], F32), Buf()) for i in range(3)]
                mg, Bmg = sb(es, "mg", [128, 8, 512], BF16), Buf()
                for t in range(NCH):
                    P.dma("sp", lambda e, t=t: e.dma_start(out=oa_t[:], in_=oa_s[:, :, tcs(t)].rearrange("h p t -> p h t")), reads=[B_oa], writes=[Bo])
                    P.dma("sp", lambda e, t=t: e.dma_start(out=ob_t[:], in_=ob_s[:, :, tcs(t)].rearrange("h p t -> p h t")), reads=[B_ob], writes=[Bo])
                    P.dma("sp", lambda e, t=t: e.dma_start(out=oc_t[:], in_=oc_s[:, :, tcs(t)].rearrange("h p t -> p h t")), reads=[B_oc], writes=[Bo])
                    for dc in range(8):
                        wg, wba, wbb, wbc, Bwg = wg_r.get()
                        for n3 in range(3):
                            load_w("pool", wg[:, :, n3, :], wsrc(3176 + n3 * 1024 + dc * 128, 128), Bwg)
                        load_w("pool", wba[:], w_br_d[l, 0, :, dc * 128:(dc + 1) * 128].rearrange("(h p) n -> p h n", p=64), Bwg)
                        load_w("pool", wbb[:], w_br_d[l, 1, :, dc * 128:(dc + 1) * 128].rearrange("(h p) n -> p h n", p=128), Bwg)
                        load_w("pool", wbc[:], w_br_d[l, 2, :, dc * 128:(dc + 1) * 128].rearrange("(h p) n -> p h n", p=64), Bwg)
                        for n3 in range(3):
                            py, pyb = PSA.get()
                            if n3 == 1:
                                for k in range(4):
                                    mm(py[:, :], wbb[:, k, :], ob_t[:, k, :], k == 0, k == 3, [Bwg, Bo], [pyb])
                            else:
                                wb_, o_ = (wba, oa_t) if n3 == 0 else (wbc, oc_t)
                                for k in range(8):
                                    mm(py[:, :], wb_[0:64, k, :], o_[0:64, k, :], k == 0, k == 7, [Bwg, Bo], [pyb])
                            pg, pgb = PSA.get()
                            for kc in range(8):
                                mm(pg[:, :], wg[:, kc, n3, :], hT[:, kc, tcs(t)], kc == 0, kc == 7, [Bwg, B_hT[kc][t]], [pgb])
                            g_t, g_b = g_r.get()
                            P.act(lambda e, g_t=g_t, pg=pg, n3=n3, dc=dc: e.activation(out=g_t[:], in_=pg[:, :], func=AF.Sigmoid,
                                                                                     bias=bg[:, n3 * 8 + dc:n3 * 8 + dc + 1]),
                                  reads=[pgb, Bbg], writes=[g_b])
                            P.dve(lambda e, n3=n3, py=py, g_t=g_t: e.tensor_tensor(a3[n3][0][:], py[:, :], g_t[:], ALU.mult),
                                  reads=[pyb, g_b], writes=[a3[n3][1]])
                        P.dve(lambda e: e.tensor_tensor(a3[0][0][:], a3[0][0][:], a3[1][0][:], ALU.add), reads=[a3[0][1], a3[1][1]], writes=[a3[0][1]])
                        P.dve(lambda e, dc=dc: e.tensor_tensor(mg[:, dc, :], a3[0][0][:], a3[2][0][:], ALU.add), reads=[a3[0][1], a3[2][1]], writes=[Bmg])
                    for dc2 in range(8):
                        po, pob = PSA.get()
                        for dc in range(8):
                            mm(po[:, :], wo[:, dc, dc2 * 128:(dc2 + 1) * 128], mg[:, dc, :], dc == 0, dc == 7, [Bwo, Bmg], [pob])
                        P.dve(lambda e, po=po, dc2=dc2, t=t: e.scalar_tensor_tensor(out=xT[:, dc2, tcs(t)], in0=po[:, :], scalar=g1c[:, dc2:dc2 + 1],
                                                                                  in1=xT[:, dc2, tcs(t)], op0=ALU.mult, op1=ALU.add),
                              reads=[pob, B_mod, B_xT[dc2][t]], writes=[B_xT[dc2][t]])
                P.barrier()

            if dbg and l == 0 and b == 0:
                P.dma("sp", lambda e: e.dma_start(out=dbg_out["x1"][:, :, :], in_=xT[:]),
                      reads=[bb for r in B_xT for bb in r], writes=[B_out])
                P.barrier()
            for es in phase("moe"):
                rw, Brw = sb(es, "rw", [128, 8, NEXP], F32), Buf()
                rb, b2t = sb(es, "rb", [1, NEXP], F32), sb(es, "b2t", [NEXP, D], F32)
                load_w("sp", rw[:], rw_d[l].rearrange("(kc p) n -> p kc n", p=128), Brw)
                load_w("sp", rb[:], rb_d[l], Brw)
                load_w("sp", b2t[:], b2_d[l], Brw)
                w1, b1 = sb(es, "w1", [128, 8, 2 * D], BF16), sb(es, "b1", [128, 16], F32)
                Bw1 = [Buf(), Buf()]
                Bb1 = Buf()
                b1p = sb(es, "b1p", [128, 8], F32)
                w2, Bw2 = sb(es, "w2", [128, 8, D], BF16), Buf()
                gl, Bgl = sb(es, "gl", [128, 8, 512], BF16), [Buf() for _ in range(8)]
                gwT, BgwT = sb(es, "gwT", [NEXP, S], F32), [Buf() for _ in range(NCH)]
                gwb, Bgwb = sb(es, "gwb", [128, 512], F32), Buf()
                with ExitStack() as es2:
                    sq_r = Ring([(sb(es2, "msq%d" % i, [128, 512], F32), Buf()) for i in range(2)])
                    rs_t, rs_b = sb(es2, "mrs", [128, 512], F32), Buf()
                    lg, Blg = sb(es2, "lg", [128, NEXP], F32), Buf()
                    ex = sb(es2, "ex", [128, NEXP], F32)
                    mk4 = sb(es2, "mk4", [128, NEXP], F32)
                    t8 = sb(es2, "t8", [128, 16], F32)
                    RL = [PSB.get(), PSB.get(), PSC.get(), PSC.get()]
                    for t in range(NCH):
                        def router_extra(c, sq, sqb, t=t):
                            for jj in range(4):
                                rt, rtb = RL[jj]
                                mm(rt[:, 0:NEXP], sq[:, jj * 128:(jj + 1) * 128], rw[:, c, :], c == 0, False, [sqb, Brw], [rtb])
                        norm_mod("n2", gs2, sh2, t, lambda c, t=t: hT[:, c, tcs(t)], lambda c, t=t: [B_hT[c][t]],
                                 (sq_r, rs_t, rs_b), extra=router_extra)
                        for jj in range(4):
                            rt, rtb = RL[jj]
                            mm(rt[:, 0:NEXP], ones_f[0:1, :], rb[0:1, :], False, True, [B_const, Brw], [rtb])
                            P.dve(lambda e, rt=rt: e.tensor_copy(lg[:], rt[:, 0:NEXP]), reads=[rtb], writes=[Blg])
                            P.dve(lambda e: e.max(out=t8[:, 0:8], in_=lg[:]), reads=[Blg], writes=[Blg])
                            P.dve(lambda e: e.tensor_scalar(t8[:, 8:9], t8[:, 0:1], -1.0, None, ALU.mult), reads=[Blg], writes=[Blg])
                            P.act(lambda e: e.activation(out=ex[:], in_=lg[:], func=AF.Exp, bias=t8[:, 8:9]), reads=[Blg], writes=[Blg])
                            P.dve(lambda e: e.tensor_scalar(mk4[:], lg[:], t8[:, 3:4], None, ALU.is_ge), reads=[Blg], writes=[Blg])
                            P.dve(lambda e: e.tensor_tensor(ex[:], ex[:], mk4[:], ALU.mult), reads=[Blg], writes=[Blg])
                            P.dve(lambda e: e.tensor_reduce(out=t8[:, 9:10], in_=ex[:], axis=AX.X, op=ALU.add), reads=[Blg], writes=[Blg])
                            P.dve(lambda e: e.reciprocal(t8[:, 10:11], t8[:, 9:10]), reads=[Blg], writes=[Blg])
                            P.dve(lambda e: e.tensor_scalar(ex[:], ex[:], t8[:, 10:11], None, ALU.mult), reads=[Blg], writes=[Blg])
                            pt, pb = PSA.get()
                            mm(pt[0:NEXP, 0:128], ex[:], ident[:], True, True, [Blg, B_const], [pb])
                            i = t * 4 + jj
                            P.dve(lambda e, pt=pt, i=i: e.tensor_copy(gwT[:, i * 128:(i + 1) * 128], pt[0:NEXP, 0:128]), reads=[pb], writes=[BgwT[t]])
                    P.barrier()
                et_r = Ring([(sb(es, "et%d" % i, [128, 512], F32), Buf()) for i in range(6)])
                for ei in range(NEXP):
                    for hf in range(2):
                        load_w("pool", w1[:, :, hf * D:(hf + 1) * D], w1_d[l, ei, :, hf * D:(hf + 1) * D].rearrange("(kc p) n -> p kc n", p=128), Bw1[hf])
                    load_w("sp", b1[:], b1T_d[l, ei], Bb1)
                    P.dve(lambda e: e.tensor_scalar(b1p[:], b1[:, 8:16], 1.0, None, ALU.add), reads=[Bb1], writes=[Bb1])
                    load_w("pool", w2[:], w2_d[l, ei].rearrange("(kc p) n -> p kc n", p=128), Bw2)
                    for t in range(NCH):
                        pt, pb = PSA.get()
                        mm(pt[:, :], ident[0:NEXP, ei:ei + 1].to_broadcast([NEXP, 128]), gwT[:, tcs(t)], True, True, [B_const, BgwT[t]], [pb])
                        P.act(lambda e, pt=pt: e.activation(out=gwb[:], in_=pt[:, :], func=AF.Identity), reads=[pb], writes=[Bgwb])
                        for fc in range(8):
                            bw = Bw1[fc // 4]
                            pg, pgb = PSA.get()
                            for kc in range(8):
                                mm(pg[:, :], w1[:, kc, fc * 256:(fc + 1) * 256:2], hT[:, kc, tcs(t)], kc == 0, kc == 7, [bw, B_hT[kc][t]], [pgb])
                            pu, pub = PSA.get()
                            for kc in range(8):
                                mm(pu[:, :], w1[:, kc, fc * 256 + 1:(fc + 1) * 256:2], hT[:, kc, tcs(t)], kc == 0, kc == 7, [bw, B_hT[kc][t]], [pub])
                            gt, gtb = et_r.get()
                            sg, sgb = et_r.get()
                            ut, utb = et_r.get()
                            P.dve(lambda e, gt=gt, pg=pg, fc=fc: e.tensor_scalar(gt[:], pg[:, :], b1[:, fc:fc + 1], 7.0, ALU.add, ALU.min),
                                  reads=[pgb, Bb1], writes=[gtb])
                            P.act(lambda e, sg=sg, gt=gt: e.activation(out=sg[:], in_=gt[:], func=AF.Sigmoid, scale=1.702), reads=[gtb], writes=[sgb])
                            P.act(lambda e, ut=ut, pu=pu, fc=fc: e.activation(out=ut[:], in_=pu[:, :], func=AF.Identity, bias=b1p[:, fc:fc + 1]),
                                  reads=[pub, Bb1], writes=[utb])
                            P.dve(lambda e, ut=ut: e.tensor_scalar(ut[:], ut[:], 8.0, -6.0, ALU.min, ALU.max), reads=[utb], writes=[utb])
                            P.dve(lambda e, gt=gt, sg=sg: e.tensor_tensor(gt[:], gt[:], sg[:], ALU.mult), reads=[gtb, sgb], writes=[gtb])
                            P.dve(lambda e, gt=gt, ut=ut: e.tensor_tensor(gt[:], gt[:], ut[:], ALU.mult), reads=[gtb, utb], writes=[gtb])
                            P.dve(lambda e, gt=gt, fc=fc: e.tensor_tensor(gl[:, fc, :], gt[:], gwb[:], ALU.mult), reads=[gtb, Bgwb], writes=[Bgl[fc]])
                        for dc in range(8):
                            py, pyb = PSA.get()
                            for fc in range(8):
                                mm(py[:, :], w2[:, fc, dc * 128:(dc + 1) * 128], gl[:, fc, :], fc == 0, fc == 7, [Bw2, Bgl[fc]], [pyb])
                            P.dve(lambda e, py=py, dc=dc, t=t: e.scalar_tensor_tensor(out=xT[:, dc, tcs(t)], in0=py[:, :], scalar=g2c[:, dc:dc + 1],
                                                                                    in1=xT[:, dc, tcs(t)], op0=ALU.mult, op1=ALU.add),
                                  reads=[pyb, B_mod, B_xT[dc][t]], writes=[B_xT[dc][t]])
                for t in range(NCH):
                    for dc in range(8):
                        py, pyb = PSA.get()
                        mm(py[:, :], b2t[:, dc * 128:(dc + 1) * 128], gwT[:, tcs(t)], True, True, [Brw, BgwT[t]], [pyb])
                        P.dve(lambda e, py=py, dc=dc, t=t: e.scalar_tensor_tensor(out=xT[:, dc, tcs(t)], in0=py[:, :], scalar=g2c[:, dc:dc + 1],
                                                                                in1=xT[:, dc, tcs(t)], op0=ALU.mult, op1=ALU.add),
                              reads=[pyb, B_mod, B_xT[dc][t]], writes=[B_xT[dc][t]])
                P.barrier()

            if dbg and l == 0 and b == 0:
                P.dma("sp", lambda e: e.dma_start(out=dbg_out["x2"][:, :, :], in_=xT[:]),
                      reads=[bb for r in B_xT for bb in r], writes=[B_out])
                P.barrier()
        with ExitStack() as es:
            sq_r = Ring([(sb(es, "fsq%d" % i, [128, 512], F32), Buf()) for i in range(3)])
            rs_t, rs_b = sb(es, "frs", [128, 512], F32), Buf()
            fin = sb(es, "fin", [128, 8, 512], F32)
            Bfin = [Buf() for _ in range(8)]
            zero8 = sb(es, "zero8", [128, 8], F32)
            Bz = Buf()
            P.dve(lambda e: e.memset(zero8[:], 0.0), writes=[B_mod])
            orow_r = Ring([(sb(es, "orow%d" % i, [128, D], F32), Buf()) for i in range(2)])
            for t in range(NCH):
                norm_mod("nf", fg, zero8, t, lambda c: fin[:, c, :], lambda c: [Bfin[c]], (sq_r, rs_t, rs_b))
                for jj in range(4):
                    orow, orb = orow_r.get()
                    for g in range(2):
                        pt, pb = PSA.get()
                        for k in range(4):
                            c = g * 4 + k
                            mm(pt[:, k * 128:(k + 1) * 128], fin[:, c, jj * 128:(jj + 1) * 128], ident[:], True, True, [Bfin[c], B_const], [pb])
                        P.act(lambda e, orow=orow, pt=pt, g=g: e.activation(out=orow[:, g * 512:(g + 1) * 512], in_=pt[:, :], func=AF.Identity), reads=[pb], writes=[orb])
                    i = t * 4 + jj
                    P.dma("sp", lambda e, orow=orow, i=i: e.dma_start(out=out_d[b, i * 128:(i + 1) * 128, :], in_=orow[:]), reads=[orb], writes=[B_out])
            P.barrier()

    P.emit(final_bufs=[B_out])
    root.close()
    return nc, P


def _rot_cols(c0, n_rot):
    h = n_rot // 2
    return list(range(c0 + h, c0 + n_rot)) + list(range(c0, c0 + h))


def _prep_inputs(inp, b0, nseq):
    f = lambda a: np.ascontiguousarray(a, dtype=np.float32)
    L = inp["w_in"].shape[0]

    def colT(v, nch):
        v = np.asarray(v)
        return f(v.reshape(v.shape[:-1] + (nch, 128)).swapaxes(-1, -2))

    m = {}
    m["x"] = f(inp["x"][b0:b0 + nseq])
    m["cT"] = f(np.asarray(inp["c"])[b0:b0 + nseq].reshape(nseq, 8, 128).transpose(2, 1, 0))
    m["pos"] = np.ascontiguousarray(np.asarray(inp["positions"])[b0:b0 + nseq], dtype=np.int32)
    m["w_ada"] = f(inp["w_ada"])
    m["b_adaT"] = colT(inp["b_ada"], 48)
    m["n1g"] = colT(inp["norm1_g"], 8)
    m["n2g"] = colT(inp["norm2_g"], 8)
    m["fg"] = colT(inp["final_g"], 8)
    w_in = np.asarray(inp["w_in"])
    m["w_in"] = f(w_in)
    rot = _rot_cols(384, 32)
    for h in range(8):
        rot += _rot_cols(1952 + h * 64, 16)
    rot += _rot_cols(2464, 16)
    for h in range(8):
        rot += _rot_cols(2592 + h * 64, 16)
    rot += _rot_cols(3104, 16)
    rot += rot[:16]
    m["w_in_rot"] = f(w_in[:, :, rot])
    m["b_gateT"] = colT(inp["b_gate"], 24)
    m["qng"] = colT(inp["mla_q_norm"], 2)
    w_uq = np.asarray(inp["mla_w_uq"])
    m["w_uq"] = f(w_uq)
    rq = []
    for h in range(8):
        rq += _rot_cols(h * 96, 32)
    m["w_uq_rot"] = f(w_uq[:, :, rq])
    m["kvng"] = colT(inp["mla_kv_norm"], 1)
    m["w_ukv"] = f(inp["mla_w_ukv"])
    cw = np.asarray(inp["conv_w"])
    m["conv_wT"] = f(cw.reshape(L, 3, 4, 128).transpose(0, 3, 2, 1))
    m["w_branch"] = f(inp["w_branch"])
    m["w_out"] = f(inp["w_out"])
    m["router_w"] = f(inp["router_w"])
    m["router_b"] = f(np.asarray(inp["router_b"]).reshape(L, 1, NEXP))
    m["exp_w1"] = f(inp["exp_w1"])
    b1 = np.asarray(inp["exp_b1"])
    b1g = b1[:, :, 0::2].reshape(L, NEXP, 8, 128).swapaxes(-1, -2)
    b1u = b1[:, :, 1::2].reshape(L, NEXP, 8, 128).swapaxes(-1, -2)
    m["exp_b1T"] = f(np.concatenate([b1g, b1u], axis=-1))
    m["exp_w2"] = f(inp["exp_w2"])
    m["exp_b2"] = f(inp["exp_b2"])
    return m


_CONST = None


def _consts():
    global _CONST
    if _CONST is None:
        c = {}
        c["ident"] = np.eye(128, dtype=np.float32)
        sel = np.zeros((NEXP, NEXP, 128), np.float32)
        for e in range(NEXP):
            sel[e, e, :] = 1.0
        c["sel"] = sel
        rc = np.zeros((32, 4), np.float32)
        i16 = np.arange(16, dtype=np.float32)
        invf32 = np.exp(-math.log(500000.0) * i16 * (2.0 / 32)).astype(np.float32)
        i8 = np.arange(8, dtype=np.float32)
        invf16 = np.exp(-math.log(500000.0) * i8 * (2.0 / 16)).astype(np.float32)
        rc[:, 0] = np.concatenate([invf32, invf32])
        rc[:, 1] = np.concatenate([-np.ones(16), np.ones(16)])
        rc[0:16, 2] = np.concatenate([invf16, invf16])
        rc[0:16, 3] = np.concatenate([-np.ones(8), np.ones(8)])
        c["ropec"] = rc
        k = np.arange(128)
        c["tri"] = (k[:, None] <= k[None, :]).astype(np.float32)
        c["ntri"] = np.where(k[None, :] <= k[:, None], 0.0, -BIG).astype(np.float32)
        _CONST = c
    return _CONST


_NC_CACHE = {}


def kernel(**inputs):
    n_cores = 8
    B = np.asarray(inputs["x"]).shape[0]
    nseq = B // n_cores
    if nseq not in _NC_CACHE:
        _NC_CACHE[nseq] = build(nseq)[0]
    nc = _NC_CACHE[nseq]
    consts = _consts()
    in_maps = []
    for core in range(n_cores):
        m = _prep_inputs(inputs, core * nseq, nseq)
        m.update(consts)
        in_maps.append(m)
    res = run_bass_kernel_spmd(nc, in_maps, core_ids=list(range(n_cores)))
    out = np.concatenate([np.asarray(r["out"]) for r in res.results], axis=0)
    return out.astype(np.float32)
```
